# Optimizing a Trainium2 kernel written in Bass

```python
import math
import jax, jax.numpy as jnp
from jax import lax
import numpy as np

D_MODEL = 1024
BATCH = 2
SEQ = 8192
DEPTH = 2

GRID_W = 64
D_MIX = D_MODEL
EPS = 1e-6
CONV_W = 256
CONV_K = 3
NA_HEADS = 4
NA_DH = 64
NA_W = NA_HEADS * NA_DH
NA_KH = 8
NA_KW = 16
GDN_HEADS = 4
GDN_DK = 128
GDN_DV = 128
GDN_W = GDN_HEADS * GDN_DV
GDN_CONV_K = 3
GDN_CHUNK = 64
GDN_QKV = 2 * GDN_HEADS * GDN_DK + GDN_W
IN_SPLITS = (CONV_W, CONV_W, CONV_W, NA_W, NA_W, NA_W, GDN_QKV, GDN_W, 2 * GDN_HEADS, 2 * GDN_HEADS)
D_IN = 3 * CONV_W + 3 * NA_W + GDN_QKV + GDN_W + 4 * GDN_HEADS
N_GROUPS = 4
EXPERTS_PER_GROUP = 8
N_EXPERTS = N_GROUPS * EXPERTS_PER_GROUP
TOP_K = 2
D_EXPERT = 512
MOE_BLOCK = 256

kernel_name = "hybrid_parallel_conv_na_gdn_hmoe_encoder"


def _rmsnorm(x, w):
    xf = x.astype(jnp.float32)
    y = xf * lax.rsqrt(jnp.mean(xf * xf, axis=-1, keepdims=True) + EPS)
    return (y * w.astype(jnp.float32)).astype(x.dtype)


def _l2norm(x):
    xf = x.astype(jnp.float32)
    return xf * lax.rsqrt(jnp.sum(xf * xf, axis=-1, keepdims=True) + EPS)


def _dwconv(x, w):
    ch = x.shape[-1]
    return lax.conv_general_dilated(x, w[:, None, :].astype(x.dtype), window_strides=(1,), padding='SAME',
                                    dimension_numbers=('NWC', 'WIO', 'NWC'), feature_group_count=ch)


def _neighbourhood_attention(q, k, v, rpb):
    bsz, t, h, dh = q.shape
    rows = t // GRID_W
    kh = min(NA_KH, rows)
    qg = q.reshape(bsz, rows, GRID_W, h, dh)
    kg = k.reshape(bsz, rows, GRID_W, h, dh)
    vg = v.reshape(bsz, rows, GRID_W, h, dh)
    col = jnp.arange(GRID_W)
    col_idx = jnp.clip(col - NA_KW // 2, 0, GRID_W - NA_KW)[:, None] + jnp.arange(NA_KW)[None, :]
    dc = col_idx - col[:, None] + (NA_KW - 1)
    row_start = jnp.clip(jnp.arange(rows) - kh // 2, 0, rows - kh)
    scale = dh ** -0.5

    def one_row(r):
        rs = row_start[r]
        q_r = lax.dynamic_index_in_dim(qg, r, axis=1, keepdims=False)
        k_rows = lax.dynamic_slice_in_dim(kg, rs, kh, axis=1)
        v_rows = lax.dynamic_slice_in_dim(vg, rs, kh, axis=1)
        k_win = k_rows[:, :, col_idx]
        v_win = v_rows[:, :, col_idx]
        dr = rs + jnp.arange(kh) - r + (NA_KH - 1)
        bias = rpb[:, dr][:, :, dc]
        s = jnp.einsum('bchd,brckhd->bhcrk', q_r, k_win).astype(jnp.float32) * scale
        s = s + jnp.transpose(bias, (0, 2, 1, 3)).astype(jnp.float32)[None]
        p = jax.nn.softmax(s.reshape(bsz, h, GRID_W, kh * NA_KW), axis=-1).reshape(s.shape).astype(v.dtype)
        return jnp.einsum('bhcrk,brckhd->bchd', p, v_win)

    out = lax.map(one_row, jnp.arange(rows))
    return jnp.transpose(out, (1, 0, 2, 3, 4)).reshape(bsz, t, h * dh)


def _gated_delta_chunked(q, k, v, g, beta):
    bsz, h, t, dk = q.shape
    dv = v.shape[-1]
    n = t // GDN_CHUNK
    cs = lambda a: a.reshape((bsz, h, n, GDN_CHUNK) + a.shape[3:])
    q, k, v, g, beta = cs(q), cs(k), cs(v), cs(g), cs(beta)
    g = jnp.cumsum(g, axis=-1)
    idx = jnp.arange(GDN_CHUNK)
    strict = idx[:, None] > idx[None, :]
    incl = idx[:, None] >= idx[None, :]
    diff = g[..., :, None] - g[..., None, :]
    dec_strict = jnp.exp(jnp.where(strict, diff, -jnp.inf))
    dec_incl = jnp.exp(jnp.where(incl, diff, -jnp.inf))
    kb = k * beta[..., None]
    lower = jnp.einsum('bhncd,bhnsd->bhncs', kb, k) * dec_strict
    rhs = jnp.concatenate([v * beta[..., None], kb * jnp.exp(g)[..., None]], axis=-1)
    sol = lax.linalg.triangular_solve(lower, rhs, left_side=True, lower=True, unit_diagonal=True)
    u, w = sol[..., :dv], sol[..., dv:]
    intra = jnp.einsum('bhncd,bhnsd->bhncs', q, k) * dec_incl
    q_dec = q * jnp.exp(g)[..., None]
    g_last = g[..., -1]
    k_dec = k * jnp.exp(g_last[..., None] - g)[..., None]

    def step(state, xs):
        u_c, w_c, intra_c, q_c, k_c, gl_c = xs
        v_new = u_c - jnp.einsum('bhcd,bhde->bhce', w_c, state)
        o_c = jnp.einsum('bhcd,bhde->bhce', q_c, state) + jnp.einsum('bhcs,bhse->bhce', intra_c, v_new)
        state = state * jnp.exp(gl_c)[..., None, None] + jnp.einsum('bhcd,bhce->bhde', k_c, v_new)
        return state, o_c

    xs = tuple(jnp.moveaxis(a, 2, 0) for a in (u, w, intra, q_dec, k_dec, g_last))
    s0 = jnp.zeros((bsz, h, dk, dv), jnp.float32)
    _, o = lax.scan(step, s0, xs)
    return jnp.moveaxis(o, 0, 2).reshape(bsz, h, t, dv)


def _gdn_mixer(qkv, z, a, b, conv_w, a_log, dt_bias, norm_w):
    bsz, t, _ = qkv.shape
    qkv = jax.nn.silu(_dwconv(qkv, conv_w))
    q = qkv[..., :GDN_HEADS * GDN_DK].reshape(bsz, t, GDN_HEADS, GDN_DK)
    k = qkv[..., GDN_HEADS * GDN_DK:2 * GDN_HEADS * GDN_DK].reshape(bsz, t, GDN_HEADS, GDN_DK)
    v = qkv[..., 2 * GDN_HEADS * GDN_DK:].reshape(bsz, t, GDN_HEADS, GDN_DV)
    q = jnp.transpose(_l2norm(q) * (GDN_DK ** -0.5), (0, 2, 1, 3))
    k = jnp.transpose(_l2norm(k), (0, 2, 1, 3))
    v = jnp.transpose(v.astype(jnp.float32), (0, 2, 1, 3))
    a = a.astype(jnp.float32).reshape(bsz, t, 2, GDN_HEADS)
    b = b.astype(jnp.float32).reshape(bsz, t, 2, GDN_HEADS)
    g = -jnp.exp(a_log.astype(jnp.float32)) * jax.nn.softplus(a + dt_bias.astype(jnp.float32))
    beta = jax.nn.sigmoid(b)
    g = jnp.transpose(g, (2, 0, 3, 1))
    beta = jnp.transpose(beta, (2, 0, 3, 1))
    flip = lambda a_: jnp.flip(a_, axis=2)
    o_fwd = _gated_delta_chunked(q, k, v, g[0], beta[0])
    o_bwd = flip(_gated_delta_chunked(flip(q), flip(k), flip(v), flip(g[1]), flip(beta[1])))
    o = jnp.transpose(o_fwd + o_bwd, (0, 2, 1, 3))
    o = o * lax.rsqrt(jnp.mean(o * o, axis=-1, keepdims=True) + EPS) * norm_w.astype(jnp.float32)
    o = o * jax.nn.silu(z.astype(jnp.float32).reshape(bsz, t, GDN_HEADS, GDN_DV))
    return o.reshape(bsz, t, GDN_W).astype(qkv.dtype)


def _hier_moe(h, wg, bg, we, be, w1, w3, w2):
    bsz, t, d = h.shape
    xt = h.reshape(bsz * t, d)
    n = xt.shape[0]
    pg = jax.nn.softmax((xt @ wg + bg).astype(jnp.float32), axis=-1)
    grp = jnp.argmax(pg, axis=-1)
    pg_top = jnp.take_along_axis(pg, grp[:, None], axis=-1)
    el = (xt @ we + be).astype(jnp.float32).reshape(n, N_GROUPS, EXPERTS_PER_GROUP)
    el_grp = jnp.take_along_axis(el, grp[:, None, None], axis=1)[:, 0]
    top_logit, top_e = lax.top_k(el_grp, TOP_K)
    gate = pg_top * jax.nn.softmax(top_logit, axis=-1)
    expert = (grp[:, None] * EXPERTS_PER_GROUP + top_e).reshape(-1)
    n_slots = n * TOP_K
    order = jnp.argsort(expert)
    sorted_e = expert[order]
    tok = order // TOP_K
    sizes = jnp.bincount(expert, length=N_EXPERTS)
    padded = (sizes + MOE_BLOCK - 1) // MOE_BLOCK * MOE_BLOCK
    pad_end = jnp.cumsum(padded)
    pad_start = pad_end - padded
    seg_start = jnp.cumsum(sizes) - sizes
    dest = pad_start[sorted_e] + jnp.arange(n_slots) - seg_start[sorted_e]
    padded_rows = (n_slots + N_EXPERTS * (MOE_BLOCK - 1) + MOE_BLOCK - 1) // MOE_BLOCK * MOE_BLOCK
    n_blocks = padded_rows // MOE_BLOCK
    buf = jnp.zeros((padded_rows, d), xt.dtype).at[dest].set(xt[tok])
    block_e = jnp.minimum(jnp.searchsorted(pad_end, jnp.arange(n_blocks) * MOE_BLOCK, side='right'), N_EXPERTS - 1)

    def expert_block(args):
        xb, e = args
        return (jax.nn.silu(xb @ w1[e]) * (xb @ w3[e])) @ w2[e]

    ys = lax.map(expert_block, (buf.reshape(n_blocks, MOE_BLOCK, d), block_e)).reshape(padded_rows, d)[dest]
    ys = ys * gate.reshape(-1)[order][:, None].astype(ys.dtype)
    return jnp.zeros_like(xt).at[tok].add(ys).reshape(bsz, t, d)


def setup_inputs(seed: int = 0) -> dict:
    key = jax.random.key(seed)
    ks = jax.random.split(key, 22)
    f32 = jnp.float32
    nrm = lambda k_, shape, s: jax.random.normal(k_, shape, f32) * s
    x = nrm(ks[0], (BATCH, SEQ, D_MODEL), 1.0)
    c = nrm(ks[1], (BATCH, D_MODEL), 1.0)
    norm_mix_w = 1.0 + nrm(ks[2], (DEPTH, D_MODEL), 0.02)
    norm_ffn_w = 1.0 + nrm(ks[3], (DEPTH, D_MODEL), 0.02)
    w_ada = nrm(ks[4], (DEPTH, D_MODEL, 6 * D_MODEL), 0.5 * D_MODEL ** -0.5)
    b_ada = nrm(ks[5], (DEPTH, 6 * D_MODEL), 0.02)
    w_in = nrm(ks[6], (DEPTH, D_MODEL, D_IN), D_MODEL ** -0.5)
    conv_a_w = nrm(ks[7], (DEPTH, CONV_K, CONV_W), CONV_K ** -0.5)
    na_rpb = nrm(ks[8], (DEPTH, NA_HEADS, 2 * NA_KH - 1, 2 * NA_KW - 1), 0.1)
    gdn_conv_w = nrm(ks[9], (DEPTH, GDN_CONV_K, GDN_QKV), GDN_CONV_K ** -0.5)
    gdn_a_log = jnp.log(jax.random.uniform(ks[10], (DEPTH, 2, GDN_HEADS), f32, 1.0, 16.0))
    dt = jnp.exp(jax.random.uniform(ks[11], (DEPTH, 2, GDN_HEADS), f32, math.log(1e-3), math.log(1e-1)))
    gdn_dt_bias = dt + jnp.log(-jnp.expm1(-dt))
    gdn_norm_w = 1.0 + nrm(ks[12], (DEPTH, GDN_DV), 0.02)
    w_out = nrm(ks[13], (DEPTH, D_MIX, D_MODEL), D_MIX ** -0.5)
    router_group_w = nrm(ks[14], (DEPTH, D_MODEL, N_GROUPS), D_MODEL ** -0.5)
    router_group_b = nrm(ks[15], (DEPTH, N_GROUPS), 0.01)
    router_expert_w = nrm(ks[16], (DEPTH, D_MODEL, N_EXPERTS), D_MODEL ** -0.5)
    router_expert_b = nrm(ks[17], (DEPTH, N_EXPERTS), 0.01)
    expert_w1 = nrm(ks[18], (DEPTH, N_EXPERTS, D_MODEL, D_EXPERT), D_MODEL ** -0.5)
    expert_w3 = nrm(ks[19], (DEPTH, N_EXPERTS, D_MODEL, D_EXPERT), D_MODEL ** -0.5)
    expert_w2 = nrm(ks[20], (DEPTH, N_EXPERTS, D_EXPERT, D_MODEL), D_EXPERT ** -0.5)
    final_norm_w = 1.0 + nrm(ks[21], (D_MODEL,), 0.02)
    return {"x": x, "c": c, "norm_mix_w": norm_mix_w, "norm_ffn_w": norm_ffn_w, "w_ada": w_ada, "b_ada": b_ada,
            "w_in": w_in, "conv_a_w": conv_a_w, "na_rpb": na_rpb, "gdn_conv_w": gdn_conv_w,
            "gdn_a_log": gdn_a_log, "gdn_dt_bias": gdn_dt_bias, "gdn_norm_w": gdn_norm_w, "w_out": w_out,
            "router_group_w": router_group_w, "router_group_b": router_group_b,
            "router_expert_w": router_expert_w, "router_expert_b": router_expert_b,
            "expert_w1": expert_w1, "expert_w3": expert_w3, "expert_w2": expert_w2, "final_norm_w": final_norm_w}


def reference(x, c, norm_mix_w, norm_ffn_w, w_ada, b_ada, w_in, conv_a_w, na_rpb, gdn_conv_w, gdn_a_log,
              gdn_dt_bias, gdn_norm_w, w_out, router_group_w, router_group_b, router_expert_w, router_expert_b,
              expert_w1, expert_w3, expert_w2, final_norm_w):
    bsz, t, _ = x.shape
    cond = jax.nn.silu(c)
    split_at = np.cumsum(IN_SPLITS)[:-1].tolist()
    for l in range(DEPTH):
        mod = cond @ w_ada[l] + b_ada[l]
        shift1, scale1, gate1, shift2, scale2, gate2 = [m[:, None, :] for m in jnp.split(mod, 6, axis=-1)]
        hmix = _rmsnorm(x, norm_mix_w[l]) * (1.0 + scale1) + shift1
        proj = hmix @ w_in[l]
        cb, cc, cx, nq, nk, nv, gqkv, gz, ga, gb = jnp.split(proj, split_at, axis=-1)
        y_conv = cb * _dwconv(cc * cx, conv_a_w[l])
        y_na = _neighbourhood_attention(nq.reshape(bsz, t, NA_HEADS, NA_DH), nk.reshape(bsz, t, NA_HEADS, NA_DH),
                                        nv.reshape(bsz, t, NA_HEADS, NA_DH), na_rpb[l])
        y_gdn = _gdn_mixer(gqkv, gz, ga, gb, gdn_conv_w[l], gdn_a_log[l], gdn_dt_bias[l], gdn_norm_w[l])
        mixed = jnp.concatenate([y_conv, y_na, y_gdn], axis=-1) @ w_out[l]
        x = x + gate1 * mixed
        hffn = _rmsnorm(x, norm_ffn_w[l]) * (1.0 + scale2) + shift2
        y_moe = _hier_moe(hffn, router_group_w[l], router_group_b[l], router_expert_w[l], router_expert_b[l],
                          expert_w1[l], expert_w3[l], expert_w2[l])
        x = x + gate2 * y_moe
    return _rmsnorm(x, final_norm_w)
```

```python
import numpy as np
import concourse.bass as bass
import concourse.mybir as mybir
from concourse.bass_utils import run_bass_kernel_spmd

F32 = mybir.dt.float32
BF16 = mybir.dt.bfloat16
AF = mybir.ActivationFunctionType
ALU = mybir.AluOpType
AX = mybir.AxisListType

ENGS = ["tensor", "vector", "scalar", "gpsimd", "sync"]


class V:
    __slots__ = ("ap", "keys")

    def __init__(self, ap, keys):
        if ap is not None and type(ap).__name__.endswith("TensorHandle"):
            ap = ap[:]
        self.ap = ap
        self.keys = tuple(keys)

    def __getitem__(self, idx):
        return V(self.ap[idx], self.keys)

    def k(self, *keys):
        return V(self.ap, keys)


class Prog:
    def __init__(self, nc, n_dma_sems=24):
        self.nc = nc
        self.q = {e: [] for e in ENGS}
        self.cnt = {e: 0 for e in ENGS}
        self.sem = {e: nc.alloc_semaphore("c_" + e) for e in ENGS}
        self.dsem = [nc.alloc_semaphore("d_%d" % i) for i in range(n_dma_sems)]
        self.duse = [0] * n_dma_sems
        self.dnext = 0
        self.waited = {e: {} for e in ENGS}
        self.last_w = {}
        self.readers = {}
        self.semid = {}
        for e in ENGS:
            self.semid[id(self.sem[e])] = e
        self.nops = 0

    def _collect(self, eng, reads, writes):
        toks = []
        for r in reads:
            for key in r.keys:
                t = self.last_w.get(key)
                if t is not None:
                    toks.append((t, True))
                if isinstance(key, str) and key.startswith("ps"):
                    for t in self.readers.get(key, ()):
                        toks.append((t, False))
        for w in writes:
            for key in w.keys:
                t = self.last_w.get(key)
                if t is not None:
                    toks.append((t, True))
                for t in self.readers.get(key, ()):
                    toks.append((t, False))
        waits = {}
        mysem = self.sem[eng]
        for (sem, val), hard in toks:
            if sem is mysem:
                if eng == "tensor" or eng == "sync" or not hard:
                    continue
            sid = id(sem)
            if self.waited[eng].get(sid, 0) >= val:
                continue
            if waits.get(sid, (None, 0))[1] < val:
                waits[sid] = (sem, val)
        for sid, (sem, val) in waits.items():
            self.waited[eng][sid] = val
        return list(waits.values())

    def _commit(self, tok, reads, writes):
        for r in reads:
            for key in r.keys:
                self.readers.setdefault(key, []).append(tok)
        for w in writes:
            for key in w.keys:
                self.last_w[key] = tok
                self.readers[key] = []

    def op(self, eng, fn, reads=(), writes=()):
        waits = self._collect(eng, reads, writes)
        self.cnt[eng] += 1
        tok = (self.sem[eng], self.cnt[eng])
        self.q[eng].append((fn, waits, (self.sem[eng], 1)))
        self._commit(tok, reads, writes)
        self.nops += 1

    def dma(self, eng, out, in_, **kw):
        i = self.dnext
        self.dnext = (self.dnext + 1) % len(self.dsem)
        sem = self.dsem[i]
        waits = self._collect(eng, [in_], [out])
        if self.duse[i] > 0:
            sid = id(sem)
            val = 16 * self.duse[i]
            if self.waited[eng].get(sid, 0) < val:
                waits = [w for w in waits if w[0] is not sem] + [(sem, val)]
                self.waited[eng][sid] = val
        self.duse[i] += 1
        tok = (sem, 16 * self.duse[i])
        oap, iap = out.ap, in_.ap
        self.q[eng].append((lambda e: e.dma_start(out=oap, in_=iap, **kw), waits, (sem, 16)))
        self._commit(tok, [in_], [out])
        self.nops += 1
        return tok

    def finish(self, eng="sync"):
        waits = []
        for i, sem in enumerate(self.dsem):
            if self.duse[i] > 0:
                waits.append((sem, 16 * self.duse[i]))
        self.q[eng].append((None, waits, None))

    def wait_all(self, eng, views):
        waits = self._collect(eng, views, [])
        self.q[eng].append((None, waits, None))

    def emit(self, block):
        nc = self.nc
        for e in ENGS:
            items = self.q[e]
            if not items:
                continue

            def body(engine, items=items):
                for fn, waits, inc in items:
                    for sem, val in waits:
                        engine.wait_ge(sem, val)
                    if fn is None:
                        continue
                    ins = fn(engine)
                    if inc is not None:
                        ins.then_inc(inc[0], inc[1])
            getattr(block, e)(body)
        self.q = {e: [] for e in ENGS}

    def mm(self, out, lhsT, rhs, start=True, stop=True):
        o, l, r = out.ap, lhsT.ap, rhs.ap
        self.op("tensor", lambda e: e.matmul(o, l, r, start=start, stop=stop), [lhsT, rhs], [out])

    def tr(self, out, in_, ident):
        o, i, d = out.ap, in_.ap, ident.ap
        self.op("tensor", lambda e: e.transpose(o, i, d), [in_, ident], [out])

    def act(self, out, in_, func, bias=None, scale=None, accum_out=None, eng="scalar"):
        o, i = out.ap, in_.ap
        kw = {}
        rd = [in_]
        wr = [out]
        if bias is not None:
            if isinstance(bias, V):
                kw["bias"] = bias.ap
                rd.append(bias)
            else:
                kw["bias"] = bias
        if scale is not None:
            if isinstance(scale, V):
                kw["scale"] = scale.ap
                rd.append(scale)
            else:
                kw["scale"] = scale
        if accum_out is not None:
            kw["accum_out"] = accum_out.ap
            wr.append(accum_out)
        self.op("scalar", lambda e: e.activation(o, i, func, **kw), rd, wr)

    def tt(self, out, in0, in1, op, eng="vector"):
        o, a, b = out.ap, in0.ap, in1.ap
        self.op(eng, lambda e: e.tensor_tensor(o, a, b, op), [in0, in1], [out])

    def ts(self, out, in0, s1, s2, op0, op1=None, eng="vector", accum_out=None):
        o, a = out.ap, in0.ap
        rd = [in0]
        wr = [out]
        if isinstance(s1, V):
            rd.append(s1)
            s1 = s1.ap
        if isinstance(s2, V):
            rd.append(s2)
            s2 = s2.ap
        kw = {}
        if accum_out is not None:
            kw["accum_out"] = accum_out.ap
            wr.append(accum_out)
        if op1 is None:
            self.op(eng, lambda e: e.tensor_scalar(o, a, s1, s2, op0, **kw), rd, wr)
        else:
            self.op(eng, lambda e: e.tensor_scalar(o, a, s1, s2, op0, op1, **kw), rd, wr)

    def stt(self, out, in0, scalar, in1, op0, op1, eng="vector"):
        o, a, b = out.ap, in0.ap, in1.ap
        rd = [in0, in1]
        if isinstance(scalar, V):
            rd.append(scalar)
            scalar = scalar.ap
        self.op(eng, lambda e: e.scalar_tensor_tensor(o, a, scalar, b, op0, op1), rd, [out])

    def copy(self, out, in_, eng="vector"):
        o, i = out.ap, in_.ap
        if eng == "scalar":
            self.op(eng, lambda e: e.copy(o, i), [in_], [out])
        else:
            self.op(eng, lambda e: e.tensor_copy(o, i), [in_], [out])

    def memset(self, out, val, eng="vector"):
        o = out.ap
        self.op(eng, lambda e: e.memset(o, val), [], [out])

    def recip(self, out, in_):
        o, i = out.ap, in_.ap
        self.op("vector", lambda e: e.reciprocal(o, i), [in_], [out])

    def reduce(self, out, in_, op, axis=AX.X):
        o, i = out.ap, in_.ap
        self.op("vector", lambda e: e.tensor_reduce(o, i, axis, op), [in_], [out])


EPS = 1e-6
NTOK = 2048
TB = 512


def emit_mod(p, nc, pools, cT, wada_d, bada, ncol_tiles, modps, wst, cond, mod_sb):
    p.act(cond, cT, AF.Silu)
    nchunk = (ncol_tiles * 128) // 512
    for ch in range(nchunk):
        buf = wst[ch % 2]
        p.dma("sync" if ch % 2 == 0 else "gpsimd", buf,
              V(wada_d[:, ch * 512:(ch + 1) * 512].rearrange("(c p) n -> p c n", p=128), []))
        for jj in range(4):
            j = ch * 4 + jj
            for kc in range(8):
                p.mm(modps[:, j:j + 1], buf[:, kc, jj * 128:(jj + 1) * 128], cond[:, kc:kc + 1],
                     start=(kc == 0), stop=(kc == 7))
    p.tt(mod_sb, modps[:, 0:ncol_tiles], bada, ALU.add)


def emit_hmix_block(p, xT, blk, ones, sq, ss, rs, rstd_b, tmp, A1, B1, hT, hF=None):
    sl = slice(blk * TB, (blk + 1) * TB)
    for kc in range(8):
        s = sq[kc % 2]
        p.act(s, xT[:, kc, sl], AF.Square)
        p.mm(ss, ones, s, start=(kc == 0), stop=(kc == 7))
    p.act(rs, ss, AF.Sqrt, scale=1.0 / 1024.0, bias=EPSV[0])
    p.recip(rstd_b, rs)
    for kc in range(8):
        t = tmp[kc % 2]
        p.stt(t, xT[:, kc, sl], A1[:, kc:kc + 1], rstd_b, ALU.mult, ALU.mult)
        if hF is not None:
            p.ts(hF[:, kc, :], t, B1[:, kc:kc + 1], None, ALU.add)
            p.copy(hT[:, kc, :], hF[:, kc, :], eng="scalar")
        else:
            p.act(hT[:, kc, :], t, AF.Identity, bias=B1[:, kc:kc + 1])


EPSV = [None]


def build_A():
    nc = bass.Bass("TRN2", target_bir_lowering=False)
    dt = nc.dram_tensor
    xT_d = dt("xT", [1024, NTOK], F32, kind="ExternalInput").ap()
    cT_d = dt("cT", [128, 8], F32, kind="ExternalInput").ap()
    wada_d = dt("wada", [1024, 2048], F32, kind="ExternalInput").ap()
    bada_d = dt("bada", [128, 16], F32, kind="ExternalInput").ap()
    nw_d = dt("nw", [128, 8], F32, kind="ExternalInput").ap()
    win_d = dt("win", [1024, 3600], F32, kind="ExternalInput").ap()
    out_d = dt("projT", [3600, NTOK], F32, kind="ExternalOutput").ap()
    from contextlib import ExitStack
    with ExitStack() as es:
        def sb(name, shape, dtype):
            return es.enter_context(nc.sbuf_tensor(name, shape, dtype))
        xT = V(sb("xTs", [128, 8, NTOK], F32), ["xT"])
        wst_t = sb("wst", [128, 2, 8, 512], F32)
        wst = [V(wst_t[:, i], ["wst%d" % i]) for i in range(2)]
        winb = sb("winb", [128, 8, 3600], BF16)
        hT_t = sb("hT", [128, 2, 8, TB], BF16)
        sq_t = sb("sq", [128, 2, TB], F32)
        tmp_t = sb("tmp", [128, 2, TB], F32)
        rs = V(sb("rs", [128, TB], F32), ["rs"])
        rstd_b = V(sb("rstd", [128, TB], F32), ["rstd"])
        stage_t = sb("stage", [128, 4, TB], F32)
        small = sb("small", [128, 64], F32)
        ones = V(sb("ones", [128, 128], F32), ["ones"])
        epsv = V(sb("epsv", [128, 1], F32), ["epsv"])
        EPSV[0] = epsv
        ps = es.enter_context(nc.psum_tensor("ps", [128, 8, 512], F32))
        block = es.enter_context(nc.Block())
        p = Prog(nc)
        cT = V(small[:, 0:8], ["cT"]); cond = V(small[:, 8:16], ["cond"])
        bada = V(small[:, 16:32], ["bada"]); mod_sb = V(small[:, 32:48], ["mod"])
        nw = V(small[:, 48:56], ["nw"]); A1 = V(small[:, 56:64], ["A1"])
        modps = V(ps[:, 0, 0:16], ["ps0"])
        ss = V(ps[:, 1, :], ["ps1"])
        p.memset(ones, 1.0)
        p.memset(epsv, EPS)
        p.dma("sync", cT, V(cT_d, []))
        p.dma("sync", bada, V(bada_d, []))
        p.dma("sync", nw, V(nw_d, []))
        for kc in range(8):
            p.dma("gpsimd" if kc % 2 else "sync", xT[:, kc, :], V(xT_d[kc * 128:(kc + 1) * 128, :], []))
        emit_mod(p, nc, None, cT, wada_d, bada, 16, modps, wst, cond, mod_sb)
        B1 = mod_sb[:, 0:8]
        p.stt(A1, mod_sb[:, 8:16], 1.0, nw, ALU.add, ALU.mult)
        ci = 0
        for c0 in range(0, 3600, 512):
            cw = min(512, 3600 - c0)
            buf = wst[ci % 2]
            p.dma("sync" if ci % 2 == 0 else "gpsimd", buf[:, :, 0:cw],
                  V(win_d[:, c0:c0 + cw].rearrange("(c p) n -> p c n", p=128), []))
            for kc in range(8):
                dst = V(winb[:, kc, c0:c0 + cw], ["winb%d" % ci])
                p.copy(dst, buf[:, kc, 0:cw], eng=("gpsimd" if kc % 2 else "vector"))
            ci += 1
        nev = 0
        for blk in range(NTOK // TB):
            hT = V(hT_t[:, blk % 2], ["hT%d" % (blk % 2)])
            sq = [V(sq_t[:, i], ["sq%d" % i]) for i in range(2)]
            tmp = [V(tmp_t[:, i], ["tmp%d" % i]) for i in range(2)]
            emit_hmix_block(p, xT, blk, ones, sq, ss, rs, rstd_b, tmp, A1, B1, hT)
            for j in range(29):
                rows = min(128, 3600 - j * 128)
                bank = 2 + (nev % 6)
                pj = V(ps[0:rows, bank, :], ["ps%d" % bank])
                ci = (j * 128) // 512
                for kc in range(8):
                    p.mm(pj, V(winb[:, kc, j * 128:j * 128 + rows], ["winb%d" % ci]), hT[:, kc, :],
                         start=(kc == 0), stop=(kc == 7))
                st = V(stage_t[0:rows, nev % 4, :], ["stage%d" % (nev % 4)])
                if nev % 2 == 0:
                    p.copy(st, pj, eng="vector")
                else:
                    p.copy(st, pj, eng="scalar")
                p.dma("sync" if nev % 2 == 0 else "gpsimd",
                      V(out_d[j * 128:j * 128 + rows, blk * TB:(blk + 1) * TB], []), st)
                nev += 1
        p.finish("sync")
        p.emit(block)
    return nc


NSPEC = 8
SPEC_ROWS = [0, 1, 2, 3, 28, 29, 30, 31]


def build_B(parts=(1, 1, 1)):
    nc = bass.Bass("TRN2", target_bir_lowering=False)
    dt = nc.dram_tensor
    convin_d = dt("convin", [768, NTOK + 2], F32, kind="ExternalInput").ap()
    cw_d = dt("cw", [128, 2, 3], F32, kind="ExternalInput").ap()
    gqkv_d = dt("gqkv", [1536, NTOK + 2], F32, kind="ExternalInput").ap()
    gw_d = dt("gw", [128, 12, 3], F32, kind="ExternalInput").ap()
    naq_d = dt("naq", [256, NTOK], F32, kind="ExternalInput").ap()
    nak_d = dt("nak", [256, NTOK], F32, kind="ExternalInput").ap()
    nave_d = dt("nave", [128, 16, 256], F32, kind="ExternalInput").ap()
    navo_d = dt("navo", [128, 16, 256], F32, kind="ExternalInput").ap()
    kspec_d = dt("kspec", [256, NSPEC * 512], F32, kind="ExternalInput").ap()
    vspec_d = dt("vspec", [128, NSPEC * 4, 256], F32, kind="ExternalInput").ap()
    rpbg_d = dt("rpbg", [128, 9, 1024], F32, kind="ExternalInput").ap()
    maskf_d = dt("maskf", [128, 1024], F32, kind="ExternalInput").ap()
    yconv_d = dt("yconvT", [256, NTOK], F32, kind="ExternalOutput").ap()
    gqkvn_d = dt("gqkvn", [1536, NTOK], F32, kind="ExternalOutput").ap()
    yna_d = dt("yna", [NTOK, 256], F32, kind="ExternalOutput").ap()
    from contextlib import ExitStack
    with ExitStack() as es:
        def sb(name, shape, dtype):
            return es.enter_context(nc.sbuf_tensor(name, shape, dtype))
        NST = 3
        stg_t = sb("stg", [128, NST, 4096], F32)
        u_t = sb("u", [128, NTOK + 2], F32)
        acc_t = sb("acc", [128, NTOK], F32)
        sil_t = sb("sil", [128, 2, NTOK], F32)
        sq_t = sb("sq", [128, 2, TB], F32)
        rs_t = sb("rs", [128, 2, TB], F32)
        ones_t = sb("ones", [128, 128], F32)
        wsm = sb("wsm", [128, 64], F32)
        qb_t = sb("qb", [128, 2, NTOK], BF16)
        kb_t = sb("kb", [128, 2, NTOK], BF16)
        ksp_t = sb("ksp", [128, 2, NSPEC * 512], BF16)
        ve_t = sb("ve", [128, 16, 4, 65], BF16)
        vo_t = sb("vo", [128, 16, 4, 65], BF16)
        vs_t = sb("vs", [128, NSPEC * 4, 4, 65], BF16)
        bias_t = sb("biass", [128, 9, 1024], F32)
        maskf_t = sb("maskfs", [128, 1024], F32)
        sc_t = sb("sc", [128, 2, 512], F32)
        pT_t = sb("pT", [128, 2, 512], BF16)
        pos_t = sb("pos", [64, 2, 260], F32)
        rden_t = sb("rden", [64, 2, 4], F32)
        yst_t = sb("yst", [64, 2, 256], F32)
        ps = es.enter_context(nc.psum_tensor("ps", [128, 8, 512], F32))
        block = es.enter_context(nc.Block())
        p = Prog(nc)
        stg_i = [0]

        def stage():
            i = stg_i[0] % NST
            stg_i[0] += 1
            return V(stg_t[:, i], ["stg%d" % i])
        dq_i = [0]

        def dq():
            dq_i[0] += 1
            return "sync" if dq_i[0] % 2 else "gpsimd"
        ones = V(ones_t, ["ones"])
        p.memset(ones, 1.0)
        cw = V(wsm[:, 0:6], ["cw"])
        gw = V(wsm[:, 6:42], ["gw"])
        epsv = V(wsm[:, 42:43], ["epsv"])
        p.memset(epsv, EPS)
        p.dma("sync", cw, V(cw_d.rearrange("p a b -> p (a b)"), []))
        p.dma("sync", gw, V(gw_d.rearrange("p a b -> p (a b)"), []))
        u = V(u_t, ["u"]); acc = V(acc_t, ["acc"])

        def conv3(src, wv, base):
            p.ts(acc, src[:, 0:NTOK], wv[:, base:base + 1], None, ALU.mult)
            p.stt(acc, src[:, 1:NTOK + 1], wv[:, base + 1:base + 2], acc, ALU.mult, ALU.add)
            p.stt(acc, src[:, 2:NTOK + 2], wv[:, base + 2:base + 3], acc, ALU.mult, ALU.add)

        for ct in (range(2) if parts[0] else []):
            bufs = []
            for g in range(3):
                st = stage()
                p.dma(dq(), st[:, 0:NTOK + 2], V(convin_d[g * 256 + ct * 128:g * 256 + (ct + 1) * 128, :], []))
                bufs.append(st)
            cb, cc, cx = bufs
            p.tt(u, cc[:, 0:NTOK + 2], cx[:, 0:NTOK + 2], ALU.mult)
            conv3(u, cw, ct * 3)
            yo = V(sil_t[:, ct], ["sil%d" % ct])
            p.tt(yo, acc, cb[:, 1:NTOK + 1], ALU.mult)
            p.dma(dq(), V(yconv_d[ct * 128:(ct + 1) * 128, :], []), yo)

        ssb = 0
        for ct in (range(12) if parts[1] else []):
            st = stage()
            p.dma(dq(), st[:, 0:NTOK + 2], V(gqkv_d[ct * 128:(ct + 1) * 128, :], []))
            conv3(st, gw, ct * 3)
            so = V(sil_t[:, ct % 2], ["sil%d" % (ct % 2)])
            p.act(so, acc, AF.Silu)
            if ct < 8:
                qscale = (128.0 ** -0.5) if ct < 4 else 1.0
                for blk in range(NTOK // TB):
                    sl = slice(blk * TB, (blk + 1) * TB)
                    sq = V(sq_t[:, ssb % 2], ["sq%d" % (ssb % 2)])
                    rs = V(rs_t[:, ssb % 2], ["rs%d" % (ssb % 2)])
                    bank = ssb % 2
                    ss = V(ps[:, bank, :], ["ps%d" % bank])
                    ssb += 1
                    p.act(sq, so[:, sl], AF.Square)
                    p.mm(ss, ones, sq)
                    p.act(rs, ss, AF.Sqrt, bias=epsv)
                    p.recip(rs, rs)
                    p.stt(so[:, sl], so[:, sl], qscale, rs, ALU.mult, ALU.mult)
            p.dma(dq(), V(gqkvn_d[ct * 128:(ct + 1) * 128, :], []), so)

        def load_cast(dst_views, src_aps, width):
            for dv, sa in zip(dst_views, src_aps):
                st = stage()
                p.dma(dq(), st[:, 0:width], V(sa, []))
                p.copy(dv, st[:, 0:width], eng="gpsimd")
        qb = V(qb_t, ["qb"]); kb = V(kb_t, ["kb"]); ksp = V(ksp_t, ["ksp"])
        load_cast([qb[:, i, :] for i in range(2)], [naq_d[i * 128:(i + 1) * 128, :] for i in range(2)], NTOK)
        load_cast([kb[:, i, :] for i in range(2)], [nak_d[i * 128:(i + 1) * 128, :] for i in range(2)], NTOK)
        load_cast([ksp[:, i, :] for i in range(2)], [kspec_d[i * 128:(i + 1) * 128, :] for i in range(2)], NSPEC * 512)
        ve = V(ve_t, ["ve"]); vo = V(vo_t, ["vo"]); vs = V(vs_t, ["vs"])
        for (dst, src_d, nt) in ((ve, nave_d, 16), (vo, navo_d, 16), (vs, vspec_d, 32)):
            p.memset(dst[:, :, :, 64:65], 1.0, eng="gpsimd")
            for c0 in range(0, nt, 16):
                st = stage()
                p.dma(dq(), st[:, 0:16 * 256], V(src_d[:, c0:c0 + 16, :].rearrange("p a b -> p (a b)"), []))
                p.copy(dst[:, c0:c0 + 16, :, 0:64],
                       V(st.ap[:, 0:16 * 256].rearrange("p (a h d) -> p a h d", a=16, h=4), st.keys), eng="vector")
        bias = V(bias_t, ["bias"]); maskf = V(maskf_t, ["maskf"])
        p.dma("sync", maskf, V(maskf_d, []))
        for i in range(9):
            p.dma(dq(), bias[:, i, :], V(rpbg_d[:, i, :], []))
        for i in range(9):
            p.stt(bias[:, i, :], bias[:, i, :], 8.0, maskf, ALU.mult, ALU.add)
        for r in (range(32) if parts[2] else []):
            par = r % 2
            if r in SPEC_ROWS:
                s = SPEC_ROWS.index(r)
                bi = 1 + s

                def ktile(tile, off, t, s=s):
                    return ksp[off:off + 64, tile, s * 512 + t * 128:s * 512 + (t + 1) * 128]

                def vtile(t, h, s=s):
                    return vs[:, s * 4 + t, h, :]
            else:
                bi = 0
                lr = r - 4

                def ktile(tile, off, t, lr=lr):
                    return kb[off:off + 64, tile, (lr + 2 * t) * 64:(lr + 2 * t + 2) * 64]
                if lr % 2 == 0:
                    def vtile(t, h, lr=lr):
                        return ve[:, lr // 2 + t, h, :]
                else:
                    def vtile(t, h, lr=lr):
                        return vo[:, (lr - 1) // 2 + t, h, :]
            pob = 6 + par
            po = V(ps[0:64, pob, 0:260], ["ps%d" % pob])
            for hl in range(2):
                off = 64 * hl
                bank = 2 + 2 * par + hl
                psc = V(ps[:, bank, :], ["ps%d" % bank])
                for j in range(2):
                    for t in range(4):
                        p.mm(psc[:, (j * 4 + t) * 64:(j * 4 + t + 1) * 64], ktile(j, off, t),
                             qb[off:off + 64, j, r * 64:(r + 1) * 64])
                sc = V(sc_t[:, hl], ["sc%d" % hl])
                bview = V(bias.ap[:, bi, :].rearrange("p (h x) -> p h x", h=4)[:, hl::2, :], bias.keys)
                p.tt(V(sc.ap.rearrange("p (h x) -> p h x", h=2), sc.keys),
                     V(psc.ap.rearrange("p (h x) -> p h x", h=2), psc.keys), bview, ALU.add)
                pT = V(pT_t[:, hl], ["pT%d" % hl])
                p.act(pT, sc, AF.Exp, scale=0.125)
                for j in range(2):
                    h = 2 * j + hl
                    for t in range(4):
                        p.mm(po[:, h * 65:(h + 1) * 65], pT[:, (j * 4 + t) * 64:(j * 4 + t + 1) * 64], vtile(t, h),
                             start=(t == 0), stop=(t == 3))
            pos = V(pos_t[:, par], ["pos%d" % par])
            p.copy(pos, po, eng="scalar")
            rden = V(rden_t[:, par], ["rden%d" % par])
            posv = V(pos.ap.rearrange("p (h d) -> p h d", h=4), pos.keys)
            p.recip(rden, V(posv.ap[:, :, 64:65].rearrange("p h o -> p (h o)"), pos.keys))
            yst = V(yst_t[:, par], ["yst%d" % par])
            for h in range(4):
                p.ts(yst[:, h * 64:(h + 1) * 64], posv[:, h, 0:64], rden[:, h:h + 1], None, ALU.mult, eng="gpsimd")
            p.dma(dq(), V(yna_d[r * 64:(r + 1) * 64, :], []), yst)
        p.finish("sync")
        p.emit(block)
    return nc


def _pad_cols(a, lo, hi, n):
    out = np.zeros((a.shape[0], hi - lo), a.dtype)
    s0, s1 = max(lo, 0), min(hi, n)
    out[:, s0 - lo:s1 - lo] = a[:, s0:s1]
    return out


def _na_tables(rpb):
    kc = np.arange(64)[:, None]; qc = np.arange(64)[None, :]
    cs = np.clip(qc - 8, 0, 48)
    inwin = (kc >= cs) & (kc < cs + 16)
    dc = np.clip(kc - qc + 15, 0, 30)
    mask = np.where(inwin, 0.0, -240000.0).astype(np.float32)
    maskf = np.zeros((128, 4, 4, 64), np.float32)
    maskf[:] = np.concatenate([mask, mask], 0)[:, None, None, :]
    def table(dr0):
        tb = np.zeros((128, 4, 4, 64), np.float32)
        for a in range(2):
            for t in range(4):
                dr = dr0 + 2 * t + a
                tb[a * 64:(a + 1) * 64, :, t, :] = np.transpose(rpb[:, dr][:, dc], (1, 0, 2))
        return tb.reshape(128, 1024)
    return table, maskf.reshape(128, 1024)


def prep_B(projT, P, l):
    table, maskf = _na_tables(P["na_rpb"][l])
    cw = np.ascontiguousarray(P["conv_a_w"][l].T.reshape(2, 128, 3).transpose(1, 0, 2))
    gw = np.ascontiguousarray(P["gdn_conv_w"][l].T.reshape(12, 128, 3).transpose(1, 0, 2))
    ins = []
    for core in range(8):
        b = core // 4; t0 = (core % 4) * NTOK
        pb = projT[:, b * 8192:(b + 1) * 8192]
        vb = pb[1280:1536].T
        v = vb[t0:t0 + NTOK]
        nave = np.ascontiguousarray(v.reshape(16, 128, 256).transpose(1, 0, 2))
        navo = np.zeros((128, 16, 256), np.float32)
        navo[:, :15] = v[64:64 + 15 * 128].reshape(15, 128, 256).transpose(1, 0, 2)
        kspec = np.zeros((256, NSPEC * 512), np.float32)
        vspec = np.zeros((128, NSPEC * 4, 256), np.float32)
        rpbg = np.zeros((128, 9, 1024), np.float32)
        rpbg[:, 0] = table(3)
        row0 = (core % 4) * 32
        for s, r in enumerate(SPEC_ROWS):
            R = row0 + r
            rs = min(max(R - 4, 0), 120)
            kspec[:, s * 512:(s + 1) * 512] = pb[1024:1280, rs * 64:rs * 64 + 512]
            vspec[:, s * 4:(s + 1) * 4] = vb[rs * 64:rs * 64 + 512].reshape(4, 128, 256).transpose(1, 0, 2)
            rpbg[:, 1 + s] = table(rs - R + 7)
        ins.append({
            "convin": _pad_cols(pb[0:768], t0 - 1, t0 + NTOK + 1, 8192),
            "cw": cw, "gw": gw,
            "gqkv": _pad_cols(pb[1536:3072], t0 - 1, t0 + NTOK + 1, 8192),
            "naq": np.ascontiguousarray(pb[768:1024, t0:t0 + NTOK]),
            "nak": np.ascontiguousarray(pb[1024:1280, t0:t0 + NTOK]),
            "nave": nave, "navo": navo, "kspec": kspec, "vspec": vspec,
            "rpbg": rpbg, "maskf": maskf,
        })
    return ins


NSC = 64
C_IDENT, C_ONES, C_TRIF, C_TRIB, C_BLK, C_SELA, C_SELB, C_MSF, C_MSB, C_MIF, C_MIB = range(11)


def gdn_consts():
    i = np.arange(128)
    t = i[:, None]; c = i[None, :]
    same = (t // 64) == (c // 64)
    cs = np.zeros((128, 11, 128), np.float32)
    cs[:, C_IDENT] = np.eye(128)
    cs[:, C_ONES] = 1.0
    cs[:, C_TRIF] = same & (t <= c)
    cs[:, C_TRIB] = same & (t >= c)
    cs[:, C_BLK] = same
    cs[:, C_SELA] = (t < 64) & (c >= 0)
    cs[:, C_SELB] = (t >= 64) & (c >= 0)
    cc = i[:, None]; ss = i[None, :]
    same2 = (cc // 64) == (ss // 64)
    cs[:, C_MSF] = np.where(same2 & (cc > ss), 0.0, 30000.0)
    cs[:, C_MSB] = np.where(same2 & (cc < ss), 0.0, 30000.0)
    sp = i[:, None]; cf = i[None, :]
    cs[:, C_MIF] = np.where(same2 & (cf >= sp), 0.0, -30000.0)
    cs[:, C_MIB] = np.where(same2 & (cf <= sp), 0.0, -30000.0)
    return cs


def build_C(nsc_run=NSC, mode=2):
    nc = bass.Bass("TRN2", target_bir_lowering=False)
    dt = nc.dram_tensor
    T = NSC * 128
    qnT_d = dt("qnT", [128, T], F32, kind="ExternalInput").ap()
    knT_d = dt("knT", [128, T], F32, kind="ExternalInput").ap()
    kn_d = dt("kn", [128, NSC, 128], F32, kind="ExternalInput").ap()
    v_d = dt("v", [128, NSC, 128], F32, kind="ExternalInput").ap()
    ab_d = dt("ab", [128, NSC, 4], F32, kind="ExternalInput").ap()
    par_d = dt("par", [128, 4], F32, kind="ExternalInput").ap()
    cst_d = dt("cst", [128, 11, 128], F32, kind="ExternalInput").ap()
    o_d = dt("o", [T, 128], F32, kind="ExternalOutput").ap()
    from contextlib import ExitStack
    with ExitStack() as es:
        def sb(name, shape, dtype):
            return es.enter_context(nc.sbuf_tensor(name, shape, dtype))
        cst_t = sb("cst_s", [128, 11, 128], F32)
        cstb_t = sb("cstb", [128, 128], BF16)
        stg_t = sb("stg", [128, 2, 2048], F32)
        qnT_t = sb("qnTb", [128, T], BF16)
        knT_t = sb("knTb", [128, T], BF16)
        kn_t = sb("knb", [128, NSC, 128], BF16)
        v_t = sb("vb", [128, NSC, 128], BF16)
        oacc_t = sb("oacc", [128, NSC, 128], F32)
        ab_t = sb("abs", [128, NSC, 4], F32)
        par_t = sb("pars", [128, 8], F32)
        NPS = 12
        pre_t = sb("pre", [128, 2, NPS, NSC], F32)
        S_t = sb("S", [128, 2, 128], F32)
        Sb_t = sb("Sb", [128, 2, 128], BF16)
        vnew_t = sb("vnew", [128, 2, 128], BF16)
        NF = 6; NB = 16
        wf_t = sb("wf", [128, 2, 2, NF, 128], F32)
        wb_t = sb("wb", [128, 2, 2, NB, 128], BF16)
        ps = es.enter_context(nc.psum_tensor("ps", [128, 8, 512], F32))
        block = es.enter_context(nc.Block())
        p = Prog(nc)
        cst = V(cst_t, ["cst"])

        def C(i):
            return cst[:, i, :]
        identb = V(cstb_t, ["cstb"])
        dq_i = [0]

        def dq():
            dq_i[0] += 1
            return "sync" if dq_i[0] % 2 else "gpsimd"
        p.dma("sync", cst, V(cst_d, []))
        p.copy(identb, C(C_IDENT))
        ab = V(ab_t, ["ab"]); par = V(par_t, ["par"])
        p.dma("sync", ab, V(ab_d, []))
        p.dma("sync", par[:, 0:4], V(par_d, []))
        si = 0
        for (dst_t, src, kind) in ((qnT_t, qnT_d, "T"), (knT_t, knT_d, "T"), (kn_t, kn_d, "N"), (v_t, v_d, "N")):
            for c4 in range(4):
                st = V(stg_t[:, si % 2], ["stg%d" % (si % 2)])
                si += 1
                if kind == "T":
                    p.dma(dq(), st, V(src[:, c4 * 2048:(c4 + 1) * 2048], []))
                    p.copy(V(dst_t[:, c4 * 2048:(c4 + 1) * 2048], [dst_t.name if hasattr(dst_t, "name") else id(dst_t)]), st,
                           eng=("gpsimd" if c4 % 2 else "vector"))
                else:
                    p.dma(dq(), st, V(src[:, c4 * 16:(c4 + 1) * 16, :].rearrange("p a b -> p (a b)"), []))
                    p.copy(V(dst_t[:, c4 * 16:(c4 + 1) * 16, :].rearrange("p a b -> p (a b)"), [id(dst_t)]), st,
                           eng=("gpsimd" if c4 % 2 else "vector"))
        qnT = V(qnT_t, [qnT_t.name if hasattr(qnT_t, "name") else id(qnT_t)])
        knT = V(knT_t, [knT_t.name if hasattr(knT_t, "name") else id(knT_t)])
        kn = V(kn_t, [id(kn_t)]); vv = V(v_t, [id(v_t)])
        p.act(par[:, 4:6], par[:, 0:2], AF.Exp)
        p.ts(par[:, 6:8], par[:, 4:6], -1.0, None, ALU.mult)
        pre = V(pre_t, ["pre"])
        mA = V(cst.ap[:, C_SELA, 0:1], cst.keys)
        mB = V(cst.ap[:, C_SELB, 0:1], cst.keys)
        for d in range(2):
            def S_(i, d=d):
                return V(pre_t[:, d, i, :], ["pre%d_%d" % (d, i)])
            a_v = V(ab_t[:, :, d], ["ab"]); b_v = V(ab_t[:, :, 2 + d], ["ab"])
            p.ts(S_(11), a_v, par[:, 2 + d:3 + d], None, ALU.add)
            p.act(S_(11), S_(11), AF.Exp)
            p.act(S_(0), S_(11), AF.Ln, bias=1.0)
            p.ts(S_(0), S_(0), par[:, 6 + d:7 + d], None, ALU.mult)
            p.act(S_(1), b_v, AF.Sigmoid)
            tri = C(C_TRIF if d == 0 else C_TRIB)
            pb = V(ps[:, 0, :], ["ps0"])
            p.mm(pb[:, 0:NSC], tri, S_(0))
            p.mm(pb[:, 64:64 + NSC], C(C_BLK), S_(0))
            p.mm(pb[:, 128:128 + NSC], C(C_SELA), S_(0))
            p.mm(pb[:, 192:192 + NSC], C(C_SELB), S_(0))
            p.copy(S_(2), pb[:, 0:NSC])
            p.copy(S_(3), pb[:, 64:64 + NSC])
            p.act(S_(9), pb[:, 128:128 + NSC], AF.Exp)
            p.act(S_(10), pb[:, 192:192 + NSC], AF.Exp)
            p.act(S_(4), S_(2), AF.Exp)
            p.tt(S_(5), S_(1), S_(4), ALU.mult)
            p.tt(S_(11), S_(3), S_(2), ALU.subtract)
            p.act(S_(11), S_(11), AF.Exp)
            p.ts(S_(6), S_(11), mA, None, ALU.mult)
            p.ts(S_(7), S_(11), mB, None, ALU.mult)
            p.ts(S_(8), S_(1), -1.0, None, ALU.mult)
        for d in range(2):
            p.memset(V(S_t[:, d], ["S%d" % d]), 0.0)
            p.memset(V(Sb_t[:, d], ["Sb%d" % d]), 0.0)
            p.memset(V(vnew_t[:, d], ["vn%d" % d]), 0.0)
        ev = [0]

        def evac(out, in_):
            ev[0] += 1
            p.copy(out, in_, eng=("scalar" if ev[0] % 2 else "vector"))

        def do_sc(d, sc, step):
            par_ = step % 2
            tok = slice(sc * 128, (sc + 1) * 128)

            def sc_(i):
                return V(pre_t[:, d, i, sc:sc + 1], ["pre%d_%d" % (d, i)])

            def F(i):
                return V(wf_t[:, d, par_, i], ["wf%d%d_%d" % (d, par_, i)])

            def B(i):
                return V(wb_t[:, d, par_, i], ["wb%d%d_%d" % (d, par_, i)])
            b0 = V(ps[:, 4 * d + 0, :], ["ps%d" % (4 * d)])
            b1 = V(ps[:, 4 * d + 1, :], ["ps%d" % (4 * d + 1)])
            b2 = V(ps[:, 4 * d + 2, :], ["ps%d" % (4 * d + 2)])
            b3 = V(ps[:, 4 * d + 3, :], ["ps%d" % (4 * d + 3)])

            def sl(bk, i):
                return bk[:, i * 128:(i + 1) * 128]
            Gdiag = F(0)
            p.ts(Gdiag, C(C_IDENT), sc_(2), None, ALU.mult)
            p.mm(sl(b0, 0), C(C_ONES), Gdiag, start=True, stop=False)
            p.mm(sl(b0, 0), C(C_IDENT), C(C_MSF if d == 0 else C_MSB), start=False, stop=True)
            p.mm(sl(b0, 1), C(C_ONES), Gdiag, start=True, stop=False)
            p.mm(sl(b0, 1), C(C_IDENT), C(C_MIF if d == 0 else C_MIB), start=False, stop=True)
            p.mm(sl(b0, 2), knT[:, tok], knT[:, tok])
            p.mm(sl(b0, 3), knT[:, tok], qnT[:, tok])
            p.ts(F(1), sl(b0, 0), sc_(2), 0.0, ALU.subtract, ALU.max)
            p.act(F(1), F(1), AF.Exp, scale=-1.0)
            p.ts(F(2), sl(b0, 1), sc_(2), 0.0, ALU.subtract, ALU.min)
            p.act(F(2), F(2), AF.Exp)
            A = B(0)
            p.stt(A, sl(b0, 2), sc_(8), F(1), ALU.mult, ALU.mult)
            p.tt(B(1), sl(b0, 3), F(2), ALU.mult)
            intraT = B(1)
            p.mm(sl(b1, 0), A, identb)
            N = B(2)
            evac(N, sl(b1, 0))
            P = B(3)
            p.tt(P, N, identb, ALU.add)
            M, Mt = N, A
            slot = 1
            for j in range(1, 6):
                Mn = B(4 + (j % 2) * 2); Mtn = B(5 + (j % 2) * 2)
                s_mt = sl(b1, slot % 4); slot += 1
                p.mm(s_mt, M, Mt)
                evac(Mtn, s_mt)
                if j < 5:
                    s_m = sl(b1, slot % 4); slot += 1
                    p.mm(s_m, Mt, M)
                    evac(Mn, s_m)
                s_p = sl(b1, slot % 4); slot += 1
                p.mm(s_p, Mtn, P)
                Pn = B(8 + (j % 2))
                p.tt(Pn, s_p, P, ALU.add)
                P = Pn
                M, Mt = Mn, Mtn
            Vb = B(10); Kbg = B(11); kdA = B(12); kdB = B(13)
            p.ts(Vb, vv[:, sc, :], sc_(1), None, ALU.mult, eng="gpsimd")
            p.ts(Kbg, kn[:, sc, :], sc_(5), None, ALU.mult, eng="gpsimd")
            p.ts(kdA, kn[:, sc, :], sc_(6), None, ALU.mult, eng="gpsimd")
            p.ts(kdB, kn[:, sc, :], sc_(7), None, ALU.mult, eng="gpsimd")
            p.mm(sl(b2, 0), P, Vb)
            p.mm(sl(b2, 1), Kbg, P)
            u_sb = F(3); wT = B(14)
            evac(u_sb, sl(b2, 0))
            evac(wT, sl(b2, 1))
            if mode < 2:
                p.copy(V(oacc_t[:, sc, :], ["oacc%d" % sc]), u_sb)
                return
            S = V(S_t[:, d], ["S%d" % d]); Sb = V(Sb_t[:, d], ["Sb%d" % d]); vn = V(vnew_t[:, d], ["vn%d" % d])
            oacc = V(oacc_t[:, sc, :], ["oacc%d" % sc])
            first = (step < NSC // 2)
            for half in ((0, 1) if d == 0 else (1, 0)):
                rows = slice(half * 64, (half + 1) * 64)
                ctok = slice(sc * 128 + half * 64, sc * 128 + (half + 1) * 64)
                p.mm(sl(b3, 0)[rows], wT[:, rows], Sb)
                p.tt(vn[rows], u_sb[rows], sl(b3, 0)[rows], ALU.subtract)
                p.mm(sl(b2, 2)[rows], qnT[:, ctok], Sb)
                p.mm(sl(b2, 3)[rows], intraT[:, rows], vn)
                p.mm(sl(b3, 1), kdA if half == 0 else kdB, vn)
                egl = sc_(9 + half)
                p.stt(Sb, S, egl, sl(b3, 1), ALU.mult, ALU.add)
                p.stt(S, S, egl, sl(b3, 1), ALU.mult, ALU.add)
                tiv = F(4)
                p.copy(tiv[rows], sl(b2, 3)[rows], eng="scalar")
                eG = V(pre_t[rows, d, 4, sc:sc + 1], ["pre%d_4" % d])
                if first:
                    p.stt(oacc[rows], sl(b2, 2)[rows], eG, tiv[rows], ALU.mult, ALU.add)
                else:
                    p.stt(F(5)[rows], sl(b2, 2)[rows], eG, tiv[rows], ALU.mult, ALU.add)
                    p.tt(oacc[rows], oacc[rows], F(5)[rows], ALU.add, eng="gpsimd")
        for step in range(nsc_run if mode > 0 else 0):
            do_sc(0, step, step)
            do_sc(1, NSC - 1 - step, step)
        for c4 in range(4):
            p.dma(dq(), V(o_d.rearrange("(n p) d -> p n d", p=128)[:, c4 * 16:(c4 + 1) * 16, :], []),
                  V(oacc_t[:, c4 * 16:(c4 + 1) * 16, :], ["oacc%d" % i for i in range(c4 * 16, (c4 + 1) * 16)]))
        p.finish("sync")
        p.emit(block)
    return nc


def prep_C(projT, gqkvn_full, P, l):
    cst = gdn_consts()
    ins = []
    for core in range(8):
        b = core // 4; h = core % 4
        tk = slice(b * 8192, (b + 1) * 8192)
        qT = gqkvn_full[h * 128:(h + 1) * 128, tk]
        kT = gqkvn_full[512 + h * 128:512 + (h + 1) * 128, tk]
        vT = gqkvn_full[1024 + h * 128:1024 + (h + 1) * 128, tk]
        def tokmaj(aT):
            return np.ascontiguousarray(aT.T.reshape(NSC, 128, 128).transpose(1, 0, 2))
        abT = np.stack([projT[3584 + h, tk], projT[3588 + h, tk], projT[3592 + h, tk], projT[3596 + h, tk]], -1)
        ab = np.ascontiguousarray(abT.reshape(NSC, 128, 4).transpose(1, 0, 2))
        par = np.zeros((128, 4), np.float32)
        par[:, 0] = P["gdn_a_log"][l][0, h]; par[:, 1] = P["gdn_a_log"][l][1, h]
        par[:, 2] = P["gdn_dt_bias"][l][0, h]; par[:, 3] = P["gdn_dt_bias"][l][1, h]
        ins.append({"qnT": np.ascontiguousarray(qT), "knT": np.ascontiguousarray(kT), "kn": tokmaj(kT),
                    "v": tokmaj(vT), "ab": ab, "par": par, "cst": cst})
    return ins


def build_D(last, n_exp=32):
    nc = bass.Bass("TRN2", target_bir_lowering=False)
    dt = nc.dram_tensor
    xT_d = dt("xT", [1024, NTOK], F32, kind="ExternalInput").ap()
    yc_d = dt("ycT", [256, NTOK], F32, kind="ExternalInput").ap()
    yn_d = dt("ynT", [256, NTOK], F32, kind="ExternalInput").ap()
    oT_d = dt("oT", [512, NTOK], F32, kind="ExternalInput").ap()
    zT_d = dt("zT", [512, NTOK], F32, kind="ExternalInput").ap()
    sm_d = dt("sm", [128, 64], F32, kind="ExternalInput").ap()
    wada_d = dt("wada", [1024, 4096], F32, kind="ExternalInput").ap()
    wout_d = dt("wout", [1024, 1024], F32, kind="ExternalInput").ap()
    wr_d = dt("wr", [1024, 36], F32, kind="ExternalInput").ap()
    rb_d = dt("rb", [128, 36], F32, kind="ExternalInput").ap()
    w1_d = dt("w1", [32, 1024, 512], F32, kind="ExternalInput").ap()
    w3_d = dt("w3", [32, 1024, 512], F32, kind="ExternalInput").ap()
    w2_d = dt("w2", [32, 512, 1024], F32, kind="ExternalInput").ap()
    sel_d = dt("sel", [32, 32, 128], F32, kind="ExternalInput").ap()
    idn_d = dt("idn", [128, 128], F32, kind="ExternalInput").ap()
    out_d = dt("outT", [1024, NTOK], F32, kind="ExternalOutput").ap()
    NB = NTOK // TB
    from contextlib import ExitStack
    with ExitStack() as es:
        def sb(name, shape, dtype):
            return es.enter_context(nc.sbuf_tensor(name, shape, dtype))
        xT_t = sb("xTs", [128, 8, NTOK], F32)
        yh_t = sb("yh", [128, 8, NTOK], BF16)
        wbuf_t = sb("wbuf", [128, 12288], BF16)
        stg_t = sb("stg", [128, 2, 4096], F32)
        hg_t = sb("hg", [128, 2, 4, TB], BF16)
        s1_t = sb("s1", [128, 2, TB], F32)
        gT_t = sb("gT", [32, NTOK], F32)
        sel_t = sb("sels", [32, 32, 128], F32)
        idn_t = sb("idns", [128, 128], F32)
        ones_t = sb("ones", [128, 128], F32)
        sq_t = sb("sq", [128, 2, TB], F32)
        tmp_t = sb("tmp", [128, 2, TB], F32)
        rs_t = sb("rs", [128, TB], F32)
        rstd_t = sb("rstd", [128, TB], F32)
        small = sb("small", [128, 128], F32)
        wr_t = sb("wrs", [128, 8, 36], F32)
        rb_t = sb("rbs", [128, 36], F32)
        rt_t = sb("rt", [128, 2, 160], F32)
        ps = es.enter_context(nc.psum_tensor("ps", [128, 8, 512], F32))
        block = es.enter_context(nc.Block())
        p = Prog(nc)
        dq_i = [0]

        def dq():
            dq_i[0] += 1
            return "sync" if dq_i[0] % 2 else "gpsimd"
        stg_i = [0]

        def stage():
            i = stg_i[0] % 2
            stg_i[0] += 1
            return V(stg_t[:, i], ["stg%d" % i])

        def PS(b):
            return V(ps[:, b, :], ["ps%d" % b])
        ones = V(ones_t, ["ones"]); idn = V(idn_t, ["idn"]); sel = V(sel_t, ["sel"])
        p.memset(ones, 1.0)
        sm = V(small[:, 0:64], ["sm"])
        epsv = V(small[:, 120:121], ["epsv"])
        p.memset(epsv, EPS)
        EPSV[0] = epsv
        p.dma("sync", sm, V(sm_d, []))
        p.dma("sync", idn, V(idn_d, []))
        p.dma("gpsimd", sel, V(sel_d, []))
        p.dma("sync", V(wr_t, ["wr"]), V(wr_d.rearrange("(c p) n -> p c n", p=128), []))
        p.dma("sync", V(rb_t, ["rb"]), V(rb_d, []))
        wr = V(wr_t, ["wr"]); rb = V(rb_t, ["rb"])
        cT = sm[:, 0:8]; nfw = sm[:, 8:16]; fw = sm[:, 16:24]; gnw = sm[:, 24:25]; bada = sm[:, 32:64]
        cond = V(small[:, 64:72], ["cond"]); mod_sb = V(small[:, 72:104], ["mod"]); A2 = V(small[:, 104:112], ["A2"])
        xT = V(xT_t, [])

        def xblk(kc, blk):
            return V(xT_t[:, kc, blk * TB:(blk + 1) * TB], ["x%d" % blk])

        def yblk(kc, blk):
            return V(yh_t[:, kc, blk * TB:(blk + 1) * TB], ["yh%d" % blk])
        allx = ["x%d" % b for b in range(NB)]; ally = ["yh%d" % b for b in range(NB)]
        for kc in range(8):
            p.dma(dq(), V(xT_t[:, kc, :], allx), V(xT_d[kc * 128:(kc + 1) * 128, :], []))
        wst = [V(stg_t[:, i].rearrange("p (c n) -> p c n", c=8), ["stg%d" % i]) for i in range(2)]
        emit_mod(p, nc, None, cT, wada_d, bada, 32, V(ps[:, 0, 0:32], ["ps0"]), wst, cond, mod_sb)
        gate1 = mod_sb[:, 0:8]; B2 = mod_sb[:, 8:16]; gate2 = mod_sb[:, 24:32]
        p.stt(A2, mod_sb[:, 16:24], 1.0, nfw, ALU.add, ALU.mult)
        for (src, k0) in ((yc_d, 0), (yn_d, 2)):
            st = stage()
            p.dma(dq(), st, V(src.rearrange("(c p) n -> p c n", p=128), []))
            p.copy(V(yh_t[:, k0:k0 + 2, :], ally), V(st.ap.rearrange("p (c n) -> p c n", c=2), st.keys), eng="gpsimd")
        for h in range(4):
            st = stage()
            p.dma(dq(), st[:, 0:NTOK], V(oT_d[h * 128:(h + 1) * 128, :], []))
            p.dma(dq(), st[:, NTOK:2 * NTOK], V(zT_d[h * 128:(h + 1) * 128, :], []))
            for blk in range(NB):
                o = st[:, blk * TB:(blk + 1) * TB]; z = st[:, NTOK + blk * TB:NTOK + (blk + 1) * TB]
                sq = V(sq_t[:, blk % 2], ["sq%d" % (blk % 2)]); tmp = V(tmp_t[:, blk % 2], ["tmp%d" % (blk % 2)])
                rs = V(rs_t, ["rs"])
                p.act(sq, o, AF.Square)
                p.mm(PS(1), ones, sq)
                p.act(rs, PS(1), AF.Sqrt, scale=1.0 / 128.0, bias=epsv)
                p.recip(rs, rs)
                p.stt(tmp, o, gnw, rs, ALU.mult, ALU.mult)
                p.act(sq, z, AF.Silu)
                p.tt(yblk(4 + h, blk), tmp, sq, ALU.mult)
        woutb = V(wbuf_t[:, 0:8192].rearrange("p (c n) -> p c n", c=8), ["w1b", "w3b"])
        for half in range(2):
            st = stage()
            p.dma(dq(), st, V(wout_d[:, half * 512:(half + 1) * 512].rearrange("(c p) n -> p c n", p=128), []))
            p.copy(woutb[:, :, half * 512:(half + 1) * 512], V(st.ap.rearrange("p (c n) -> p c n", c=8), st.keys), eng="gpsimd")
        gT = V(gT_t, ["gT"])
        for blk in range(NB):
            for dtl in range(8):
                bank = 1 + dtl % 4
                for kc in range(8):
                    p.mm(PS(bank), woutb[:, kc, dtl * 128:(dtl + 1) * 128], yblk(kc, blk), start=(kc == 0), stop=(kc == 7))
                p.stt(xblk(dtl, blk), PS(bank), gate1[:, dtl:dtl + 1], xblk(dtl, blk), ALU.mult, ALU.add)
            sq = [V(sq_t[:, i], ["sq%d" % i]) for i in range(2)]
            tmp = [V(tmp_t[:, i], ["tmp%d" % i]) for i in range(2)]
            st = stage()
            hF = V(st.ap.rearrange("p (c n) -> p c n", c=8), st.keys)
            xv = V(xT_t, ["x%d" % blk])
            hT = V(yh_t[:, :, blk * TB:(blk + 1) * TB], ["yh%d" % blk])
            emit_hmix_block(p, xv, blk, ones, sq, PS(0), V(rs_t, ["rs"]), V(rstd_t, ["rstd"]), tmp, A2, B2, hT, hF=hF)
            for tt_ in range(4):
                L = V(ps[:, 5 + tt_ % 2, 0:36], ["ps%d" % (5 + tt_ % 2)])
                for kc in range(8):
                    p.mm(L, hF[:, kc, tt_ * 128:(tt_ + 1) * 128], wr[:, kc, :], start=(kc == 0), stop=(kc == 7))
                R = V(rt_t[:, tt_ % 2], ["rt%d" % (tt_ % 2)])
                Lb = R[:, 0:36]; lg = R[:, 0:4]; le = R[:, 4:36]
                m = R[:, 36:37]; negm = R[:, 37:38]; ohg = R[:, 40:44]; e4 = R[:, 44:48]; ssum = R[:, 38:39]
                pgt = R[:, 39:40]; pen = R[:, 48:52]; lem = R[:, 52:84]; m1 = R[:, 84:85]; oh1 = R[:, 88:120]
                lem2 = R[:, 120:152]; m2 = R[:, 85:86]; dd = R[:, 86:87]; ed = R[:, 87:88]
                c1 = R[:, 152:153]; c2 = R[:, 153:154]; den = R[:, 154:155]
                p.tt(Lb, L, rb, ALU.add)
                p.reduce(m, lg, ALU.max)
                p.ts(ohg, lg, m, None, ALU.is_equal)
                p.ts(negm, m, -1.0, None, ALU.mult)
                p.act(e4, lg, AF.Exp, bias=negm)
                p.reduce(ssum, e4, ALU.add)
                p.recip(pgt, ssum)
                p.ts(pen, ohg, 1.0, 1e30, ALU.subtract, ALU.mult)
                for g in range(4):
                    p.ts(lem[:, g * 8:(g + 1) * 8], le[:, g * 8:(g + 1) * 8], pen[:, g:g + 1], None, ALU.add)
                p.reduce(m1, lem, ALU.max)
                p.ts(oh1, lem, m1, None, ALU.is_equal)
                p.stt(lem2, oh1, -1e30, lem, ALU.mult, ALU.add)
                p.reduce(m2, lem2, ALU.max)
                p.tt(dd, m2, m1, ALU.subtract)
                p.act(ed, dd, AF.Exp)
                p.ts(den, ed, 1.0, None, ALU.add)
                p.recip(den, den)
                p.tt(c1, den, pgt, ALU.mult)
                p.tt(c2, c1, ed, ALU.mult)
                p.ts(lem2, lem2, m2, None, ALU.is_equal)
                p.ts(oh1, oh1, c1, None, ALU.mult)
                p.stt(oh1, lem2, c2, oh1, ALU.mult, ALU.add)
                gp = V(ps[0:32, 7, 0:128], ["ps7"])
                p.tr(gp, oh1, idn)
                c0 = blk * TB + tt_ * 128
                p.copy(gT[:, c0:c0 + 128], gp, eng="scalar")
        w1b = V(wbuf_t[:, 0:4096].rearrange("p (c n) -> p c n", c=8), ["w1b"])
        w3b = V(wbuf_t[:, 4096:8192].rearrange("p (c n) -> p c n", c=8), ["w3b"])
        w2b = V(wbuf_t[:, 8192:12288].rearrange("p (c n) -> p c n", c=4), ["w2b"])
        it = 0
        for e in range(n_exp):
            for (dst, src, cc) in ((w1b, w1_d[e], 8), (w3b, w3_d[e], 8), (w2b, w2_d[e], 4)):
                st = stage()
                p.dma(dq(), st, V(src.rearrange("(c p) n -> p c n", p=128), []))
                p.copy(dst, V(st.ap.rearrange("p (c n) -> p c n", c=cc), st.keys), eng="gpsimd")
            for blk in range(NB):
                bsl = slice(blk * TB, (blk + 1) * TB)
                hT = V(yh_t[:, :, bsl], ["yh%d" % blk])
                gb = PS(4)
                p.mm(gb, sel[:, e, :], gT[:, bsl])
                hg = V(hg_t[:, it % 2], ["hg%d" % (it % 2)])
                for ht in range(4):
                    h1 = PS(ht % 2); h3 = PS(2 + ht % 2)
                    for kc in range(8):
                        p.mm(h1, w1b[:, kc, ht * 128:(ht + 1) * 128], hT[:, kc, :], start=(kc == 0), stop=(kc == 7))
                    for kc in range(8):
                        p.mm(h3, w3b[:, kc, ht * 128:(ht + 1) * 128], hT[:, kc, :], start=(kc == 0), stop=(kc == 7))
                    s1 = V(s1_t[:, ht % 2], ["s1%d" % (ht % 2)])
                    p.act(s1, h1, AF.Silu)
                    p.tt(s1, s1, gb, ALU.mult)
                    p.tt(hg[:, ht, :], h3, s1, ALU.mult)
                for dtl in range(8):
                    yb = PS(5 + dtl % 3)
                    for ht in range(4):
                        p.mm(yb, w2b[:, ht, dtl * 128:(dtl + 1) * 128], hg[:, ht, :], start=(ht == 0), stop=(ht == 3))
                    p.stt(xblk(dtl, blk), yb, gate2[:, dtl:dtl + 1], xblk(dtl, blk), ALU.mult, ALU.add)
                it += 1
        for blk in range(NB):
            bsl = slice(blk * TB, (blk + 1) * TB)
            if last:
                xv = V(xT_t, ["x%d" % blk])
                for kc in range(8):
                    s = V(sq_t[:, kc % 2], ["sq%d" % (kc % 2)])
                    p.act(s, xv[:, kc, bsl], AF.Square)
                    p.mm(PS(0), ones, s, start=(kc == 0), stop=(kc == 7))
                rs = V(rs_t, ["rs"]); rstd = V(rstd_t, ["rstd"])
                p.act(rs, PS(0), AF.Sqrt, scale=1.0 / 1024.0, bias=epsv)
                p.recip(rstd, rs)
                for kc in range(8):
                    p.stt(xblk(kc, blk), xblk(kc, blk), fw[:, kc:kc + 1], rstd, ALU.mult, ALU.mult)
            for kc in range(8):
                p.dma(dq(), V(out_d[kc * 128:(kc + 1) * 128, bsl], []), xblk(kc, blk))
        p.finish("sync")
        p.emit(block)
    return nc


def _lay_pc(v, n):
    return np.ascontiguousarray(np.asarray(v).reshape(n, 128).T)


def prep_D(xT_full, ycT_full, ynaT_full, oT_full, projT, P, l):
    sel = np.zeros((32, 32, 128), np.float32)
    for e in range(32):
        sel[e, e, :] = 1.0
    idn = np.eye(128, dtype=np.float32)
    wr = np.ascontiguousarray(np.concatenate([P["router_group_w"][l], P["router_expert_w"][l]], 1))
    rbv = np.concatenate([P["router_group_b"][l], P["router_expert_b"][l]])
    rb = np.ascontiguousarray(np.broadcast_to(rbv[None, :], (128, 36))).astype(np.float32)
    wada = np.ascontiguousarray(P["w_ada"][l][:, 2048:6144])
    ins = []
    for core in range(8):
        b = core // 4
        tk = slice(core * NTOK, (core + 1) * NTOK)
        sm = np.zeros((128, 64), np.float32)
        sm[:, 0:8] = _lay_pc(P["c"][b], 8)
        sm[:, 8:16] = _lay_pc(P["norm_ffn_w"][l], 8)
        sm[:, 16:24] = _lay_pc(P["final_norm_w"], 8)
        sm[:, 24] = P["gdn_norm_w"][l]
        sm[:, 32:64] = _lay_pc(P["b_ada"][l][2048:6144], 32)
        ins.append({
            "xT": np.ascontiguousarray(xT_full[:, tk]), "ycT": np.ascontiguousarray(ycT_full[:, tk]),
            "ynT": np.ascontiguousarray(ynaT_full[:, tk]), "oT": np.ascontiguousarray(oT_full[:, tk]),
            "zT": np.ascontiguousarray(projT[3072:3584, tk]), "sm": sm, "wada": wada,
            "wout": P["w_out"][l], "wr": wr, "rb": rb,
            "w1": P["expert_w1"][l], "w3": P["expert_w3"][l], "w2": P["expert_w2"][l], "sel": sel, "idn": idn,
        })
    return ins


def prep_A(xT_full, P, l):
    ins = []
    wada = np.ascontiguousarray(P["w_ada"][l][:, 0:2048])
    for core in range(8):
        b = core // 4
        ins.append({
            "xT": np.ascontiguousarray(xT_full[:, core * NTOK:(core + 1) * NTOK]),
            "cT": _lay_pc(P["c"][b], 8),
            "wada": wada,
            "bada": _lay_pc(P["b_ada"][l][0:2048], 16),
            "nw": _lay_pc(P["norm_mix_w"][l], 8),
            "win": P["w_in"][l],
        })
    return ins


TSEQ = 8192
NSH = 4


def _phase(nc):
    from contextlib import ExitStack
    return ExitStack()


def phase_A(nc, p, xsrc, W, S, l):
    with _phase(nc) as es:
        def sb(name, shape, dtype):
            return es.enter_context(nc.sbuf_tensor("A%d_%s" % (l, name), shape, dtype))
        xT = V(sb("xTs", [128, 8, NTOK], F32), ["xT"])
        wst_t = sb("wst", [128, 2, 8, 512], F32)
        wst = [V(wst_t[:, i], ["wst%d" % i]) for i in range(2)]
        winb = sb("winb", [128, 8, 3600], BF16)
        hT_t = sb("hT", [128, 2, 8, TB], BF16)
        sq_t = sb("sq", [128, 2, TB], F32)
        tmp_t = sb("tmp", [128, 2, TB], F32)
        rs = V(sb("rs", [128, TB], F32), ["rs"])
        rstd_b = V(sb("rstd", [128, TB], F32), ["rstd"])
        stage_t = sb("stage", [128, 4, TB], F32)
        tk_t = sb("tk", [128, 2, 272], F32)
        small = sb("small", [128, 64], F32)
        ones = V(sb("ones", [128, 128], F32), ["ones"])
        epsv = V(sb("epsv", [128, 1], F32), ["epsv"])
        EPSV[0] = epsv
        ps = es.enter_context(nc.psum_tensor("A%d_ps" % l, [128, 8, 512], F32))
        block = es.enter_context(nc.Block())
        cT = V(small[:, 0:8], ["cT"]); cond = V(small[:, 8:16], ["cond"])
        bada = V(small[:, 16:32], ["bada"]); mod_sb = V(small[:, 32:48], ["mod"])
        nw = V(small[:, 48:56], ["nw"]); A1 = V(small[:, 56:64], ["A1"])
        modps = V(ps[:, 0, 0:16], ["ps0"])
        ss = V(ps[:, 1, :], ["ps1"])
        p.memset(ones, 1.0)
        p.memset(epsv, EPS)
        p.dma("sync", cT, V(W["cT"], []))
        p.dma("sync", bada, V(W["badaA%d" % l], []))
        p.dma("sync", nw, V(W["nw%d" % l], []))
        emit_mod(p, nc, None, cT, W["wada%d" % l][:, 0:2048], bada, 16, modps, wst, cond, mod_sb)
        B1 = mod_sb[:, 0:8]
        p.stt(A1, mod_sb[:, 8:16], 1.0, nw, ALU.add, ALU.mult)
        win_d = W["win%d" % l]
        ci = 0
        for c0 in range(0, 3600, 512):
            cw = min(512, 3600 - c0)
            buf = wst[ci % 2]
            p.dma("sync" if ci % 2 == 0 else "gpsimd", buf[:, :, 0:cw],
                  V(win_d[:, c0:c0 + cw].rearrange("(c p) n -> p c n", p=128), []))
            for kc in range(8):
                dst = V(winb[:, kc, c0:c0 + cw], ["winb%d" % ci])
                p.copy(dst, buf[:, kc, 0:cw], eng=("gpsimd" if kc % 2 else "vector"))
            ci += 1
        nev = 0
        ntk = 0
        for s in range(NSH):
            t0 = s * NTOK
            for kc in range(8):
                p.dma("gpsimd" if kc % 2 else "sync", xT[:, kc, :], V(xsrc[kc * 128:(kc + 1) * 128, t0:t0 + NTOK], []))
            for blk in range(NTOK // TB):
                hT = V(hT_t[:, blk % 2], ["hT%d" % (blk % 2)])
                sq = [V(sq_t[:, i], ["sq%d" % i]) for i in range(2)]
                tmp = [V(tmp_t[:, i], ["tmp%d" % i]) for i in range(2)]
                emit_hmix_block(p, xT, blk, ones, sq, ss, rs, rstd_b, tmp, A1, B1, hT)
                for j in range(29):
                    rows = min(128, 3600 - j * 128)
                    bank = 2 + (nev % 5)
                    pj = V(ps[0:rows, bank, :], ["ps%d" % bank])
                    cj = (j * 128) // 512
                    for kc in range(8):
                        p.mm(pj, V(winb[:, kc, j * 128:j * 128 + rows], ["winb%d" % cj]), hT[:, kc, :],
                             start=(kc == 0), stop=(kc == 7))
                    st = V(stage_t[0:rows, nev % 4, :], ["stage%d" % (nev % 4)])
                    p.copy(st, pj, eng=("vector" if nev % 2 == 0 else "scalar"))
                    p.dma("sync" if nev % 2 == 0 else "gpsimd",
                          V(S["projT"][j * 128:j * 128 + rows, t0 + blk * TB:t0 + (blk + 1) * TB], []), st)
                    nev += 1
                for tt_ in range(4):
                    pt = V(ps[:, 7, 0:272], ["ps7"])
                    tsl = slice(tt_ * 128, (tt_ + 1) * 128)
                    for kc in range(8):
                        p.mm(pt[:, 0:256], hT[:, kc, tsl], V(winb[:, kc, 1280:1536], ["winb2"]),
                             start=(kc == 0), stop=(kc == 7))
                    for kc in range(8):
                        p.mm(pt[:, 256:272], hT[:, kc, tsl], V(winb[:, kc, 3584:3600], ["winb7"]),
                             start=(kc == 0), stop=(kc == 7))
                    tk = V(tk_t[:, ntk % 2], ["tk%d" % (ntk % 2)])
                    ntk += 1
                    p.copy(tk, pt, eng="vector")
                    r0 = t0 + blk * TB + tt_ * 128
                    p.dma("sync", V(S["nvtok"][r0:r0 + 128, :], []), tk[:, 0:256])
                    p.dma("gpsimd", V(S["abtok"][r0:r0 + 128, :], []), tk[:, 256:272])
        p.finish("sync")
        p.emit(block)


def phase_B(nc, p, W, S, l):
    with _phase(nc) as es:
        def sb(name, shape, dtype):
            return es.enter_context(nc.sbuf_tensor("B%d_%s" % (l, name), shape, dtype))
        NST = 3
        stg_t = sb("stg", [128, NST, 4096], F32)
        u_t = sb("u", [128, NTOK + 2], F32)
        acc_t = sb("acc", [128, NTOK], F32)
        sil_t = sb("sil", [128, 2, NTOK], F32)
        sq_t = sb("sq", [128, 2, TB], F32)
        rs_t = sb("rs", [128, 2, TB], F32)
        ones_t = sb("ones", [128, 128], F32)
        idn_t = sb("idn", [128, 128], F32)
        wsm = sb("wsm", [128, 64], F32)
        qb_t = sb("qb", [128, 2, NTOK], BF16)
        kb_t = sb("kb", [128, 2, NTOK], BF16)
        ksp_t = sb("ksp", [128, 2, NSPEC * 512], BF16)
        ve_t = sb("ve", [128, 16, 4, 65], BF16)
        vo_t = sb("vo", [128, 16, 4, 65], BF16)
        vs_t = sb("vs", [128, NSPEC * 4, 4, 65], BF16)
        bias_t = sb("biass", [128, 9, 1024], F32)
        maskf_t = sb("maskfs", [128, 1024], F32)
        sc_t = sb("sc", [128, 2, 512], F32)
        pT_t = sb("pT", [128, 2, 512], BF16)
        pos_t = sb("pos", [64, 2, 260], F32)
        rden_t = sb("rden", [64, 2, 4], F32)
        yst_t = sb("yst", [64, 2, 256], F32)
        ytr_t = sb("ytr", [128, 2, 2, 64], F32)
        ps = es.enter_context(nc.psum_tensor("B%d_ps" % l, [128, 8, 512], F32))
        block = es.enter_context(nc.Block())
        stg_i = [0]

        def stage():
            i = stg_i[0] % NST
            stg_i[0] += 1
            return V(stg_t[:, i], ["stg%d" % i])
        dq_i = [0]

        def dq():
            dq_i[0] += 1
            return "sync" if dq_i[0] % 2 else "gpsimd"
        ones = V(ones_t, ["ones"]); idn = V(idn_t, ["idn"])
        p.memset(ones, 1.0)
        p.dma("sync", idn, V(W["idn"], []))
        cw = V(wsm[:, 0:6], ["cw"])
        gw = V(wsm[:, 6:42], ["gw"])
        epsv = V(wsm[:, 42:43], ["epsv"])
        p.memset(epsv, EPS)
        p.dma("sync", cw, V(W["cw%d" % l].rearrange("p a b -> p (a b)"), []))
        p.dma("sync", gw, V(W["gw%d" % l].rearrange("p a b -> p (a b)"), []))
        u = V(u_t, ["u"]); acc = V(acc_t, ["acc"])
        maskf = V(maskf_t, ["maskf"])
        p.dma("sync", maskf, V(W["maskf"], []))
        projT = S["projT"]; nvtok = S["nvtok"]

        def conv3(src, wv, base):
            p.ts(acc, src[:, 0:NTOK], wv[:, base:base + 1], None, ALU.mult)
            p.stt(acc, src[:, 1:NTOK + 1], wv[:, base + 1:base + 2], acc, ALU.mult, ALU.add)
            p.stt(acc, src[:, 2:NTOK + 2], wv[:, base + 2:base + 3], acc, ALU.mult, ALU.add)

        def load_halo(st, row0, t0):
            lo = t0 - 1; hi = t0 + NTOK + 1
            a = 0; b = NTOK + 2
            if lo < 0:
                p.memset(st[:, 0:1], 0.0, eng="gpsimd")
                lo = 0; a = 1
            if hi > TSEQ:
                p.memset(st[:, NTOK + 1:NTOK + 2], 0.0, eng="gpsimd")
                hi = TSEQ; b = NTOK + 1
            p.dma(dq(), st[:, a:b], V(projT[row0:row0 + 128, lo:hi], []))
        ssb = 0
        for s in range(NSH):
            t0 = s * NTOK
            tsl = slice(t0, t0 + NTOK)
            for ct in range(2):
                bufs = []
                for g in range(3):
                    st = stage()
                    load_halo(st, g * 256 + ct * 128, t0)
                    bufs.append(st)
                cb, cc, cx = bufs
                p.tt(u, cc[:, 0:NTOK + 2], cx[:, 0:NTOK + 2], ALU.mult)
                conv3(u, cw, ct * 3)
                yo = V(sil_t[:, ct], ["sil%d" % ct])
                p.tt(yo, acc, cb[:, 1:NTOK + 1], ALU.mult)
                p.dma(dq(), V(S["ycT"][ct * 128:(ct + 1) * 128, tsl], []), yo)
            for ct in range(12):
                st = stage()
                load_halo(st, 1536 + ct * 128, t0)
                conv3(st, gw, ct * 3)
                so = V(sil_t[:, ct % 2], ["sil%d" % (ct % 2)])
                p.act(so, acc, AF.Silu)
                if ct < 8:
                    qscale = (128.0 ** -0.5) if ct < 4 else 1.0
                    for blk in range(NTOK // TB):
                        sl = slice(blk * TB, (blk + 1) * TB)
                        sq = V(sq_t[:, ssb % 2], ["sq%d" % (ssb % 2)])
                        rs = V(rs_t[:, ssb % 2], ["rs%d" % (ssb % 2)])
                        bank = ssb % 2
                        ss = V(ps[:, bank, :], ["ps%d" % bank])
                        ssb += 1
                        p.act(sq, so[:, sl], AF.Square)
                        p.mm(ss, ones, sq)
                        p.act(rs, ss, AF.Sqrt, bias=epsv)
                        p.recip(rs, rs)
                        p.stt(so[:, sl], so[:, sl], qscale, rs, ALU.mult, ALU.mult)
                p.dma(dq(), V(S["gqkvn"][ct * 128:(ct + 1) * 128, tsl], []), so)
            qb = V(qb_t, ["qb"]); kb = V(kb_t, ["kb"]); ksp = V(ksp_t, ["ksp"])
            for i in range(2):
                for (dst, r0) in ((qb, 768), (kb, 1024)):
                    st = stage()
                    p.dma(dq(), st[:, 0:NTOK], V(projT[r0 + i * 128:r0 + (i + 1) * 128, tsl], []))
                    p.copy(dst[:, i, :], st[:, 0:NTOK], eng="gpsimd")
            row0 = s * 32
            spec = []
            for si, r in enumerate(SPEC_ROWS):
                R = row0 + r
                rs_ = min(max(R - 4, 0), 120)
                spec.append(rs_)
            for i in range(2):
                st = stage()
                for si, rs_ in enumerate(spec):
                    p.dma(dq(), st[:, si * 512:(si + 1) * 512], V(projT[1024 + i * 128:1024 + (i + 1) * 128, rs_ * 64:rs_ * 64 + 512], []))
                p.copy(ksp[:, i, :], st, eng="gpsimd")
            ve = V(ve_t, ["ve"]); vo = V(vo_t, ["vo"]); vs = V(vs_t, ["vs"])
            for dst in (ve, vo, vs):
                p.memset(dst[:, :, :, 64:65], 1.0, eng="gpsimd")
            st = stage()
            p.dma(dq(), st, V(nvtok[t0:t0 + NTOK, :].rearrange("(n p) c -> p n c", p=128), []))
            p.copy(ve[:, :, :, 0:64], V(st.ap.rearrange("p (a h d) -> p a h d", a=16, h=4), st.keys), eng="vector")
            st = stage()
            p.dma(dq(), st[:, 0:15 * 256], V(nvtok[t0 + 64:t0 + 64 + 15 * 128, :].rearrange("(n p) c -> p n c", p=128), []))
            p.copy(vo[:, 0:15, :, 0:64], V(st.ap[:, 0:15 * 256].rearrange("p (a h d) -> p a h d", a=15, h=4), st.keys), eng="vector")
            for half in range(2):
                st = stage()
                for q4 in range(4):
                    si = half * 4 + q4
                    rs_ = spec[si]
                    p.dma(dq(), st[:, q4 * 1024:(q4 + 1) * 1024],
                          V(nvtok[rs_ * 64:rs_ * 64 + 512, :].rearrange("(n p) c -> p n c", p=128), []))
                p.copy(vs[:, half * 16:(half + 1) * 16, :, 0:64],
                       V(st.ap.rearrange("p (a h d) -> p a h d", a=16, h=4), st.keys), eng="vector")
            bias = V(bias_t, ["bias"])
            for i in range(9):
                p.dma(dq(), bias[:, i, :], V(W["rpbg%d" % l][s, :, i, :], []))
            for i in range(9):
                p.stt(bias[:, i, :], bias[:, i, :], 8.0, maskf, ALU.mult, ALU.add)
            for r in range(32):
                par = r % 2
                if r in SPEC_ROWS:
                    sidx = SPEC_ROWS.index(r)
                    bi = 1 + sidx

                    def ktile(tile, off, t, sidx=sidx):
                        return ksp[off:off + 64, tile, sidx * 512 + t * 128:sidx * 512 + (t + 1) * 128]

                    def vtile(t, h, sidx=sidx):
                        return vs[:, sidx * 4 + t, h, :]
                else:
                    bi = 0
                    lr = r - 4

                    def ktile(tile, off, t, lr=lr):
                        return kb[off:off + 64, tile, (lr + 2 * t) * 64:(lr + 2 * t + 2) * 64]
                    if lr % 2 == 0:
                        def vtile(t, h, lr=lr):
                            return ve[:, lr // 2 + t, h, :]
                    else:
                        def vtile(t, h, lr=lr):
                            return vo[:, (lr - 1) // 2 + t, h, :]
                pob = 6 + par
                po = V(ps[0:64, pob, 0:260], ["ps%d" % pob])
                for hl in range(2):
                    off = 64 * hl
                    bank = 2 + 2 * par + hl
                    psc = V(ps[:, bank, :], ["ps%d" % bank])
                    for j in range(2):
                        for t in range(4):
                            p.mm(psc[:, (j * 4 + t) * 64:(j * 4 + t + 1) * 64], ktile(j, off, t),
                                 qb[off:off + 64, j, r * 64:(r + 1) * 64])
                    sc = V(sc_t[:, hl], ["sc%d" % hl])
                    bview = V(bias.ap[:, bi, :].rearrange("p (h x) -> p h x", h=4)[:, hl::2, :], bias.keys)
                    p.tt(V(sc.ap.rearrange("p (h x) -> p h x", h=2), sc.keys),
                         V(psc.ap.rearrange("p (h x) -> p h x", h=2), psc.keys), bview, ALU.add)
                    pT = V(pT_t[:, hl], ["pT%d" % hl])
                    p.act(pT, sc, AF.Exp, scale=0.125)
                    for j in range(2):
                        h = 2 * j + hl
                        for t in range(4):
                            p.mm(po[:, h * 65:(h + 1) * 65], pT[:, (j * 4 + t) * 64:(j * 4 + t + 1) * 64], vtile(t, h),
                                 start=(t == 0), stop=(t == 3))
                pos = V(pos_t[:, par], ["pos%d" % par])
                p.copy(pos, po, eng="scalar")
                rden = V(rden_t[:, par], ["rden%d" % par])
                posv = V(pos.ap.rearrange("p (h d) -> p h d", h=4), pos.keys)
                p.recip(rden, V(posv.ap[:, :, 64:65].rearrange("p h o -> p (h o)"), pos.keys))
                yst = V(yst_t[:, par], ["yst%d" % par])
                for h in range(4):
                    p.ts(yst[:, h * 64:(h + 1) * 64], posv[:, h, 0:64], rden[:, h:h + 1], None, ALU.mult, eng="gpsimd")
                ytr = V(ytr_t[:, par], ["ytr%d" % par])
                for j in range(2):
                    pt = V(ps[:, 0 + j, 0:64], ["ps%d" % j])
                    p.tr(pt, yst[:, j * 128:(j + 1) * 128], idn[0:64, 0:64])
                    p.copy(ytr[:, j, :], pt, eng=("vector" if j else "scalar"))
                    p.dma(dq(), V(S["ynaT"][j * 128:(j + 1) * 128, t0 + r * 64:t0 + (r + 1) * 64], []), ytr[:, j, :])
        p.finish("sync")
        p.emit(block)


def phase_C(nc, p, W, S, l):
    T = TSEQ
    with _phase(nc) as es:
        def sb(name, shape, dtype):
            return es.enter_context(nc.sbuf_tensor("C%d_%s" % (l, name), shape, dtype))
        cst_t = sb("cst_s", [128, 11, 128], F32)
        cstb_t = sb("cstb", [128, 128], BF16)
        stg_t = sb("stg", [128, 2, 2048], F32)
        qnT_t = sb("qnTb", [128, T], BF16)
        knT_t = sb("knTb", [128, T], BF16)
        vT_t = sb("vTb", [128, T], BF16)
        kn_t = sb("knb", [128, NSC, 128], BF16)
        v_t = sb("vb", [128, NSC, 128], BF16)
        oacc_t = sb("oacc", [128, NSC, 128], F32)
        ab_t = sb("abs", [128, NSC, 16], F32)
        par_t = sb("pars", [128, 8], F32)
        NPS = 12
        pre_t = sb("pre", [128, 2, NPS, NSC], F32)
        S_t = sb("S", [128, 2, 128], F32)
        Sb_t = sb("Sb", [128, 2, 128], BF16)
        vnew_t = sb("vnew", [128, 2, 128], BF16)
        NF = 6; NB = 16
        wf_t = sb("wf", [128, 2, 2, NF, 128], F32)
        wb_t = sb("wb", [128, 2, 2, NB, 128], BF16)
        ps = es.enter_context(nc.psum_tensor("C%d_ps" % l, [128, 8, 512], F32))
        block = es.enter_context(nc.Block())
        cst = V(cst_t, ["cst"])

        def C(i):
            return cst[:, i, :]
        identb = V(cstb_t, ["cstb"])
        dq_i = [0]

        def dq():
            dq_i[0] += 1
            return "sync" if dq_i[0] % 2 else "gpsimd"
        p.dma("sync", cst, V(W["cst"], []))
        p.copy(identb, C(C_IDENT))
        ab = V(ab_t, ["ab"]); par = V(par_t, ["par"])
        p.dma("sync", ab, V(S["abtok"].rearrange("(n p) c -> p n c", p=128), []))
        qnT = V(qnT_t, ["qnT"]); knT = V(knT_t, ["knT"]); vT = V(vT_t, ["vT"])
        kn = V(kn_t, ["kn"]); vv = V(v_t, ["vv"])
        pre = V(pre_t, ["pre"])
        mA = V(cst.ap[:, C_SELA, 0:1], cst.keys)
        mB = V(cst.ap[:, C_SELB, 0:1], cst.keys)
        ev = [0]

        def evac(out, in_):
            ev[0] += 1
            p.copy(out, in_, eng=("scalar" if ev[0] % 2 else "vector"))
        si = 0
        for h in range(4):
            p.dma("sync", par[:, 0:4], V(W["par%d" % l][h], []))
            for (dst, r0) in ((qnT, h * 128), (knT, 512 + h * 128), (vT, 1024 + h * 128)):
                for c4 in range(4):
                    st = V(stg_t[:, si % 2], ["stg%d" % (si % 2)])
                    si += 1
                    p.dma(dq(), st, V(S["gqkvn"][r0:r0 + 128, c4 * 2048:(c4 + 1) * 2048], []))
                    p.copy(dst[:, c4 * 2048:(c4 + 1) * 2048], st, eng=("gpsimd" if c4 % 2 else "vector"))
            for sc in range(NSC):
                tok = slice(sc * 128, (sc + 1) * 128)
                bk = V(ps[:, sc % 2, :], ["ps%d" % (sc % 2)])
                p.mm(bk[:, 0:128], knT[:, tok], identb)
                p.mm(bk[:, 128:256], vT[:, tok], identb)
                evac(kn[:, sc, :], bk[:, 0:128])
                evac(vv[:, sc, :], bk[:, 128:256])
            p.act(par[:, 4:6], par[:, 0:2], AF.Exp)
            p.ts(par[:, 6:8], par[:, 4:6], -1.0, None, ALU.mult)
            for d in range(2):
                def S_(i, d=d):
                    return V(pre_t[:, d, i, :], ["pre%d_%d" % (d, i)])
                a_v = V(ab_t[:, :, 4 * d + h], ["ab"]); b_v = V(ab_t[:, :, 8 + 4 * d + h], ["ab"])
                p.ts(S_(11), a_v, par[:, 2 + d:3 + d], None, ALU.add)
                p.act(S_(11), S_(11), AF.Exp)
                p.act(S_(0), S_(11), AF.Ln, bias=1.0)
                p.ts(S_(0), S_(0), par[:, 6 + d:7 + d], None, ALU.mult)
                p.act(S_(1), b_v, AF.Sigmoid)
                tri = C(C_TRIF if d == 0 else C_TRIB)
                pb = V(ps[:, 0, :], ["ps0"])
                p.mm(pb[:, 0:NSC], tri, S_(0))
                p.mm(pb[:, 64:64 + NSC], C(C_BLK), S_(0))
                p.mm(pb[:, 128:128 + NSC], C(C_SELA), S_(0))
                p.mm(pb[:, 192:192 + NSC], C(C_SELB), S_(0))
                p.copy(S_(2), pb[:, 0:NSC])
                p.copy(S_(3), pb[:, 64:64 + NSC])
                p.act(S_(9), pb[:, 128:128 + NSC], AF.Exp)
                p.act(S_(10), pb[:, 192:192 + NSC], AF.Exp)
                p.act(S_(4), S_(2), AF.Exp)
                p.tt(S_(5), S_(1), S_(4), ALU.mult)
                p.tt(S_(11), S_(3), S_(2), ALU.subtract)
                p.act(S_(11), S_(11), AF.Exp)
                p.ts(S_(6), S_(11), mA, None, ALU.mult)
                p.ts(S_(7), S_(11), mB, None, ALU.mult)
                p.ts(S_(8), S_(1), -1.0, None, ALU.mult)
            for d in range(2):
                p.memset(V(S_t[:, d], ["S%d" % d]), 0.0)
                p.memset(V(Sb_t[:, d], ["Sb%d" % d]), 0.0)
                p.memset(V(vnew_t[:, d], ["vn%d" % d]), 0.0)

            def do_sc(d, sc, step):
                par_ = step % 2
                tok = slice(sc * 128, (sc + 1) * 128)

                def sc_(i):
                    return V(pre_t[:, d, i, sc:sc + 1], ["pre%d_%d" % (d, i)])

                def F(i):
                    return V(wf_t[:, d, par_, i], ["wf%d%d_%d" % (d, par_, i)])

                def B(i):
                    return V(wb_t[:, d, par_, i], ["wb%d%d_%d" % (d, par_, i)])
                b0 = V(ps[:, 4 * d + 0, :], ["ps%d" % (4 * d)])
                b1 = V(ps[:, 4 * d + 1, :], ["ps%d" % (4 * d + 1)])
                b2 = V(ps[:, 4 * d + 2, :], ["ps%d" % (4 * d + 2)])
                b3 = V(ps[:, 4 * d + 3, :], ["ps%d" % (4 * d + 3)])

                def sl(bk, i):
                    return bk[:, i * 128:(i + 1) * 128]
                Gdiag = F(0)
                p.ts(Gdiag, C(C_IDENT), sc_(2), None, ALU.mult)
                p.mm(sl(b0, 0), C(C_ONES), Gdiag, start=True, stop=False)
                p.mm(sl(b0, 0), C(C_IDENT), C(C_MSF if d == 0 else C_MSB), start=False, stop=True)
                p.mm(sl(b0, 1), C(C_ONES), Gdiag, start=True, stop=False)
                p.mm(sl(b0, 1), C(C_IDENT), C(C_MIF if d == 0 else C_MIB), start=False, stop=True)
                p.mm(sl(b0, 2), knT[:, tok], knT[:, tok])
                p.mm(sl(b0, 3), knT[:, tok], qnT[:, tok])
                p.ts(F(1), sl(b0, 0), sc_(2), 0.0, ALU.subtract, ALU.max)
                p.act(F(1), F(1), AF.Exp, scale=-1.0)
                p.ts(F(2), sl(b0, 1), sc_(2), 0.0, ALU.subtract, ALU.min)
                p.act(F(2), F(2), AF.Exp)
                A = B(0)
                p.stt(A, sl(b0, 2), sc_(8), F(1), ALU.mult, ALU.mult)
                p.tt(B(1), sl(b0, 3), F(2), ALU.mult)
                intraT = B(1)
                p.mm(sl(b1, 0), A, identb)
                N = B(2)
                evac(N, sl(b1, 0))
                P = B(3)
                p.tt(P, N, identb, ALU.add)
                M, Mt = N, A
                slot = 1
                for j in range(1, 6):
                    Mn = B(4 + (j % 2) * 2); Mtn = B(5 + (j % 2) * 2)
                    s_mt = sl(b1, slot % 4); slot += 1
                    p.mm(s_mt, M, Mt)
                    evac(Mtn, s_mt)
                    if j < 5:
                        s_m = sl(b1, slot % 4); slot += 1
                        p.mm(s_m, Mt, M)
                        evac(Mn, s_m)
                    s_p = sl(b1, slot % 4); slot += 1
                    p.mm(s_p, Mtn, P)
                    Pn = B(8 + (j % 2))
                    p.tt(Pn, s_p, P, ALU.add)
                    P = Pn
                    M, Mt = Mn, Mtn
                Vb = B(10); Kbg = B(11); kdA = B(12); kdB = B(13)
                p.ts(Vb, vv[:, sc, :], sc_(1), None, ALU.mult, eng="gpsimd")
                p.ts(Kbg, kn[:, sc, :], sc_(5), None, ALU.mult, eng="gpsimd")
                p.ts(kdA, kn[:, sc, :], sc_(6), None, ALU.mult, eng="gpsimd")
                p.ts(kdB, kn[:, sc, :], sc_(7), None, ALU.mult, eng="gpsimd")
                p.mm(sl(b2, 0), P, Vb)
                p.mm(sl(b2, 1), Kbg, P)
                u_sb = F(3); wT = B(14)
                evac(u_sb, sl(b2, 0))
                evac(wT, sl(b2, 1))
                St = V(S_t[:, d], ["S%d" % d]); Sb = V(Sb_t[:, d], ["Sb%d" % d]); vn = V(vnew_t[:, d], ["vn%d" % d])
                oacc = V(oacc_t[:, sc, :], ["oacc%d" % sc])
                first = (step < NSC // 2)
                for half in ((0, 1) if d == 0 else (1, 0)):
                    rows = slice(half * 64, (half + 1) * 64)
                    ctok = slice(sc * 128 + half * 64, sc * 128 + (half + 1) * 64)
                    p.mm(sl(b3, 0)[rows], wT[:, rows], Sb)
                    p.tt(vn[rows], u_sb[rows], sl(b3, 0)[rows], ALU.subtract)
                    p.mm(sl(b2, 2)[rows], qnT[:, ctok], Sb)
                    p.mm(sl(b2, 3)[rows], intraT[:, rows], vn)
                    p.mm(sl(b3, 1), kdA if half == 0 else kdB, vn)
                    egl = sc_(9 + half)
                    p.stt(Sb, St, egl, sl(b3, 1), ALU.mult, ALU.add)
                    p.stt(St, St, egl, sl(b3, 1), ALU.mult, ALU.add)
                    tiv = F(4)
                    p.copy(tiv[rows], sl(b2, 3)[rows], eng="scalar")
                    eG = V(pre_t[rows, d, 4, sc:sc + 1], ["pre%d_4" % d])
                    if first:
                        p.stt(oacc[rows], sl(b2, 2)[rows], eG, tiv[rows], ALU.mult, ALU.add)
                    else:
                        p.stt(F(5)[rows], sl(b2, 2)[rows], eG, tiv[rows], ALU.mult, ALU.add)
                        p.tt(oacc[rows], oacc[rows], F(5)[rows], ALU.add, eng="gpsimd")
            for step in range(NSC):
                do_sc(0, step, step)
                do_sc(1, NSC - 1 - step, step)
            for c4 in range(4):
                st = V(stg_t[:, si % 2], ["stg%d" % (si % 2)])
                si += 1
                for q in range(16):
                    sc = c4 * 16 + q
                    bk = V(ps[:, sc % 2, 0:128], ["ps%d" % (sc % 2)])
                    p.tr(bk, V(oacc_t[:, sc, :], ["oacc%d" % sc]), C(C_IDENT))
                    evac(st[:, q * 128:(q + 1) * 128], bk)
                p.dma(dq(), V(S["oT"][h * 128:(h + 1) * 128, c4 * 2048:(c4 + 1) * 2048], []), st)
        p.finish("sync")
        p.emit(block)


def phase_D(nc, p, xsrc, xdst, W, S, l, last):
    NBk = NTOK // TB
    with _phase(nc) as es:
        def sb(name, shape, dtype):
            return es.enter_context(nc.sbuf_tensor("D%d_%s" % (l, name), shape, dtype))
        xT_t = sb("xTs", [128, 8, NTOK], F32)
        yh_t = sb("yh", [128, 8, NTOK], BF16)
        wbuf_t = sb("wbuf", [128, 12288], BF16)
        stg_t = sb("stg", [128, 2, 4096], F32)
        hg_t = sb("hg", [128, 2, 4, TB], BF16)
        s1_t = sb("s1", [128, 2, TB], F32)
        gT_t = sb("gT", [32, NTOK], F32)
        sel_t = sb("sels", [32, 32, 128], F32)
        idn_t = sb("idns", [128, 128], F32)
        ones_t = sb("ones", [128, 128], F32)
        sq_t = sb("sq", [128, 2, TB], F32)
        tmp_t = sb("tmp", [128, 2, TB], F32)
        rs_t = sb("rs", [128, TB], F32)
        rstd_t = sb("rstd", [128, TB], F32)
        small = sb("small", [128, 128], F32)
        wr_t = sb("wrs", [128, 8, 36], F32)
        rb_t = sb("rbs", [128, 36], F32)
        rt_t = sb("rt", [128, 2, 160], F32)
        ps = es.enter_context(nc.psum_tensor("D%d_ps" % l, [128, 8, 512], F32))
        block = es.enter_context(nc.Block())
        dq_i = [0]

        def dq():
            dq_i[0] += 1
            return "sync" if dq_i[0] % 2 else "gpsimd"
        stg_i = [0]

        def stage():
            i = stg_i[0] % 2
            stg_i[0] += 1
            return V(stg_t[:, i], ["stg%d" % i])

        def PS(b):
            return V(ps[:, b, :], ["ps%d" % b])
        ones = V(ones_t, ["ones"]); idn = V(idn_t, ["idn"]); sel = V(sel_t, ["sel"])
        p.memset(ones, 1.0)
        sm = V(small[:, 0:64], ["sm"])
        epsv = V(small[:, 120:121], ["epsv"])
        p.memset(epsv, EPS)
        EPSV[0] = epsv
        p.dma("sync", sm, V(W["smD%d" % l], []))
        p.dma("sync", idn, V(W["idn"], []))
        p.dma("gpsimd", sel, V(W["sel"], []))
        p.dma("sync", V(wr_t, ["wr"]), V(W["wr%d" % l].rearrange("(c p) n -> p c n", p=128), []))
        p.dma("sync", V(rb_t, ["rb"]), V(W["rb%d" % l], []))
        wr = V(wr_t, ["wr"]); rb = V(rb_t, ["rb"])
        cT = sm[:, 0:8]; nfw = sm[:, 8:16]; fw = sm[:, 16:24]; gnw = sm[:, 24:25]; bada = sm[:, 32:64]
        cond = V(small[:, 64:72], ["cond"]); mod_sb = V(small[:, 72:104], ["mod"]); A2 = V(small[:, 104:112], ["A2"])
        wst = [V(stg_t[:, i].rearrange("p (c n) -> p c n", c=8), ["stg%d" % i]) for i in range(2)]
        emit_mod(p, nc, None, cT, W["wada%d" % l][:, 2048:6144], bada, 32, V(ps[:, 0, 0:32], ["ps0"]), wst, cond, mod_sb)
        gate1 = mod_sb[:, 0:8]; B2 = mod_sb[:, 8:16]; gate2 = mod_sb[:, 24:32]
        p.stt(A2, mod_sb[:, 16:24], 1.0, nfw, ALU.add, ALU.mult)
        w1_d = W["w1_%d" % l]; w3_d = W["w3_%d" % l]; w2_d = W["w2_%d" % l]; wout_d = W["wout%d" % l]
        it = 0
        for s in range(NSH):
            t0 = s * NTOK
            tsl = slice(t0, t0 + NTOK)

            def xblk(kc, blk):
                return V(xT_t[:, kc, blk * TB:(blk + 1) * TB], ["x%d" % blk])

            def yblk(kc, blk):
                return V(yh_t[:, kc, blk * TB:(blk + 1) * TB], ["yh%d" % blk])
            allx = ["x%d" % b for b in range(NBk)]; ally = ["yh%d" % b for b in range(NBk)]
            for kc in range(8):
                p.dma(dq(), V(xT_t[:, kc, :], allx), V(xsrc[kc * 128:(kc + 1) * 128, tsl], []))
            for (src, k0) in ((S["ycT"], 0), (S["ynaT"], 2)):
                st = stage()
                p.dma(dq(), st, V(src[:, tsl].rearrange("(c p) n -> p c n", p=128), []))
                p.copy(V(yh_t[:, k0:k0 + 2, :], ally), V(st.ap.rearrange("p (c n) -> p c n", c=2), st.keys), eng="gpsimd")
            for h in range(4):
                st = stage()
                p.dma(dq(), st[:, 0:NTOK], V(S["oT"][h * 128:(h + 1) * 128, tsl], []))
                p.dma(dq(), st[:, NTOK:2 * NTOK], V(S["projT"][3072 + h * 128:3072 + (h + 1) * 128, tsl], []))
                for blk in range(NBk):
                    o = st[:, blk * TB:(blk + 1) * TB]; z = st[:, NTOK + blk * TB:NTOK + (blk + 1) * TB]
                    sq = V(sq_t[:, blk % 2], ["sq%d" % (blk % 2)]); tmp = V(tmp_t[:, blk % 2], ["tmp%d" % (blk % 2)])
                    rs = V(rs_t, ["rs"])
                    p.act(sq, o, AF.Square)
                    p.mm(PS(1), ones, sq)
                    p.act(rs, PS(1), AF.Sqrt, scale=1.0 / 128.0, bias=epsv)
                    p.recip(rs, rs)
                    p.stt(tmp, o, gnw, rs, ALU.mult, ALU.mult)
                    p.act(sq, z, AF.Silu)
                    p.tt(yblk(4 + h, blk), tmp, sq, ALU.mult)
            woutb = V(wbuf_t[:, 0:8192].rearrange("p (c n) -> p c n", c=8), ["w1b", "w3b"])
            for half in range(2):
                st = stage()
                p.dma(dq(), st, V(wout_d[:, half * 512:(half + 1) * 512].rearrange("(c p) n -> p c n", p=128), []))
                p.copy(woutb[:, :, half * 512:(half + 1) * 512], V(st.ap.rearrange("p (c n) -> p c n", c=8), st.keys), eng="gpsimd")
            gT = V(gT_t, ["gT"])
            for blk in range(NBk):
                for dtl in range(8):
                    bank = 1 + dtl % 4
                    for kc in range(8):
                        p.mm(PS(bank), woutb[:, kc, dtl * 128:(dtl + 1) * 128], yblk(kc, blk), start=(kc == 0), stop=(kc == 7))
                    p.stt(xblk(dtl, blk), PS(bank), gate1[:, dtl:dtl + 1], xblk(dtl, blk), ALU.mult, ALU.add)
                sq = [V(sq_t[:, i], ["sq%d" % i]) for i in range(2)]
                tmp = [V(tmp_t[:, i], ["tmp%d" % i]) for i in range(2)]
                st = stage()
                hF = V(st.ap.rearrange("p (c n) -> p c n", c=8), st.keys)
                xv = V(xT_t, ["x%d" % blk])
                hT = V(yh_t[:, :, blk * TB:(blk + 1) * TB], ["yh%d" % blk])
                emit_hmix_block(p, xv, blk, ones, sq, PS(0), V(rs_t, ["rs"]), V(rstd_t, ["rstd"]), tmp, A2, B2, hT, hF=hF)
                for tt_ in range(4):
                    L = V(ps[:, 5 + tt_ % 2, 0:36], ["ps%d" % (5 + tt_ % 2)])
                    for kc in range(8):
                        p.mm(L, hF[:, kc, tt_ * 128:(tt_ + 1) * 128], wr[:, kc, :], start=(kc == 0), stop=(kc == 7))
                    R = V(rt_t[:, tt_ % 2], ["rt%d" % (tt_ % 2)])
                    Lb = R[:, 0:36]; lg = R[:, 0:4]; le = R[:, 4:36]
                    m = R[:, 36:37]; negm = R[:, 37:38]; ohg = R[:, 40:44]; e4 = R[:, 44:48]; ssum = R[:, 38:39]
                    pgt = R[:, 39:40]; pen = R[:, 48:52]; lem = R[:, 52:84]; m1 = R[:, 84:85]; oh1 = R[:, 88:120]
                    lem2 = R[:, 120:152]; m2 = R[:, 85:86]; dd = R[:, 86:87]; ed = R[:, 87:88]
                    c1 = R[:, 152:153]; c2 = R[:, 153:154]; den = R[:, 154:155]
                    p.tt(Lb, L, rb, ALU.add)
                    p.reduce(m, lg, ALU.max)
                    p.ts(ohg, lg, m, None, ALU.is_equal)
                    p.ts(negm, m, -1.0, None, ALU.mult)
                    p.act(e4, lg, AF.Exp, bias=negm)
                    p.reduce(ssum, e4, ALU.add)
                    p.recip(pgt, ssum)
                    p.ts(pen, ohg, 1.0, 1e30, ALU.subtract, ALU.mult)
                    for g in range(4):
                        p.ts(lem[:, g * 8:(g + 1) * 8], le[:, g * 8:(g + 1) * 8], pen[:, g:g + 1], None, ALU.add)
                    p.reduce(m1, lem, ALU.max)
                    p.ts(oh1, lem, m1, None, ALU.is_equal)
                    p.stt(lem2, oh1, -1e30, lem, ALU.mult, ALU.add)
                    p.reduce(m2, lem2, ALU.max)
                    p.tt(dd, m2, m1, ALU.subtract)
                    p.act(ed, dd, AF.Exp)
                    p.ts(den, ed, 1.0, None, ALU.add)
                    p.recip(den, den)
                    p.tt(c1, den, pgt, ALU.mult)
                    p.tt(c2, c1, ed, ALU.mult)
                    p.ts(lem2, lem2, m2, None, ALU.is_equal)
                    p.ts(oh1, oh1, c1, None, ALU.mult)
                    p.stt(oh1, lem2, c2, oh1, ALU.mult, ALU.add)
                    gp = V(ps[0:32, 7, 0:128], ["ps7"])
                    p.tr(gp, oh1, idn)
                    c0 = blk * TB + tt_ * 128
                    p.copy(gT[:, c0:c0 + 128], gp, eng="scalar")
            w1b = V(wbuf_t[:, 0:4096].rearrange("p (c n) -> p c n", c=8), ["w1b"])
            w3b = V(wbuf_t[:, 4096:8192].rearrange("p (c n) -> p c n", c=8), ["w3b"])
            w2b = V(wbuf_t[:, 8192:12288].rearrange("p (c n) -> p c n", c=4), ["w2b"])
            for e in range(32):
                for (dst, src, cc) in ((w1b, w1_d[e], 8), (w3b, w3_d[e], 8), (w2b, w2_d[e], 4)):
                    st = stage()
                    p.dma(dq(), st, V(src.rearrange("(c p) n -> p c n", p=128), []))
                    p.copy(dst, V(st.ap.rearrange("p (c n) -> p c n", c=cc), st.keys), eng="gpsimd")
                for blk in range(NBk):
                    bsl = slice(blk * TB, (blk + 1) * TB)
                    hT = V(yh_t[:, :, bsl], ["yh%d" % blk])
                    gb = PS(4)
                    p.mm(gb, sel[:, e, :], gT[:, bsl])
                    hg = V(hg_t[:, it % 2], ["hg%d" % (it % 2)])
                    for ht in range(4):
                        h1 = PS(ht % 2); h3 = PS(2 + ht % 2)
                        for kc in range(8):
                            p.mm(h1, w1b[:, kc, ht * 128:(ht + 1) * 128], hT[:, kc, :], start=(kc == 0), stop=(kc == 7))
                        for kc in range(8):
                            p.mm(h3, w3b[:, kc, ht * 128:(ht + 1) * 128], hT[:, kc, :], start=(kc == 0), stop=(kc == 7))
                        s1 = V(s1_t[:, ht % 2], ["s1%d" % (ht % 2)])
                        p.act(s1, h1, AF.Silu)
                        p.tt(s1, s1, gb, ALU.mult)
                        p.tt(hg[:, ht, :], h3, s1, ALU.mult)
                    for dtl in range(8):
                        yb = PS(5 + dtl % 3)
                        for ht in range(4):
                            p.mm(yb, w2b[:, ht, dtl * 128:(dtl + 1) * 128], hg[:, ht, :], start=(ht == 0), stop=(ht == 3))
                        p.stt(xblk(dtl, blk), yb, gate2[:, dtl:dtl + 1], xblk(dtl, blk), ALU.mult, ALU.add)
                    it += 1
            for blk in range(NBk):
                bsl = slice(blk * TB, (blk + 1) * TB)
                if last:
                    xv = V(xT_t, ["x%d" % blk])
                    for kc in range(8):
                        sqv = V(sq_t[:, kc % 2], ["sq%d" % (kc % 2)])
                        p.act(sqv, xv[:, kc, bsl], AF.Square)
                        p.mm(PS(0), ones, sqv, start=(kc == 0), stop=(kc == 7))
                    rs = V(rs_t, ["rs"]); rstd = V(rstd_t, ["rstd"])
                    p.act(rs, PS(0), AF.Sqrt, scale=1.0 / 1024.0, bias=epsv)
                    p.recip(rstd, rs)
                    for kc in range(8):
                        p.stt(xblk(kc, blk), xblk(kc, blk), fw[:, kc:kc + 1], rstd, ALU.mult, ALU.mult)
                for kc in range(8):
                    p.dma(dq(), V(xdst[kc * 128:(kc + 1) * 128, t0 + blk * TB:t0 + (blk + 1) * TB], []), xblk(kc, blk))
        p.finish("sync")
        p.emit(block)


FUSED_W_SHAPES = {
    "cT": [128, 8], "maskf": [128, 1024], "cst": [128, 11, 128], "sel": [32, 32, 128], "idn": [128, 128],
}
for _l in range(2):
    FUSED_W_SHAPES.update({
        "wada%d" % _l: [1024, 6144], "badaA%d" % _l: [128, 16], "nw%d" % _l: [128, 8], "win%d" % _l: [1024, 3600],
        "cw%d" % _l: [128, 2, 3], "gw%d" % _l: [128, 12, 3], "rpbg%d" % _l: [4, 128, 9, 1024], "par%d" % _l: [4, 128, 4],
        "smD%d" % _l: [128, 64], "wout%d" % _l: [1024, 1024], "wr%d" % _l: [1024, 36], "rb%d" % _l: [128, 36],
        "w1_%d" % _l: [32, 1024, 512], "w3_%d" % _l: [32, 1024, 512], "w2_%d" % _l: [32, 512, 1024],
    })


def build_F(nlayers=2, phases="ABCD"):
    nc = bass.Bass("TRN2", target_bir_lowering=False)
    dt = nc.dram_tensor
    xT_d = dt("xT", [1024, TSEQ], F32, kind="ExternalInput").ap()
    W = {k: dt(k, shp, F32, kind="ExternalInput").ap() for k, shp in FUSED_W_SHAPES.items()
         if not (k[-1].isdigit() and int(k[-1]) >= nlayers)}
    out_d = dt("outT", [1024, TSEQ], F32, kind="ExternalOutput").ap()
    S = {
        "projT": dt("s_projT", [3600, TSEQ], F32, kind="Internal").ap(),
        "nvtok": dt("s_nvtok", [TSEQ, 256], F32, kind="Internal").ap(),
        "abtok": dt("s_abtok", [TSEQ, 16], F32, kind="Internal").ap(),
        "ycT": dt("s_ycT", [256, TSEQ], F32, kind="Internal").ap(),
        "ynaT": dt("s_ynaT", [256, TSEQ], F32, kind="Internal").ap(),
        "gqkvn": dt("s_gqkvn", [1536, TSEQ], F32, kind="Internal").ap(),
        "oT": dt("s_oT", [512, TSEQ], F32, kind="Internal").ap(),
    }
    x1_d = dt("s_x1T", [1024, TSEQ], F32, kind="Internal").ap()
    dbg = {}
    if phases != "ABCD":
        for k, v in S.items():
            dbg[k] = dt("dbg_" + k, list(v.shape), F32, kind="ExternalOutput").ap()
    p = Prog(nc)
    for l in range(nlayers):
        xsrc = xT_d if l == 0 else x1_d
        last = (l == nlayers - 1)
        xdst = out_d if last else x1_d
        if "A" in phases:
            phase_A(nc, p, xsrc, W, S, l)
        if "B" in phases:
            phase_B(nc, p, W, S, l)
        if "C" in phases:
            phase_C(nc, p, W, S, l)
        if "D" in phases:
            phase_D(nc, p, xsrc, xdst, W, S, l, last and nlayers == 2)
    if dbg:
        with nc.Block() as block:
            i = 0
            for k in S:
                p.dma("sync" if i % 2 else "gpsimd", V(dbg[k], []), V(S[k], []))
                i += 1
            p.finish("sync")
            p.emit(block)
    return nc


def prep_F(P, nlayers=2):
    table_mask = None
    ins = []
    sel = np.zeros((32, 32, 128), np.float32)
    for e in range(32):
        sel[e, e, :] = 1.0
    idn = np.eye(128, dtype=np.float32)
    cst = gdn_consts()
    shared = {}
    for l in range(nlayers):
        table, maskf = _na_tables(P["na_rpb"][l])
        rpbg = np.zeros((4, 128, 9, 1024), np.float32)
        for s in range(4):
            rpbg[s, :, 0] = table(3)
            for si, r in enumerate(SPEC_ROWS):
                R = s * 32 + r
                rs = min(max(R - 4, 0), 120)
                rpbg[s, :, 1 + si] = table(rs - R + 7)
        par = np.zeros((4, 128, 4), np.float32)
        for h in range(4):
            par[h, :, 0] = P["gdn_a_log"][l][0, h]; par[h, :, 1] = P["gdn_a_log"][l][1, h]
            par[h, :, 2] = P["gdn_dt_bias"][l][0, h]; par[h, :, 3] = P["gdn_dt_bias"][l][1, h]
        rbv = np.concatenate([P["router_group_b"][l], P["router_expert_b"][l]])
        shared.update({
            "maskf": maskf,
            "wada%d" % l: P["w_ada"][l], "badaA%d" % l: _lay_pc(P["b_ada"][l][0:2048], 16),
            "nw%d" % l: _lay_pc(P["norm_mix_w"][l], 8), "win%d" % l: P["w_in"][l],
            "cw%d" % l: np.ascontiguousarray(P["conv_a_w"][l].T.reshape(2, 128, 3).transpose(1, 0, 2)),
            "gw%d" % l: np.ascontiguousarray(P["gdn_conv_w"][l].T.reshape(12, 128, 3).transpose(1, 0, 2)),
            "rpbg%d" % l: rpbg, "par%d" % l: par,
            "wout%d" % l: P["w_out"][l],
            "wr%d" % l: np.ascontiguousarray(np.concatenate([P["router_group_w"][l], P["router_expert_w"][l]], 1)),
            "rb%d" % l: np.ascontiguousarray(np.broadcast_to(rbv[None, :], (128, 36))).astype(np.float32),
            "w1_%d" % l: P["expert_w1"][l], "w3_%d" % l: P["expert_w3"][l], "w2_%d" % l: P["expert_w2"][l],
        })
    shared.update({"cst": cst, "sel": sel, "idn": idn})
    for b in range(2):
        d = dict(shared)
        d["xT"] = np.ascontiguousarray(P["x"][b].T)
        d["cT"] = _lay_pc(P["c"][b], 8)
        for l in range(nlayers):
            sm = np.zeros((128, 64), np.float32)
            sm[:, 0:8] = _lay_pc(P["c"][b], 8)
            sm[:, 8:16] = _lay_pc(P["norm_ffn_w"][l], 8)
            sm[:, 16:24] = _lay_pc(P["final_norm_w"], 8)
            sm[:, 24] = P["gdn_norm_w"][l]
            sm[:, 32:64] = _lay_pc(P["b_ada"][l][2048:6144], 32)
            d["smD%d" % l] = sm
        ins.append(d)
    return ins


_NC = {}


def kernel(**inputs):
    P = {k: np.ascontiguousarray(np.asarray(v, dtype=np.float32)) for k, v in inputs.items()}
    if "F" not in _NC:
        _NC["F"] = build_F()
    ins = prep_F(P)
    res = run_bass_kernel_spmd(_NC["F"], ins, core_ids=[0, 1]).results
    out = np.stack([np.ascontiguousarray(res[b]["outT"].T) for b in range(2)], 0)
    return out.astype(np.float32)
```

```python
import numpy as np
import concourse.bass as bass
import concourse.mybir as mybir
from concourse.bass_utils import run_bass_kernel_spmd

F32 = mybir.dt.float32
BF16 = mybir.dt.bfloat16
AF = mybir.ActivationFunctionType
ALU = mybir.AluOpType
AX = mybir.AxisListType

ENGS = ["tensor", "vector", "scalar", "gpsimd", "sync"]


class V:
    __slots__ = ("ap", "keys")

    def __init__(self, ap, keys):
        if ap is not None and type(ap).__name__.endswith("TensorHandle"):
            ap = ap[:]
        self.ap = ap
        self.keys = tuple(keys)

    def __getitem__(self, idx):
        return V(self.ap[idx], self.keys)

    def k(self, *keys):
        return V(self.ap, keys)


class Prog:
    def __init__(self, nc, n_dma_sems=24):
        self.nc = nc
        self.q = {e: [] for e in ENGS}
        self.cnt = {e: 0 for e in ENGS}
        self.sem = {e: nc.alloc_semaphore("c_" + e) for e in ENGS}
        self.dsem = [nc.alloc_semaphore("d_%d" % i) for i in range(n_dma_sems)]
        self.duse = [0] * n_dma_sems
        self.dnext = 0
        self.waited = {e: {} for e in ENGS}
        self.last_w = {}
        self.readers = {}
        self.semid = {}
        for e in ENGS:
            self.semid[id(self.sem[e])] = e
        self.nops = 0

    def _collect(self, eng, reads, writes):
        toks = []
        for r in reads:
            for key in r.keys:
                t = self.last_w.get(key)
                if t is not None:
                    toks.append((t, True))
                if isinstance(key, str) and key.startswith("ps"):
                    for t in self.readers.get(key, ()):
                        toks.append((t, False))
        for w in writes:
            for key in w.keys:
                t = self.last_w.get(key)
                if t is not None:
                    toks.append((t, True))
                for t in self.readers.get(key, ()):
                    toks.append((t, False))
        waits = {}
        mysem = self.sem[eng]
        for (sem, val), hard in toks:
            if sem is mysem:
                if eng == "tensor" or eng == "sync" or not hard:
                    continue
            sid = id(sem)
            if self.waited[eng].get(sid, 0) >= val:
                continue
            if waits.get(sid, (None, 0))[1] < val:
                waits[sid] = (sem, val)
        for sid, (sem, val) in waits.items():
            self.waited[eng][sid] = val
        return list(waits.values())

    def _commit(self, tok, reads, writes):
        for r in reads:
            for key in r.keys:
                self.readers.setdefault(key, []).append(tok)
        for w in writes:
            for key in w.keys:
                self.last_w[key] = tok
                self.readers[key] = []

    def op(self, eng, fn, reads=(), writes=()):
        waits = self._collect(eng, reads, writes)
        self.cnt[eng] += 1
        tok = (self.sem[eng], self.cnt[eng])
        self.q[eng].append((fn, waits, (self.sem[eng], 1)))
        self._commit(tok, reads, writes)
        self.nops += 1

    def dma(self, eng, out, in_, **kw):
        i = self.dnext
        self.dnext = (self.dnext + 1) % len(self.dsem)
        sem = self.dsem[i]
        waits = self._collect(eng, [in_], [out])
        if self.duse[i] > 0:
            sid = id(sem)
            val = 16 * self.duse[i]
            if self.waited[eng].get(sid, 0) < val:
                waits = [w for w in waits if w[0] is not sem] + [(sem, val)]
                self.waited[eng][sid] = val
        self.duse[i] += 1
        tok = (sem, 16 * self.duse[i])
        oap, iap = out.ap, in_.ap
        self.q[eng].append((lambda e: e.dma_start(out=oap, in_=iap, **kw), waits, (sem, 16)))
        self._commit(tok, [in_], [out])
        self.nops += 1
        return tok

    def finish(self, eng="sync"):
        waits = []
        for i, sem in enumerate(self.dsem):
            if self.duse[i] > 0:
                waits.append((sem, 16 * self.duse[i]))
        self.q[eng].append((None, waits, None))

    def wait_all(self, eng, views):
        waits = self._collect(eng, views, [])
        self.q[eng].append((None, waits, None))

    def emit(self, block):
        nc = self.nc
        for e in ENGS:
            items = self.q[e]
            if not items:
                continue

            def body(engine, items=items):
                for fn, waits, inc in items:
                    for sem, val in waits:
                        engine.wait_ge(sem, val)
                    if fn is None:
                        continue
                    ins = fn(engine)
                    if inc is not None:
                        ins.then_inc(inc[0], inc[1])
            getattr(block, e)(body)
        self.q = {e: [] for e in ENGS}

    def mm(self, out, lhsT, rhs, start=True, stop=True):
        o, l, r = out.ap, lhsT.ap, rhs.ap
        self.op("tensor", lambda e: e.matmul(o, l, r, start=start, stop=stop), [lhsT, rhs], [out])

    def tr(self, out, in_, ident):
        o, i, d = out.ap, in_.ap, ident.ap
        self.op("tensor", lambda e: e.transpose(o, i, d), [in_, ident], [out])

    def act(self, out, in_, func, bias=None, scale=None, accum_out=None, eng="scalar"):
        o, i = out.ap, in_.ap
        kw = {}
        rd = [in_]
        wr = [out]
        if bias is not None:
            if isinstance(bias, V):
                kw["bias"] = bias.ap
                rd.append(bias)
            else:
                kw["bias"] = bias
        if scale is not None:
            if isinstance(scale, V):
                kw["scale"] = scale.ap
                rd.append(scale)
            else:
                kw["scale"] = scale
        if accum_out is not None:
            kw["accum_out"] = accum_out.ap
            wr.append(accum_out)
        self.op("scalar", lambda e: e.activation(o, i, func, **kw), rd, wr)

    def tt(self, out, in0, in1, op, eng="vector"):
        o, a, b = out.ap, in0.ap, in1.ap
        self.op(eng, lambda e: e.tensor_tensor(o, a, b, op), [in0, in1], [out])

    def ts(self, out, in0, s1, s2, op0, op1=None, eng="vector", accum_out=None):
        o, a = out.ap, in0.ap
        rd = [in0]
        wr = [out]
        if isinstance(s1, V):
            rd.append(s1)
            s1 = s1.ap
        if isinstance(s2, V):
            rd.append(s2)
            s2 = s2.ap
        kw = {}
        if accum_out is not None:
            kw["accum_out"] = accum_out.ap
            wr.append(accum_out)
        if op1 is None:
            self.op(eng, lambda e: e.tensor_scalar(o, a, s1, s2, op0, **kw), rd, wr)
        else:
            self.op(eng, lambda e: e.tensor_scalar(o, a, s1, s2, op0, op1, **kw), rd, wr)

    def stt(self, out, in0, scalar, in1, op0, op1, eng="vector"):
        o, a, b = out.ap, in0.ap, in1.ap
        rd = [in0, in1]
        if isinstance(scalar, V):
            rd.append(scalar)
            scalar = scalar.ap
        self.op(eng, lambda e: e.scalar_tensor_tensor(o, a, scalar, b, op0, op1), rd, [out])

    def copy(self, out, in_, eng="vector"):
        o, i = out.ap, in_.ap
        if eng == "scalar":
            self.op(eng, lambda e: e.copy(o, i), [in_], [out])
        else:
            self.op(eng, lambda e: e.tensor_copy(o, i), [in_], [out])

    def memset(self, out, val, eng="vector"):
        o = out.ap
        self.op(eng, lambda e: e.memset(o, val), [], [out])

    def recip(self, out, in_):
        o, i = out.ap, in_.ap
        self.op("vector", lambda e: e.reciprocal(o, i), [in_], [out])

    def reduce(self, out, in_, op, axis=AX.X):
        o, i = out.ap, in_.ap
        self.op("vector", lambda e: e.tensor_reduce(o, i, axis, op), [in_], [out])


EPS = 1e-6
NTOK = 2048
TB = 512


def emit_mod(p, nc, pools, cT, wada_d, bada, ncol_tiles, modps, wst, cond, mod_sb):
    p.act(cond, cT, AF.Silu)
    nchunk = (ncol_tiles * 128) // 512
    for ch in range(nchunk):
        buf = wst[ch % 2]
        p.dma("sync" if ch % 2 == 0 else "gpsimd", buf,
              V(wada_d[:, ch * 512:(ch + 1) * 512].rearrange("(c p) n -> p c n", p=128), []))
        for jj in range(4):
            j = ch * 4 + jj
            for kc in range(8):
                p.mm(modps[:, j:j + 1], buf[:, kc, jj * 128:(jj + 1) * 128], cond[:, kc:kc + 1],
                     start=(kc == 0), stop=(kc == 7))
    p.tt(mod_sb, modps[:, 0:ncol_tiles], bada, ALU.add)


def emit_hmix_block(p, xT, blk, ones, sq, ss, rs, rstd_b, tmp, A1, B1, hT, hF=None):
    sl = slice(blk * TB, (blk + 1) * TB)
    for kc in range(8):
        s = sq[kc % 2]
        p.act(s, xT[:, kc, sl], AF.Square)
        p.mm(ss, ones, s, start=(kc == 0), stop=(kc == 7))
    p.act(rs, ss, AF.Sqrt, scale=1.0 / 1024.0, bias=EPSV[0])
    p.recip(rstd_b, rs)
    for kc in range(8):
        t = tmp[kc % 2]
        p.stt(t, xT[:, kc, sl], A1[:, kc:kc + 1], rstd_b, ALU.mult, ALU.mult)
        if hF is not None:
            p.ts(hF[:, kc, :], t, B1[:, kc:kc + 1], None, ALU.add)
            p.copy(hT[:, kc, :], hF[:, kc, :], eng="scalar")
        else:
            p.act(hT[:, kc, :], t, AF.Identity, bias=B1[:, kc:kc + 1])


EPSV = [None]


def build_A():
    nc = bass.Bass("TRN2", target_bir_lowering=False)
    dt = nc.dram_tensor
    xT_d = dt("xT", [1024, NTOK], F32, kind="ExternalInput").ap()
    cT_d = dt("cT", [128, 8], F32, kind="ExternalInput").ap()
    wada_d = dt("wada", [1024, 2048], F32, kind="ExternalInput").ap()
    bada_d = dt("bada", [128, 16], F32, kind="ExternalInput").ap()
    nw_d = dt("nw", [128, 8], F32, kind="ExternalInput").ap()
    win_d = dt("win", [1024, 3600], F32, kind="ExternalInput").ap()
    out_d = dt("projT", [3600, NTOK], F32, kind="ExternalOutput").ap()
    from contextlib import ExitStack
    with ExitStack() as es:
        def sb(name, shape, dtype):
            return es.enter_context(nc.sbuf_tensor(name, shape, dtype))
        xT = V(sb("xTs", [128, 8, NTOK], F32), ["xT"])
        wst_t = sb("wst", [128, 2, 8, 512], F32)
        wst = [V(wst_t[:, i], ["wst%d" % i]) for i in range(2)]
        winb = sb("winb", [128, 8, 3600], BF16)
        hT_t = sb("hT", [128, 2, 8, TB], BF16)
        sq_t = sb("sq", [128, 2, TB], F32)
        tmp_t = sb("tmp", [128, 2, TB], F32)
        rs = V(sb("rs", [128, TB], F32), ["rs"])
        rstd_b = V(sb("rstd", [128, TB], F32), ["rstd"])
        stage_t = sb("stage", [128, 4, TB], F32)
        small = sb("small", [128, 64], F32)
        ones = V(sb("ones", [128, 128], F32), ["ones"])
        epsv = V(sb("epsv", [128, 1], F32), ["epsv"])
        EPSV[0] = epsv
        ps = es.enter_context(nc.psum_tensor("ps", [128, 8, 512], F32))
        block = es.enter_context(nc.Block())
        p = Prog(nc)
        cT = V(small[:, 0:8], ["cT"]); cond = V(small[:, 8:16], ["cond"])
        bada = V(small[:, 16:32], ["bada"]); mod_sb = V(small[:, 32:48], ["mod"])
        nw = V(small[:, 48:56], ["nw"]); A1 = V(small[:, 56:64], ["A1"])
        modps = V(ps[:, 0, 0:16], ["ps0"])
        ss = V(ps[:, 1, :], ["ps1"])
        p.memset(ones, 1.0)
        p.memset(epsv, EPS)
        p.dma("sync", cT, V(cT_d, []))
        p.dma("sync", bada, V(bada_d, []))
        p.dma("sync", nw, V(nw_d, []))
        for kc in range(8):
            p.dma("gpsimd" if kc % 2 else "sync", xT[:, kc, :], V(xT_d[kc * 128:(kc + 1) * 128, :], []))
        emit_mod(p, nc, None, cT, wada_d, bada, 16, modps, wst, cond, mod_sb)
        B1 = mod_sb[:, 0:8]
        p.stt(A1, mod_sb[:, 8:16], 1.0, nw, ALU.add, ALU.mult)
        ci = 0
        for c0 in range(0, 3600, 512):
            cw = min(512, 3600 - c0)
            buf = wst[ci % 2]
            p.dma("sync" if ci % 2 == 0 else "gpsimd", buf[:, :, 0:cw],
                  V(win_d[:, c0:c0 + cw].rearrange("(c p) n -> p c n", p=128), []))
            for kc in range(8):
                dst = V(winb[:, kc, c0:c0 + cw], ["winb%d" % ci])
                p.copy(dst, buf[:, kc, 0:cw], eng=("gpsimd" if kc % 2 else "vector"))
            ci += 1
        nev = 0
        for blk in range(NTOK // TB):
            hT = V(hT_t[:, blk % 2], ["hT%d" % (blk % 2)])
            sq = [V(sq_t[:, i], ["sq%d" % i]) for i in range(2)]
            tmp = [V(tmp_t[:, i], ["tmp%d" % i]) for i in range(2)]
            emit_hmix_block(p, xT, blk, ones, sq, ss, rs, rstd_b, tmp, A1, B1, hT)
            for j in range(29):
                rows = min(128, 3600 - j * 128)
                bank = 2 + (nev % 6)
                pj = V(ps[0:rows, bank, :], ["ps%d" % bank])
                ci = (j * 128) // 512
                for kc in range(8):
                    p.mm(pj, V(winb[:, kc, j * 128:j * 128 + rows], ["winb%d" % ci]), hT[:, kc, :],
                         start=(kc == 0), stop=(kc == 7))
                st = V(stage_t[0:rows, nev % 4, :], ["stage%d" % (nev % 4)])
                if nev % 2 == 0:
                    p.copy(st, pj, eng="vector")
                else:
                    p.copy(st, pj, eng="scalar")
                p.dma("sync" if nev % 2 == 0 else "gpsimd",
                      V(out_d[j * 128:j * 128 + rows, blk * TB:(blk + 1) * TB], []), st)
                nev += 1
        p.finish("sync")
        p.emit(block)
    return nc


NSPEC = 8
SPEC_ROWS = [0, 1, 2, 3, 28, 29, 30, 31]


def build_B(parts=(1, 1, 1)):
    nc = bass.Bass("TRN2", target_bir_lowering=False)
    dt = nc.dram_tensor
    convin_d = dt("convin", [768, NTOK + 2], F32, kind="ExternalInput").ap()
    cw_d = dt("cw", [128, 2, 3], F32, kind="ExternalInput").ap()
    gqkv_d = dt("gqkv", [1536, NTOK + 2], F32, kind="ExternalInput").ap()
    gw_d = dt("gw", [128, 12, 3], F32, kind="ExternalInput").ap()
    naq_d = dt("naq", [256, NTOK], F32, kind="ExternalInput").ap()
    nak_d = dt("nak", [256, NTOK], F32, kind="ExternalInput").ap()
    nave_d = dt("nave", [128, 16, 256], F32, kind="ExternalInput").ap()
    navo_d = dt("navo", [128, 16, 256], F32, kind="ExternalInput").ap()
    kspec_d = dt("kspec", [256, NSPEC * 512], F32, kind="ExternalInput").ap()
    vspec_d = dt("vspec", [128, NSPEC * 4, 256], F32, kind="ExternalInput").ap()
    rpbg_d = dt("rpbg", [128, 9, 1024], F32, kind="ExternalInput").ap()
    maskf_d = dt("maskf", [128, 1024], F32, kind="ExternalInput").ap()
    yconv_d = dt("yconvT", [256, NTOK], F32, kind="ExternalOutput").ap()
    gqkvn_d = dt("gqkvn", [1536, NTOK], F32, kind="ExternalOutput").ap()
    yna_d = dt("yna", [NTOK, 256], F32, kind="ExternalOutput").ap()
    from contextlib import ExitStack
    with ExitStack() as es:
        def sb(name, shape, dtype):
            return es.enter_context(nc.sbuf_tensor(name, shape, dtype))
        NST = 3
        stg_t = sb("stg", [128, NST, 4096], F32)
        u_t = sb("u", [128, NTOK + 2], F32)
        acc_t = sb("acc", [128, NTOK], F32)
        sil_t = sb("sil", [128, 2, NTOK], F32)
        sq_t = sb("sq", [128, 2, TB], F32)
        rs_t = sb("rs", [128, 2, TB], F32)
        ones_t = sb("ones", [128, 128], F32)
        wsm = sb("wsm", [128, 64], F32)
        qb_t = sb("qb", [128, 2, NTOK], BF16)
        kb_t = sb("kb", [128, 2, NTOK], BF16)
        ksp_t = sb("ksp", [128, 2, NSPEC * 512], BF16)
        ve_t = sb("ve", [128, 16, 4, 65], BF16)
        vo_t = sb("vo", [128, 16, 4, 65], BF16)
        vs_t = sb("vs", [128, NSPEC * 4, 4, 65], BF16)
        bias_t = sb("biass", [128, 9, 1024], F32)
        maskf_t = sb("maskfs", [128, 1024], F32)
        sc_t = sb("sc", [128, 2, 512], F32)
        pT_t = sb("pT", [128, 2, 512], BF16)
        pos_t = sb("pos", [64, 2, 260], F32)
        rden_t = sb("rden", [64, 2, 4], F32)
        yst_t = sb("yst", [64, 2, 256], F32)
        ps = es.enter_context(nc.psum_tensor("ps", [128, 8, 512], F32))
        block = es.enter_context(nc.Block())
        p = Prog(nc)
        stg_i = [0]

        def stage():
            i = stg_i[0] % NST
            stg_i[0] += 1
            return V(stg_t[:, i], ["stg%d" % i])
        dq_i = [0]

        def dq():
            dq_i[0] += 1
            return "sync" if dq_i[0] % 2 else "gpsimd"
        ones = V(ones_t, ["ones"])
        p.memset(ones, 1.0)
        cw = V(wsm[:, 0:6], ["cw"])
        gw = V(wsm[:, 6:42], ["gw"])
        epsv = V(wsm[:, 42:43], ["epsv"])
        p.memset(epsv, EPS)
        p.dma("sync", cw, V(cw_d.rearrange("p a b -> p (a b)"), []))
        p.dma("sync", gw, V(gw_d.rearrange("p a b -> p (a b)"), []))
        u = V(u_t, ["u"]); acc = V(acc_t, ["acc"])

        def conv3(src, wv, base):
            p.ts(acc, src[:, 0:NTOK], wv[:, base:base + 1], None, ALU.mult)
            p.stt(acc, src[:, 1:NTOK + 1], wv[:, base + 1:base + 2], acc, ALU.mult, ALU.add)
            p.stt(acc, src[:, 2:NTOK + 2], wv[:, base + 2:base + 3], acc, ALU.mult, ALU.add)

        for ct in (range(2) if parts[0] else []):
            bufs = []
            for g in range(3):
                st = stage()
                p.dma(dq(), st[:, 0:NTOK + 2], V(convin_d[g * 256 + ct * 128:g * 256 + (ct + 1) * 128, :], []))
                bufs.append(st)
            cb, cc, cx = bufs
            p.tt(u, cc[:, 0:NTOK + 2], cx[:, 0:NTOK + 2], ALU.mult)
            conv3(u, cw, ct * 3)
            yo = V(sil_t[:, ct], ["sil%d" % ct])
            p.tt(yo, acc, cb[:, 1:NTOK + 1], ALU.mult)
            p.dma(dq(), V(yconv_d[ct * 128:(ct + 1) * 128, :], []), yo)

        ssb = 0
        for ct in (range(12) if parts[1] else []):
            st = stage()
            p.dma(dq(), st[:, 0:NTOK + 2], V(gqkv_d[ct * 128:(ct + 1) * 128, :], []))
            conv3(st, gw, ct * 3)
            so = V(sil_t[:, ct % 2], ["sil%d" % (ct % 2)])
            p.act(so, acc, AF.Silu)
            if ct < 8:
                qscale = (128.0 ** -0.5) if ct < 4 else 1.0
                for blk in range(NTOK // TB):
                    sl = slice(blk * TB, (blk + 1) * TB)
                    sq = V(sq_t[:, ssb % 2], ["sq%d" % (ssb % 2)])
                    rs = V(rs_t[:, ssb % 2], ["rs%d" % (ssb % 2)])
                    bank = ssb % 2
                    ss = V(ps[:, bank, :], ["ps%d" % bank])
                    ssb += 1
                    p.act(sq, so[:, sl], AF.Square)
                    p.mm(ss, ones, sq)
                    p.act(rs, ss, AF.Sqrt, bias=epsv)
                    p.recip(rs, rs)
                    p.stt(so[:, sl], so[:, sl], qscale, rs, ALU.mult, ALU.mult)
            p.dma(dq(), V(gqkvn_d[ct * 128:(ct + 1) * 128, :], []), so)

        def load_cast(dst_views, src_aps, width):
            for dv, sa in zip(dst_views, src_aps):
                st = stage()
                p.dma(dq(), st[:, 0:width], V(sa, []))
                p.copy(dv, st[:, 0:width], eng="gpsimd")
        qb = V(qb_t, ["qb"]); kb = V(kb_t, ["kb"]); ksp = V(ksp_t, ["ksp"])
        load_cast([qb[:, i, :] for i in range(2)], [naq_d[i * 128:(i + 1) * 128, :] for i in range(2)], NTOK)
        load_cast([kb[:, i, :] for i in range(2)], [nak_d[i * 128:(i + 1) * 128, :] for i in range(2)], NTOK)
        load_cast([ksp[:, i, :] for i in range(2)], [kspec_d[i * 128:(i + 1) * 128, :] for i in range(2)], NSPEC * 512)
        ve = V(ve_t, ["ve"]); vo = V(vo_t, ["vo"]); vs = V(vs_t, ["vs"])
        for (dst, src_d, nt) in ((ve, nave_d, 16), (vo, navo_d, 16), (vs, vspec_d, 32)):
            p.memset(dst[:, :, :, 64:65], 1.0, eng="gpsimd")
            for c0 in range(0, nt, 16):
                st = stage()
                p.dma(dq(), st[:, 0:16 * 256], V(src_d[:, c0:c0 + 16, :].rearrange("p a b -> p (a b)"), []))
                p.copy(dst[:, c0:c0 + 16, :, 0:64],
                       V(st.ap[:, 0:16 * 256].rearrange("p (a h d) -> p a h d", a=16, h=4), st.keys), eng="vector")
        bias = V(bias_t, ["bias"]); maskf = V(maskf_t, ["maskf"])
        p.dma("sync", maskf, V(maskf_d, []))
        for i in range(9):
            p.dma(dq(), bias[:, i, :], V(rpbg_d[:, i, :], []))
        for i in range(9):
            p.stt(bias[:, i, :], bias[:, i, :], 8.0, maskf, ALU.mult, ALU.add)
        for r in (range(32) if parts[2] else []):
            par = r % 2
            if r in SPEC_ROWS:
                s = SPEC_ROWS.index(r)
                bi = 1 + s

                def ktile(tile, off, t, s=s):
                    return ksp[off:off + 64, tile, s * 512 + t * 128:s * 512 + (t + 1) * 128]

                def vtile(t, h, s=s):
                    return vs[:, s * 4 + t, h, :]
            else:
                bi = 0
                lr = r - 4

                def ktile(tile, off, t, lr=lr):
                    return kb[off:off + 64, tile, (lr + 2 * t) * 64:(lr + 2 * t + 2) * 64]
                if lr % 2 == 0:
                    def vtile(t, h, lr=lr):
                        return ve[:, lr // 2 + t, h, :]
                else:
                    def vtile(t, h, lr=lr):
                        return vo[:, (lr - 1) // 2 + t, h, :]
            pob = 6 + par
            po = V(ps[0:64, pob, 0:260], ["ps%d" % pob])
            for hl in range(2):
                off = 64 * hl
                bank = 2 + 2 * par + hl
                psc = V(ps[:, bank, :], ["ps%d" % bank])
                for j in range(2):
                    for t in range(4):
                        p.mm(psc[:, (j * 4 + t) * 64:(j * 4 + t + 1) * 64], ktile(j, off, t),
                             qb[off:off + 64, j, r * 64:(r + 1) * 64])
                sc = V(sc_t[:, hl], ["sc%d" % hl])
                bview = V(bias.ap[:, bi, :].rearrange("p (h x) -> p h x", h=4)[:, hl::2, :], bias.keys)
                p.tt(V(sc.ap.rearrange("p (h x) -> p h x", h=2), sc.keys),
                     V(psc.ap.rearrange("p (h x) -> p h x", h=2), psc.keys), bview, ALU.add)
                pT = V(pT_t[:, hl], ["pT%d" % hl])
                p.act(pT, sc, AF.Exp, scale=0.125)
                for j in range(2):
                    h = 2 * j + hl
                    for t in range(4):
                        p.mm(po[:, h * 65:(h + 1) * 65], pT[:, (j * 4 + t) * 64:(j * 4 + t + 1) * 64], vtile(t, h),
                             start=(t == 0), stop=(t == 3))
            pos = V(pos_t[:, par], ["pos%d" % par])
            p.copy(pos, po, eng="scalar")
            rden = V(rden_t[:, par], ["rden%d" % par])
            posv = V(pos.ap.rearrange("p (h d) -> p h d", h=4), pos.keys)
            p.recip(rden, V(posv.ap[:, :, 64:65].rearrange("p h o -> p (h o)"), pos.keys))
            yst = V(yst_t[:, par], ["yst%d" % par])
            for h in range(4):
                p.ts(yst[:, h * 64:(h + 1) * 64], posv[:, h, 0:64], rden[:, h:h + 1], None, ALU.mult, eng="gpsimd")
            p.dma(dq(), V(yna_d[r * 64:(r + 1) * 64, :], []), yst)
        p.finish("sync")
        p.emit(block)
    return nc


def _pad_cols(a, lo, hi, n):
    out = np.zeros((a.shape[0], hi - lo), a.dtype)
    s0, s1 = max(lo, 0), min(hi, n)
    out[:, s0 - lo:s1 - lo] = a[:, s0:s1]
    return out


def _na_tables(rpb):
    kc = np.arange(64)[:, None]; qc = np.arange(64)[None, :]
    cs = np.clip(qc - 8, 0, 48)
    inwin = (kc >= cs) & (kc < cs + 16)
    dc = np.clip(kc - qc + 15, 0, 30)
    mask = np.where(inwin, 0.0, -240000.0).astype(np.float32)
    maskf = np.zeros((128, 4, 4, 64), np.float32)
    maskf[:] = np.concatenate([mask, mask], 0)[:, None, None, :]
    def table(dr0):
        tb = np.zeros((128, 4, 4, 64), np.float32)
        for a in range(2):
            for t in range(4):
                dr = dr0 + 2 * t + a
                tb[a * 64:(a + 1) * 64, :, t, :] = np.transpose(rpb[:, dr][:, dc], (1, 0, 2))
        return tb.reshape(128, 1024)
    return table, maskf.reshape(128, 1024)


def prep_B(projT, P, l):
    table, maskf = _na_tables(P["na_rpb"][l])
    cw = np.ascontiguousarray(P["conv_a_w"][l].T.reshape(2, 128, 3).transpose(1, 0, 2))
    gw = np.ascontiguousarray(P["gdn_conv_w"][l].T.reshape(12, 128, 3).transpose(1, 0, 2))
    ins = []
    for core in range(8):
        b = core // 4; t0 = (core % 4) * NTOK
        pb = projT[:, b * 8192:(b + 1) * 8192]
        vb = pb[1280:1536].T
        v = vb[t0:t0 + NTOK]
        nave = np.ascontiguousarray(v.reshape(16, 128, 256).transpose(1, 0, 2))
        navo = np.zeros((128, 16, 256), np.float32)
        navo[:, :15] = v[64:64 + 15 * 128].reshape(15, 128, 256).transpose(1, 0, 2)
        kspec = np.zeros((256, NSPEC * 512), np.float32)
        vspec = np.zeros((128, NSPEC * 4, 256), np.float32)
        rpbg = np.zeros((128, 9, 1024), np.float32)
        rpbg[:, 0] = table(3)
        row0 = (core % 4) * 32
        for s, r in enumerate(SPEC_ROWS):
            R = row0 + r
            rs = min(max(R - 4, 0), 120)
            kspec[:, s * 512:(s + 1) * 512] = pb[1024:1280, rs * 64:rs * 64 + 512]
            vspec[:, s * 4:(s + 1) * 4] = vb[rs * 64:rs * 64 + 512].reshape(4, 128, 256).transpose(1, 0, 2)
            rpbg[:, 1 + s] = table(rs - R + 7)
        ins.append({
            "convin": _pad_cols(pb[0:768], t0 - 1, t0 + NTOK + 1, 8192),
            "cw": cw, "gw": gw,
            "gqkv": _pad_cols(pb[1536:3072], t0 - 1, t0 + NTOK + 1, 8192),
            "naq": np.ascontiguousarray(pb[768:1024, t0:t0 + NTOK]),
            "nak": np.ascontiguousarray(pb[1024:1280, t0:t0 + NTOK]),
            "nave": nave, "navo": navo, "kspec": kspec, "vspec": vspec,
            "rpbg": rpbg, "maskf": maskf,
        })
    return ins


NSC = 64
C_IDENT, C_ONES, C_TRIF, C_TRIB, C_BLK, C_SELA, C_SELB, C_MSF, C_MSB, C_MIF, C_MIB = range(11)


def gdn_consts():
    i = np.arange(128)
    t = i[:, None]; c = i[None, :]
    same = (t // 64) == (c // 64)
    cs = np.zeros((128, 11, 128), np.float32)
    cs[:, C_IDENT] = np.eye(128)
    cs[:, C_ONES] = 1.0
    cs[:, C_TRIF] = same & (t <= c)
    cs[:, C_TRIB] = same & (t >= c)
    cs[:, C_BLK] = same
    cs[:, C_SELA] = (t < 64) & (c >= 0)
    cs[:, C_SELB] = (t >= 64) & (c >= 0)
    cc = i[:, None]; ss = i[None, :]
    same2 = (cc // 64) == (ss // 64)
    cs[:, C_MSF] = np.where(same2 & (cc > ss), 0.0, 30000.0)
    cs[:, C_MSB] = np.where(same2 & (cc < ss), 0.0, 30000.0)
    sp = i[:, None]; cf = i[None, :]
    cs[:, C_MIF] = np.where(same2 & (cf >= sp), 0.0, -30000.0)
    cs[:, C_MIB] = np.where(same2 & (cf <= sp), 0.0, -30000.0)
    return cs


def build_C(nsc_run=NSC, mode=2):
    nc = bass.Bass("TRN2", target_bir_lowering=False)
    dt = nc.dram_tensor
    T = NSC * 128
    qnT_d = dt("qnT", [128, T], F32, kind="ExternalInput").ap()
    knT_d = dt("knT", [128, T], F32, kind="ExternalInput").ap()
    kn_d = dt("kn", [128, NSC, 128], F32, kind="ExternalInput").ap()
    v_d = dt("v", [128, NSC, 128], F32, kind="ExternalInput").ap()
    ab_d = dt("ab", [128, NSC, 4], F32, kind="ExternalInput").ap()
    par_d = dt("par", [128, 4], F32, kind="ExternalInput").ap()
    cst_d = dt("cst", [128, 11, 128], F32, kind="ExternalInput").ap()
    o_d = dt("o", [T, 128], F32, kind="ExternalOutput").ap()
    from contextlib import ExitStack
    with ExitStack() as es:
        def sb(name, shape, dtype):
            return es.enter_context(nc.sbuf_tensor(name, shape, dtype))
        cst_t = sb("cst_s", [128, 11, 128], F32)
        cstb_t = sb("cstb", [128, 128], BF16)
        stg_t = sb("stg", [128, 2, 2048], F32)
        qnT_t = sb("qnTb", [128, T], BF16)
        knT_t = sb("knTb", [128, T], BF16)
        kn_t = sb("knb", [128, NSC, 128], BF16)
        v_t = sb("vb", [128, NSC, 128], BF16)
        oacc_t = sb("oacc", [128, NSC, 128], F32)
        ab_t = sb("abs", [128, NSC, 4], F32)
        par_t = sb("pars", [128, 8], F32)
        NPS = 12
        pre_t = sb("pre", [128, 2, NPS, NSC], F32)
        S_t = sb("S", [128, 2, 128], F32)
        Sb_t = sb("Sb", [128, 2, 128], BF16)
        vnew_t = sb("vnew", [128, 2, 128], BF16)
        NF = 6; NB = 16
        wf_t = sb("wf", [128, 2, 2, NF, 128], F32)
        wb_t = sb("wb", [128, 2, 2, NB, 128], BF16)
        ps = es.enter_context(nc.psum_tensor("ps", [128, 8, 512], F32))
        block = es.enter_context(nc.Block())
        p = Prog(nc)
        cst = V(cst_t, ["cst"])

        def C(i):
            return cst[:, i, :]
        identb = V(cstb_t, ["cstb"])
        dq_i = [0]

        def dq():
            dq_i[0] += 1
            return "sync" if dq_i[0] % 2 else "gpsimd"
        p.dma("sync", cst, V(cst_d, []))
        p.copy(identb, C(C_IDENT))
        ab = V(ab_t, ["ab"]); par = V(par_t, ["par"])
        p.dma("sync", ab, V(ab_d, []))
        p.dma("sync", par[:, 0:4], V(par_d, []))
        si = 0
        for (dst_t, src, kind) in ((qnT_t, qnT_d, "T"), (knT_t, knT_d, "T"), (kn_t, kn_d, "N"), (v_t, v_d, "N")):
            for c4 in range(4):
                st = V(stg_t[:, si % 2], ["stg%d" % (si % 2)])
                si += 1
                if kind == "T":
                    p.dma(dq(), st, V(src[:, c4 * 2048:(c4 + 1) * 2048], []))
                    p.copy(V(dst_t[:, c4 * 2048:(c4 + 1) * 2048], [dst_t.name if hasattr(dst_t, "name") else id(dst_t)]), st,
                           eng=("gpsimd" if c4 % 2 else "vector"))
                else:
                    p.dma(dq(), st, V(src[:, c4 * 16:(c4 + 1) * 16, :].rearrange("p a b -> p (a b)"), []))
                    p.copy(V(dst_t[:, c4 * 16:(c4 + 1) * 16, :].rearrange("p a b -> p (a b)"), [id(dst_t)]), st,
                           eng=("gpsimd" if c4 % 2 else "vector"))
        qnT = V(qnT_t, [qnT_t.name if hasattr(qnT_t, "name") else id(qnT_t)])
        knT = V(knT_t, [knT_t.name if hasattr(knT_t, "name") else id(knT_t)])
        kn = V(kn_t, [id(kn_t)]); vv = V(v_t, [id(v_t)])
        p.act(par[:, 4:6], par[:, 0:2], AF.Exp)
        p.ts(par[:, 6:8], par[:, 4:6], -1.0, None, ALU.mult)
        pre = V(pre_t, ["pre"])
        mA = V(cst.ap[:, C_SELA, 0:1], cst.keys)
        mB = V(cst.ap[:, C_SELB, 0:1], cst.keys)
        for d in range(2):
            def S_(i, d=d):
                return V(pre_t[:, d, i, :], ["pre%d_%d" % (d, i)])
            a_v = V(ab_t[:, :, d], ["ab"]); b_v = V(ab_t[:, :, 2 + d], ["ab"])
            p.ts(S_(11), a_v, par[:, 2 + d:3 + d], None, ALU.add)
            p.act(S_(11), S_(11), AF.Exp)
            p.act(S_(0), S_(11), AF.Ln, bias=1.0)
            p.ts(S_(0), S_(0), par[:, 6 + d:7 + d], None, ALU.mult)
            p.act(S_(1), b_v, AF.Sigmoid)
            tri = C(C_TRIF if d == 0 else C_TRIB)
            pb = V(ps[:, 0, :], ["ps0"])
            p.mm(pb[:, 0:NSC], tri, S_(0))
            p.mm(pb[:, 64:64 + NSC], C(C_BLK), S_(0))
            p.mm(pb[:, 128:128 + NSC], C(C_SELA), S_(0))
            p.mm(pb[:, 192:192 + NSC], C(C_SELB), S_(0))
            p.copy(S_(2), pb[:, 0:NSC])
            p.copy(S_(3), pb[:, 64:64 + NSC])
            p.act(S_(9), pb[:, 128:128 + NSC], AF.Exp)
            p.act(S_(10), pb[:, 192:192 + NSC], AF.Exp)
            p.act(S_(4), S_(2), AF.Exp)
            p.tt(S_(5), S_(1), S_(4), ALU.mult)
            p.tt(S_(11), S_(3), S_(2), ALU.subtract)
            p.act(S_(11), S_(11), AF.Exp)
            p.ts(S_(6), S_(11), mA, None, ALU.mult)
            p.ts(S_(7), S_(11), mB, None, ALU.mult)
            p.ts(S_(8), S_(1), -1.0, None, ALU.mult)
        for d in range(2):
            p.memset(V(S_t[:, d], ["S%d" % d]), 0.0)
            p.memset(V(Sb_t[:, d], ["Sb%d" % d]), 0.0)
            p.memset(V(vnew_t[:, d], ["vn%d" % d]), 0.0)
        ev = [0]

        def evac(out, in_):
            ev[0] += 1
            p.copy(out, in_, eng=("scalar" if ev[0] % 2 else "vector"))

        def do_sc(d, sc, step):
            par_ = step % 2
            tok = slice(sc * 128, (sc + 1) * 128)

            def sc_(i):
                return V(pre_t[:, d, i, sc:sc + 1], ["pre%d_%d" % (d, i)])

            def F(i):
                return V(wf_t[:, d, par_, i], ["wf%d%d_%d" % (d, par_, i)])

            def B(i):
                return V(wb_t[:, d, par_, i], ["wb%d%d_%d" % (d, par_, i)])
            b0 = V(ps[:, 4 * d + 0, :], ["ps%d" % (4 * d)])
            b1 = V(ps[:, 4 * d + 1, :], ["ps%d" % (4 * d + 1)])
            b2 = V(ps[:, 4 * d + 2, :], ["ps%d" % (4 * d + 2)])
            b3 = V(ps[:, 4 * d + 3, :], ["ps%d" % (4 * d + 3)])

            def sl(bk, i):
                return bk[:, i * 128:(i + 1) * 128]
            Gdiag = F(0)
            p.ts(Gdiag, C(C_IDENT), sc_(2), None, ALU.mult)
            p.mm(sl(b0, 0), C(C_ONES), Gdiag, start=True, stop=False)
            p.mm(sl(b0, 0), C(C_IDENT), C(C_MSF if d == 0 else C_MSB), start=False, stop=True)
            p.mm(sl(b0, 1), C(C_ONES), Gdiag, start=True, stop=False)
            p.mm(sl(b0, 1), C(C_IDENT), C(C_MIF if d == 0 else C_MIB), start=False, stop=True)
            p.mm(sl(b0, 2), knT[:, tok], knT[:, tok])
            p.mm(sl(b0, 3), knT[:, tok], qnT[:, tok])
            p.ts(F(1), sl(b0, 0), sc_(2), 0.0, ALU.subtract, ALU.max)
            p.act(F(1), F(1), AF.Exp, scale=-1.0)
            p.ts(F(2), sl(b0, 1), sc_(2), 0.0, ALU.subtract, ALU.min)
            p.act(F(2), F(2), AF.Exp)
            A = B(0)
            p.stt(A, sl(b0, 2), sc_(8), F(1), ALU.mult, ALU.mult)
            p.tt(B(1), sl(b0, 3), F(2), ALU.mult)
            intraT = B(1)
            p.mm(sl(b1, 0), A, identb)
            N = B(2)
            evac(N, sl(b1, 0))
            P = B(3)
            p.tt(P, N, identb, ALU.add)
            M, Mt = N, A
            slot = 1
            for j in range(1, 6):
                Mn = B(4 + (j % 2) * 2); Mtn = B(5 + (j % 2) * 2)
                s_mt = sl(b1, slot % 4); slot += 1
                p.mm(s_mt, M, Mt)
                evac(Mtn, s_mt)
                if j < 5:
                    s_m = sl(b1, slot % 4); slot += 1
                    p.mm(s_m, Mt, M)
                    evac(Mn, s_m)
                s_p = sl(b1, slot % 4); slot += 1
                p.mm(s_p, Mtn, P)
                Pn = B(8 + (j % 2))
                p.tt(Pn, s_p, P, ALU.add)
                P = Pn
                M, Mt = Mn, Mtn
            Vb = B(10); Kbg = B(11); kdA = B(12); kdB = B(13)
            p.ts(Vb, vv[:, sc, :], sc_(1), None, ALU.mult, eng="gpsimd")
            p.ts(Kbg, kn[:, sc, :], sc_(5), None, ALU.mult, eng="gpsimd")
            p.ts(kdA, kn[:, sc, :], sc_(6), None, ALU.mult, eng="gpsimd")
            p.ts(kdB, kn[:, sc, :], sc_(7), None, ALU.mult, eng="gpsimd")
            p.mm(sl(b2, 0), P, Vb)
            p.mm(sl(b2, 1), Kbg, P)
            u_sb = F(3); wT = B(14)
            evac(u_sb, sl(b2, 0))
            evac(wT, sl(b2, 1))
            if mode < 2:
                p.copy(V(oacc_t[:, sc, :], ["oacc%d" % sc]), u_sb)
                return
            S = V(S_t[:, d], ["S%d" % d]); Sb = V(Sb_t[:, d], ["Sb%d" % d]); vn = V(vnew_t[:, d], ["vn%d" % d])
            oacc = V(oacc_t[:, sc, :], ["oacc%d" % sc])
            first = (step < NSC // 2)
            for half in ((0, 1) if d == 0 else (1, 0)):
                rows = slice(half * 64, (half + 1) * 64)
                ctok = slice(sc * 128 + half * 64, sc * 128 + (half + 1) * 64)
                p.mm(sl(b3, 0)[rows], wT[:, rows], Sb)
                p.tt(vn[rows], u_sb[rows], sl(b3, 0)[rows], ALU.subtract)
                p.mm(sl(b2, 2)[rows], qnT[:, ctok], Sb)
                p.mm(sl(b2, 3)[rows], intraT[:, rows], vn)
                p.mm(sl(b3, 1), kdA if half == 0 else kdB, vn)
                egl = sc_(9 + half)
                p.stt(Sb, S, egl, sl(b3, 1), ALU.mult, ALU.add)
                p.stt(S, S, egl, sl(b3, 1), ALU.mult, ALU.add)
                tiv = F(4)
                p.copy(tiv[rows], sl(b2, 3)[rows], eng="scalar")
                eG = V(pre_t[rows, d, 4, sc:sc + 1], ["pre%d_4" % d])
                if first:
                    p.stt(oacc[rows], sl(b2, 2)[rows], eG, tiv[rows], ALU.mult, ALU.add)
                else:
                    p.stt(F(5)[rows], sl(b2, 2)[rows], eG, tiv[rows], ALU.mult, ALU.add)
                    p.tt(oacc[rows], oacc[rows], F(5)[rows], ALU.add, eng="gpsimd")
        for step in range(nsc_run if mode > 0 else 0):
            do_sc(0, step, step)
            do_sc(1, NSC - 1 - step, step)
        for c4 in range(4):
            p.dma(dq(), V(o_d.rearrange("(n p) d -> p n d", p=128)[:, c4 * 16:(c4 + 1) * 16, :], []),
                  V(oacc_t[:, c4 * 16:(c4 + 1) * 16, :], ["oacc%d" % i for i in range(c4 * 16, (c4 + 1) * 16)]))
        p.finish("sync")
        p.emit(block)
    return nc


def prep_C(projT, gqkvn_full, P, l):
    cst = gdn_consts()
    ins = []
    for core in range(8):
        b = core // 4; h = core % 4
        tk = slice(b * 8192, (b + 1) * 8192)
        qT = gqkvn_full[h * 128:(h + 1) * 128, tk]
        kT = gqkvn_full[512 + h * 128:512 + (h + 1) * 128, tk]
        vT = gqkvn_full[1024 + h * 128:1024 + (h + 1) * 128, tk]
        def tokmaj(aT):
            return np.ascontiguousarray(aT.T.reshape(NSC, 128, 128).transpose(1, 0, 2))
        abT = np.stack([projT[3584 + h, tk], projT[3588 + h, tk], projT[3592 + h, tk], projT[3596 + h, tk]], -1)
        ab = np.ascontiguousarray(abT.reshape(NSC, 128, 4).transpose(1, 0, 2))
        par = np.zeros((128, 4), np.float32)
        par[:, 0] = P["gdn_a_log"][l][0, h]; par[:, 1] = P["gdn_a_log"][l][1, h]
        par[:, 2] = P["gdn_dt_bias"][l][0, h]; par[:, 3] = P["gdn_dt_bias"][l][1, h]
        ins.append({"qnT": np.ascontiguousarray(qT), "knT": np.ascontiguousarray(kT), "kn": tokmaj(kT),
                    "v": tokmaj(vT), "ab": ab, "par": par, "cst": cst})
    return ins


def build_D(last, n_exp=32):
    nc = bass.Bass("TRN2", target_bir_lowering=False)
    dt = nc.dram_tensor
    xT_d = dt("xT", [1024, NTOK], F32, kind="ExternalInput").ap()
    yc_d = dt("ycT", [256, NTOK], F32, kind="ExternalInput").ap()
    yn_d = dt("ynT", [256, NTOK], F32, kind="ExternalInput").ap()
    oT_d = dt("oT", [512, NTOK], F32, kind="ExternalInput").ap()
    zT_d = dt("zT", [512, NTOK], F32, kind="ExternalInput").ap()
    sm_d = dt("sm", [128, 64], F32, kind="ExternalInput").ap()
    wada_d = dt("wada", [1024, 4096], F32, kind="ExternalInput").ap()
    wout_d = dt("wout", [1024, 1024], F32, kind="ExternalInput").ap()
    wr_d = dt("wr", [1024, 36], F32, kind="ExternalInput").ap()
    rb_d = dt("rb", [128, 36], F32, kind="ExternalInput").ap()
    w1_d = dt("w1", [32, 1024, 512], F32, kind="ExternalInput").ap()
    w3_d = dt("w3", [32, 1024, 512], F32, kind="ExternalInput").ap()
    w2_d = dt("w2", [32, 512, 1024], F32, kind="ExternalInput").ap()
    sel_d = dt("sel", [32, 32, 128], F32, kind="ExternalInput").ap()
    idn_d = dt("idn", [128, 128], F32, kind="ExternalInput").ap()
    out_d = dt("outT", [1024, NTOK], F32, kind="ExternalOutput").ap()
    NB = NTOK // TB
    from contextlib import ExitStack
    with ExitStack() as es:
        def sb(name, shape, dtype):
            return es.enter_context(nc.sbuf_tensor(name, shape, dtype))
        xT_t = sb("xTs", [128, 8, NTOK], F32)
        yh_t = sb("yh", [128, 8, NTOK], BF16)
        wbuf_t = sb("wbuf", [128, 12288], BF16)
        stg_t = sb("stg", [128, 2, 4096], F32)
        hg_t = sb("hg", [128, 2, 4, TB], BF16)
        s1_t = sb("s1", [128, 2, TB], F32)
        gT_t = sb("gT", [32, NTOK], F32)
        sel_t = sb("sels", [32, 32, 128], F32)
        idn_t = sb("idns", [128, 128], F32)
        ones_t = sb("ones", [128, 128], F32)
        sq_t = sb("sq", [128, 2, TB], F32)
        tmp_t = sb("tmp", [128, 2, TB], F32)
        rs_t = sb("rs", [128, TB], F32)
        rstd_t = sb("rstd", [128, TB], F32)
        small = sb("small", [128, 128], F32)
        wr_t = sb("wrs", [128, 8, 36], F32)
        rb_t = sb("rbs", [128, 36], F32)
        rt_t = sb("rt", [128, 2, 160], F32)
        ps = es.enter_context(nc.psum_tensor("ps", [128, 8, 512], F32))
        block = es.enter_context(nc.Block())
        p = Prog(nc)
        dq_i = [0]

        def dq():
            dq_i[0] += 1
            return "sync" if dq_i[0] % 2 else "gpsimd"
        stg_i = [0]

        def stage():
            i = stg_i[0] % 2
            stg_i[0] += 1
            return V(stg_t[:, i], ["stg%d" % i])

        def PS(b):
            return V(ps[:, b, :], ["ps%d" % b])
        ones = V(ones_t, ["ones"]); idn = V(idn_t, ["idn"]); sel = V(sel_t, ["sel"])
        p.memset(ones, 1.0)
        sm = V(small[:, 0:64], ["sm"])
        epsv = V(small[:, 120:121], ["epsv"])
        p.memset(epsv, EPS)
        EPSV[0] = epsv
        p.dma("sync", sm, V(sm_d, []))
        p.dma("sync", idn, V(idn_d, []))
        p.dma("gpsimd", sel, V(sel_d, []))
        p.dma("sync", V(wr_t, ["wr"]), V(wr_d.rearrange("(c p) n -> p c n", p=128), []))
        p.dma("sync", V(rb_t, ["rb"]), V(rb_d, []))
        wr = V(wr_t, ["wr"]); rb = V(rb_t, ["rb"])
        cT = sm[:, 0:8]; nfw = sm[:, 8:16]; fw = sm[:, 16:24]; gnw = sm[:, 24:25]; bada = sm[:, 32:64]
        cond = V(small[:, 64:72], ["cond"]); mod_sb = V(small[:, 72:104], ["mod"]); A2 = V(small[:, 104:112], ["A2"])
        xT = V(xT_t, [])

        def xblk(kc, blk):
            return V(xT_t[:, kc, blk * TB:(blk + 1) * TB], ["x%d" % blk])

        def yblk(kc, blk):
            return V(yh_t[:, kc, blk * TB:(blk + 1) * TB], ["yh%d" % blk])
        allx = ["x%d" % b for b in range(NB)]; ally = ["yh%d" % b for b in range(NB)]
        for kc in range(8):
            p.dma(dq(), V(xT_t[:, kc, :], allx), V(xT_d[kc * 128:(kc + 1) * 128, :], []))
        wst = [V(stg_t[:, i].rearrange("p (c n) -> p c n", c=8), ["stg%d" % i]) for i in range(2)]
        emit_mod(p, nc, None, cT, wada_d, bada, 32, V(ps[:, 0, 0:32], ["ps0"]), wst, cond, mod_sb)
        gate1 = mod_sb[:, 0:8]; B2 = mod_sb[:, 8:16]; gate2 = mod_sb[:, 24:32]
        p.stt(A2, mod_sb[:, 16:24], 1.0, nfw, ALU.add, ALU.mult)
        for (src, k0) in ((yc_d, 0), (yn_d, 2)):
            st = stage()
            p.dma(dq(), st, V(src.rearrange("(c p) n -> p c n", p=128), []))
            p.copy(V(yh_t[:, k0:k0 + 2, :], ally), V(st.ap.rearrange("p (c n) -> p c n", c=2), st.keys), eng="gpsimd")
        for h in range(4):
            st = stage()
            p.dma(dq(), st[:, 0:NTOK], V(oT_d[h * 128:(h + 1) * 128, :], []))
            p.dma(dq(), st[:, NTOK:2 * NTOK], V(zT_d[h * 128:(h + 1) * 128, :], []))
            for blk in range(NB):
                o = st[:, blk * TB:(blk + 1) * TB]; z = st[:, NTOK + blk * TB:NTOK + (blk + 1) * TB]
                sq = V(sq_t[:, blk % 2], ["sq%d" % (blk % 2)]); tmp = V(tmp_t[:, blk % 2], ["tmp%d" % (blk % 2)])
                rs = V(rs_t, ["rs"])
                p.act(sq, o, AF.Square)
                p.mm(PS(1), ones, sq)
                p.act(rs, PS(1), AF.Sqrt, scale=1.0 / 128.0, bias=epsv)
                p.recip(rs, rs)
                p.stt(tmp, o, gnw, rs, ALU.mult, ALU.mult)
                p.act(sq, z, AF.Silu)
                p.tt(yblk(4 + h, blk), tmp, sq, ALU.mult)
        woutb = V(wbuf_t[:, 0:8192].rearrange("p (c n) -> p c n", c=8), ["w1b", "w3b"])
        for half in range(2):
            st = stage()
            p.dma(dq(), st, V(wout_d[:, half * 512:(half + 1) * 512].rearrange("(c p) n -> p c n", p=128), []))
            p.copy(woutb[:, :, half * 512:(half + 1) * 512], V(st.ap.rearrange("p (c n) -> p c n", c=8), st.keys), eng="gpsimd")
        gT = V(gT_t, ["gT"])
        for blk in range(NB):
            for dtl in range(8):
                bank = 1 + dtl % 4
                for kc in range(8):
                    p.mm(PS(bank), woutb[:, kc, dtl * 128:(dtl + 1) * 128], yblk(kc, blk), start=(kc == 0), stop=(kc == 7))
                p.stt(xblk(dtl, blk), PS(bank), gate1[:, dtl:dtl + 1], xblk(dtl, blk), ALU.mult, ALU.add)
            sq = [V(sq_t[:, i], ["sq%d" % i]) for i in range(2)]
            tmp = [V(tmp_t[:, i], ["tmp%d" % i]) for i in range(2)]
            st = stage()
            hF = V(st.ap.rearrange("p (c n) -> p c n", c=8), st.keys)
            xv = V(xT_t, ["x%d" % blk])
            hT = V(yh_t[:, :, blk * TB:(blk + 1) * TB], ["yh%d" % blk])
            emit_hmix_block(p, xv, blk, ones, sq, PS(0), V(rs_t, ["rs"]), V(rstd_t, ["rstd"]), tmp, A2, B2, hT, hF=hF)
            for tt_ in range(4):
                L = V(ps[:, 5 + tt_ % 2, 0:36], ["ps%d" % (5 + tt_ % 2)])
                for kc in range(8):
                    p.mm(L, hF[:, kc, tt_ * 128:(tt_ + 1) * 128], wr[:, kc, :], start=(kc == 0), stop=(kc == 7))
                R = V(rt_t[:, tt_ % 2], ["rt%d" % (tt_ % 2)])
                Lb = R[:, 0:36]; lg = R[:, 0:4]; le = R[:, 4:36]
                m = R[:, 36:37]; negm = R[:, 37:38]; ohg = R[:, 40:44]; e4 = R[:, 44:48]; ssum = R[:, 38:39]
                pgt = R[:, 39:40]; pen = R[:, 48:52]; lem = R[:, 52:84]; m1 = R[:, 84:85]; oh1 = R[:, 88:120]
                lem2 = R[:, 120:152]; m2 = R[:, 85:86]; dd = R[:, 86:87]; ed = R[:, 87:88]
                c1 = R[:, 152:153]; c2 = R[:, 153:154]; den = R[:, 154:155]
                p.tt(Lb, L, rb, ALU.add)
                p.reduce(m, lg, ALU.max)
                p.ts(ohg, lg, m, None, ALU.is_equal)
                p.ts(negm, m, -1.0, None, ALU.mult)
                p.act(e4, lg, AF.Exp, bias=negm)
                p.reduce(ssum, e4, ALU.add)
                p.recip(pgt, ssum)
                p.ts(pen, ohg, 1.0, 1e30, ALU.subtract, ALU.mult)
                for g in range(4):
                    p.ts(lem[:, g * 8:(g + 1) * 8], le[:, g * 8:(g + 1) * 8], pen[:, g:g + 1], None, ALU.add)
                p.reduce(m1, lem, ALU.max)
                p.ts(oh1, lem, m1, None, ALU.is_equal)
                p.stt(lem2, oh1, -1e30, lem, ALU.mult, ALU.add)
                p.reduce(m2, lem2, ALU.max)
                p.tt(dd, m2, m1, ALU.subtract)
                p.act(ed, dd, AF.Exp)
                p.ts(den, ed, 1.0, None, ALU.add)
                p.recip(den, den)
                p.tt(c1, den, pgt, ALU.mult)
                p.tt(c2, c1, ed, ALU.mult)
                p.ts(lem2, lem2, m2, None, ALU.is_equal)
                p.ts(oh1, oh1, c1, None, ALU.mult)
                p.stt(oh1, lem2, c2, oh1, ALU.mult, ALU.add)
                gp = V(ps[0:32, 7, 0:128], ["ps7"])
                p.tr(gp, oh1, idn)
                c0 = blk * TB + tt_ * 128
                p.copy(gT[:, c0:c0 + 128], gp, eng="scalar")
        w1b = V(wbuf_t[:, 0:4096].rearrange("p (c n) -> p c n", c=8), ["w1b"])
        w3b = V(wbuf_t[:, 4096:8192].rearrange("p (c n) -> p c n", c=8), ["w3b"])
        w2b = V(wbuf_t[:, 8192:12288].rearrange("p (c n) -> p c n", c=4), ["w2b"])
        it = 0
        for e in range(n_exp):
            for (dst, src, cc) in ((w1b, w1_d[e], 8), (w3b, w3_d[e], 8), (w2b, w2_d[e], 4)):
                st = stage()
                p.dma(dq(), st, V(src.rearrange("(c p) n -> p c n", p=128), []))
                p.copy(dst, V(st.ap.rearrange("p (c n) -> p c n", c=cc), st.keys), eng="gpsimd")
            for blk in range(NB):
                bsl = slice(blk * TB, (blk + 1) * TB)
                hT = V(yh_t[:, :, bsl], ["yh%d" % blk])
                gb = PS(4)
                p.mm(gb, sel[:, e, :], gT[:, bsl])
                hg = V(hg_t[:, it % 2], ["hg%d" % (it % 2)])
                for ht in range(4):
                    h1 = PS(ht % 2); h3 = PS(2 + ht % 2)
                    for kc in range(8):
                        p.mm(h1, w1b[:, kc, ht * 128:(ht + 1) * 128], hT[:, kc, :], start=(kc == 0), stop=(kc == 7))
                    for kc in range(8):
                        p.mm(h3, w3b[:, kc, ht * 128:(ht + 1) * 128], hT[:, kc, :], start=(kc == 0), stop=(kc == 7))
                    s1 = V(s1_t[:, ht % 2], ["s1%d" % (ht % 2)])
                    p.act(s1, h1, AF.Silu)
                    p.tt(s1, s1, gb, ALU.mult)
                    p.tt(hg[:, ht, :], h3, s1, ALU.mult)
                for dtl in range(8):
                    yb = PS(5 + dtl % 3)
                    for ht in range(4):
                        p.mm(yb, w2b[:, ht, dtl * 128:(dtl + 1) * 128], hg[:, ht, :], start=(ht == 0), stop=(ht == 3))
                    p.stt(xblk(dtl, blk), yb, gate2[:, dtl:dtl + 1], xblk(dtl, blk), ALU.mult, ALU.add)
                it += 1
        for blk in range(NB):
            bsl = slice(blk * TB, (blk + 1) * TB)
            if last:
                xv = V(xT_t, ["x%d" % blk])
                for kc in range(8):
                    s = V(sq_t[:, kc % 2], ["sq%d" % (kc % 2)])
                    p.act(s, xv[:, kc, bsl], AF.Square)
                    p.mm(PS(0), ones, s, start=(kc == 0), stop=(kc == 7))
                rs = V(rs_t, ["rs"]); rstd = V(rstd_t, ["rstd"])
                p.act(rs, PS(0), AF.Sqrt, scale=1.0 / 1024.0, bias=epsv)
                p.recip(rstd, rs)
                for kc in range(8):
                    p.stt(xblk(kc, blk), xblk(kc, blk), fw[:, kc:kc + 1], rstd, ALU.mult, ALU.mult)
            for kc in range(8):
                p.dma(dq(), V(out_d[kc * 128:(kc + 1) * 128, bsl], []), xblk(kc, blk))
        p.finish("sync")
        p.emit(block)
    return nc


def _lay_pc(v, n):
    return np.ascontiguousarray(np.asarray(v).reshape(n, 128).T)


def prep_D(xT_full, ycT_full, ynaT_full, oT_full, projT, P, l):
    sel = np.zeros((32, 32, 128), np.float32)
    for e in range(32):
        sel[e, e, :] = 1.0
    idn = np.eye(128, dtype=np.float32)
    wr = np.ascontiguousarray(np.concatenate([P["router_group_w"][l], P["router_expert_w"][l]], 1))
    rbv = np.concatenate([P["router_group_b"][l], P["router_expert_b"][l]])
    rb = np.ascontiguousarray(np.broadcast_to(rbv[None, :], (128, 36))).astype(np.float32)
    wada = np.ascontiguousarray(P["w_ada"][l][:, 2048:6144])
    ins = []
    for core in range(8):
        b = core // 4
        tk = slice(core * NTOK, (core + 1) * NTOK)
        sm = np.zeros((128, 64), np.float32)
        sm[:, 0:8] = _lay_pc(P["c"][b], 8)
        sm[:, 8:16] = _lay_pc(P["norm_ffn_w"][l], 8)
        sm[:, 16:24] = _lay_pc(P["final_norm_w"], 8)
        sm[:, 24] = P["gdn_norm_w"][l]
        sm[:, 32:64] = _lay_pc(P["b_ada"][l][2048:6144], 32)
        ins.append({
            "xT": np.ascontiguousarray(xT_full[:, tk]), "ycT": np.ascontiguousarray(ycT_full[:, tk]),
            "ynT": np.ascontiguousarray(ynaT_full[:, tk]), "oT": np.ascontiguousarray(oT_full[:, tk]),
            "zT": np.ascontiguousarray(projT[3072:3584, tk]), "sm": sm, "wada": wada,
            "wout": P["w_out"][l], "wr": wr, "rb": rb,
            "w1": P["expert_w1"][l], "w3": P["expert_w3"][l], "w2": P["expert_w2"][l], "sel": sel, "idn": idn,
        })
    return ins


def prep_A(xT_full, P, l):
    ins = []
    wada = np.ascontiguousarray(P["w_ada"][l][:, 0:2048])
    for core in range(8):
        b = core // 4
        ins.append({
            "xT": np.ascontiguousarray(xT_full[:, core * NTOK:(core + 1) * NTOK]),
            "cT": _lay_pc(P["c"][b], 8),
            "wada": wada,
            "bada": _lay_pc(P["b_ada"][l][0:2048], 16),
            "nw": _lay_pc(P["norm_mix_w"][l], 8),
            "win": P["w_in"][l],
        })
    return ins


TSEQ = 8192
NSH = 4


def _phase(nc):
    from contextlib import ExitStack
    return ExitStack()


def phase_A(nc, p, xsrc, W, S, l):
    with _phase(nc) as es:
        def sb(name, shape, dtype):
            return es.enter_context(nc.sbuf_tensor("A%d_%s" % (l, name), shape, dtype))
        xT = V(sb("xTs", [128, 8, NTOK], F32), ["xT"])
        wst_t = sb("wst", [128, 2, 8, 512], F32)
        wst = [V(wst_t[:, i], ["wst%d" % i]) for i in range(2)]
        winb = sb("winb", [128, 8, 3600], BF16)
        hT_t = sb("hT", [128, 2, 8, TB], BF16)
        sq_t = sb("sq", [128, 2, TB], F32)
        tmp_t = sb("tmp", [128, 2, TB], F32)
        rs = V(sb("rs", [128, TB], F32), ["rs"])
        rstd_b = V(sb("rstd", [128, TB], F32), ["rstd"])
        stage_t = sb("stage", [128, 4, TB], F32)
        tk_t = sb("tk", [128, 2, 272], F32)
        small = sb("small", [128, 64], F32)
        ones = V(sb("ones", [128, 128], F32), ["ones"])
        epsv = V(sb("epsv", [128, 1], F32), ["epsv"])
        EPSV[0] = epsv
        ps = es.enter_context(nc.psum_tensor("A%d_ps" % l, [128, 8, 512], F32))
        block = es.enter_context(nc.Block())
        cT = V(small[:, 0:8], ["cT"]); cond = V(small[:, 8:16], ["cond"])
        bada = V(small[:, 16:32], ["bada"]); mod_sb = V(small[:, 32:48], ["mod"])
        nw = V(small[:, 48:56], ["nw"]); A1 = V(small[:, 56:64], ["A1"])
        modps = V(ps[:, 0, 0:16], ["ps0"])
        ss = V(ps[:, 1, :], ["ps1"])
        p.memset(ones, 1.0)
        p.memset(epsv, EPS)
        p.dma("sync", cT, V(W["cT"], []))
        p.dma("sync", bada, V(W["badaA%d" % l], []))
        p.dma("sync", nw, V(W["nw%d" % l], []))
        emit_mod(p, nc, None, cT, W["wada%d" % l][:, 0:2048], bada, 16, modps, wst, cond, mod_sb)
        B1 = mod_sb[:, 0:8]
        p.stt(A1, mod_sb[:, 8:16], 1.0, nw, ALU.add, ALU.mult)
        win_d = W["win%d" % l]
        ci = 0
        for c0 in range(0, 3600, 512):
            cw = min(512, 3600 - c0)
            buf = wst[ci % 2]
            p.dma("sync" if ci % 2 == 0 else "gpsimd", buf[:, :, 0:cw],
                  V(win_d[:, c0:c0 + cw].rearrange("(c p) n -> p c n", p=128), []))
            for kc in range(8):
                dst = V(winb[:, kc, c0:c0 + cw], ["winb%d" % ci])
                p.copy(dst, buf[:, kc, 0:cw], eng=("gpsimd" if kc % 2 else "vector"))
            ci += 1
        nev = 0
        ntk = 0
        for s in range(NSH):
            t0 = s * NTOK
            for kc in range(8):
                p.dma("gpsimd" if kc % 2 else "sync", xT[:, kc, :], V(xsrc[kc * 128:(kc + 1) * 128, t0:t0 + NTOK], []))
            for blk in range(NTOK // TB):
                hT = V(hT_t[:, blk % 2], ["hT%d" % (blk % 2)])
                sq = [V(sq_t[:, i], ["sq%d" % i]) for i in range(2)]
                tmp = [V(tmp_t[:, i], ["tmp%d" % i]) for i in range(2)]
                emit_hmix_block(p, xT, blk, ones, sq, ss, rs, rstd_b, tmp, A1, B1, hT)
                for j in range(29):
                    rows = min(128, 3600 - j * 128)
                    bank = 2 + (nev % 5)
                    pj = V(ps[0:rows, bank, :], ["ps%d" % bank])
                    cj = (j * 128) // 512
                    for kc in range(8):
                        p.mm(pj, V(winb[:, kc, j * 128:j * 128 + rows], ["winb%d" % cj]), hT[:, kc, :],
                             start=(kc == 0), stop=(kc == 7))
                    st = V(stage_t[0:rows, nev % 4, :], ["stage%d" % (nev % 4)])
                    p.copy(st, pj, eng=("vector" if nev % 2 == 0 else "scalar"))
                    p.dma("sync" if nev % 2 == 0 else "gpsimd",
                          V(S["projT"][j * 128:j * 128 + rows, t0 + blk * TB:t0 + (blk + 1) * TB], []), st)
                    nev += 1
                for tt_ in range(4):
                    pt = V(ps[:, 7, 0:272], ["ps7"])
                    tsl = slice(tt_ * 128, (tt_ + 1) * 128)
                    for kc in range(8):
                        p.mm(pt[:, 0:256], hT[:, kc, tsl], V(winb[:, kc, 1280:1536], ["winb2"]),
                             start=(kc == 0), stop=(kc == 7))
                    for kc in range(8):
                        p.mm(pt[:, 256:272], hT[:, kc, tsl], V(winb[:, kc, 3584:3600], ["winb7"]),
                             start=(kc == 0), stop=(kc == 7))
                    tk = V(tk_t[:, ntk % 2], ["tk%d" % (ntk % 2)])
                    ntk += 1
                    p.copy(tk, pt, eng="vector")
                    r0 = t0 + blk * TB + tt_ * 128
                    p.dma("sync", V(S["nvtok"][r0:r0 + 128, :], []), tk[:, 0:256])
                    p.dma("gpsimd", V(S["abtok"][r0:r0 + 128, :], []), tk[:, 256:272])
        p.finish("sync")
        p.emit(block)


def phase_B(nc, p, W, S, l):
    with _phase(nc) as es:
        def sb(name, shape, dtype):
            return es.enter_context(nc.sbuf_tensor("B%d_%s" % (l, name), shape, dtype))
        NST = 3
        stg_t = sb("stg", [128, NST, 4096], F32)
        u_t = sb("u", [128, NTOK + 2], F32)
        acc_t = sb("acc", [128, NTOK], F32)
        sil_t = sb("sil", [128, 2, NTOK], F32)
        sq_t = sb("sq", [128, 2, TB], F32)
        rs_t = sb("rs", [128, 2, TB], F32)
        ones_t = sb("ones", [128, 128], F32)
        idn_t = sb("idn", [128, 128], F32)
        wsm = sb("wsm", [128, 64], F32)
        qb_t = sb("qb", [128, 2, NTOK], BF16)
        kb_t = sb("kb", [128, 2, NTOK], BF16)
        ksp_t = sb("ksp", [128, 2, NSPEC * 512], BF16)
        ve_t = sb("ve", [128, 16, 4, 65], BF16)
        vo_t = sb("vo", [128, 16, 4, 65], BF16)
        vs_t = sb("vs", [128, NSPEC * 4, 4, 65], BF16)
        bias_t = sb("biass", [128, 9, 1024], F32)
        maskf_t = sb("maskfs", [128, 1024], F32)
        sc_t = sb("sc", [128, 2, 512], F32)
        pT_t = sb("pT", [128, 2, 512], BF16)
        pos_t = sb("pos", [64, 2, 260], F32)
        rden_t = sb("rden", [64, 2, 4], F32)
        yst_t = sb("yst", [64, 2, 256], F32)
        ytr_t = sb("ytr", [128, 2, 2, 64], F32)
        ps = es.enter_context(nc.psum_tensor("B%d_ps" % l, [128, 8, 512], F32))
        block = es.enter_context(nc.Block())
        stg_i = [0]

        def stage():
            i = stg_i[0] % NST
            stg_i[0] += 1
            return V(stg_t[:, i], ["stg%d" % i])
        dq_i = [0]

        def dq():
            dq_i[0] += 1
            return "sync" if dq_i[0] % 2 else "gpsimd"
        ones = V(ones_t, ["ones"]); idn = V(idn_t, ["idn"])
        p.memset(ones, 1.0)
        p.dma("sync", idn, V(W["idn"], []))
        cw = V(wsm[:, 0:6], ["cw"])
        gw = V(wsm[:, 6:42], ["gw"])
        epsv = V(wsm[:, 42:43], ["epsv"])
        p.memset(epsv, EPS)
        p.dma("sync", cw, V(W["cw%d" % l].rearrange("p a b -> p (a b)"), []))
        p.dma("sync", gw, V(W["gw%d" % l].rearrange("p a b -> p (a b)"), []))
        u = V(u_t, ["u"]); acc = V(acc_t, ["acc"])
        maskf = V(maskf_t, ["maskf"])
        p.dma("sync", maskf, V(W["maskf"], []))
        projT = S["projT"]; nvtok = S["nvtok"]

        def conv3(src, wv, base):
            p.ts(acc, src[:, 0:NTOK], wv[:, base:base + 1], None, ALU.mult)
            p.stt(acc, src[:, 1:NTOK + 1], wv[:, base + 1:base + 2], acc, ALU.mult, ALU.add)
            p.stt(acc, src[:, 2:NTOK + 2], wv[:, base + 2:base + 3], acc, ALU.mult, ALU.add)

        def load_halo(st, row0, t0):
            lo = t0 - 1; hi = t0 + NTOK + 1
            a = 0; b = NTOK + 2
            if lo < 0:
                p.memset(st[:, 0:1], 0.0, eng="gpsimd")
                lo = 0; a = 1
            if hi > TSEQ:
                p.memset(st[:, NTOK + 1:NTOK + 2], 0.0, eng="gpsimd")
                hi = TSEQ; b = NTOK + 1
            p.dma(dq(), st[:, a:b], V(projT[row0:row0 + 128, lo:hi], []))
        ssb = 0
        for s in range(NSH):
            t0 = s * NTOK
            tsl = slice(t0, t0 + NTOK)
            for ct in range(2):
                bufs = []
                for g in range(3):
                    st = stage()
                    load_halo(st, g * 256 + ct * 128, t0)
                    bufs.append(st)
                cb, cc, cx = bufs
                p.tt(u, cc[:, 0:NTOK + 2], cx[:, 0:NTOK + 2], ALU.mult)
                conv3(u, cw, ct * 3)
                yo = V(sil_t[:, ct], ["sil%d" % ct])
                p.tt(yo, acc, cb[:, 1:NTOK + 1], ALU.mult)
                p.dma(dq(), V(S["ycT"][ct * 128:(ct + 1) * 128, tsl], []), yo)
            for ct in range(12):
                st = stage()
                load_halo(st, 1536 + ct * 128, t0)
                conv3(st, gw, ct * 3)
                so = V(sil_t[:, ct % 2], ["sil%d" % (ct % 2)])
                p.act(so, acc, AF.Silu)
                if ct < 8:
                    qscale = (128.0 ** -0.5) if ct < 4 else 1.0
                    for blk in range(NTOK // TB):
                        sl = slice(blk * TB, (blk + 1) * TB)
                        sq = V(sq_t[:, ssb % 2], ["sq%d" % (ssb % 2)])
                        rs = V(rs_t[:, ssb % 2], ["rs%d" % (ssb % 2)])
                        bank = ssb % 2
                        ss = V(ps[:, bank, :], ["ps%d" % bank])
                        ssb += 1
                        p.act(sq, so[:, sl], AF.Square)
                        p.mm(ss, ones, sq)
                        p.act(rs, ss, AF.Sqrt, bias=epsv)
                        p.recip(rs, rs)
                        p.stt(so[:, sl], so[:, sl], qscale, rs, ALU.mult, ALU.mult)
                p.dma(dq(), V(S["gqkvn"][ct * 128:(ct + 1) * 128, tsl], []), so)
            qb = V(qb_t, ["qb"]); kb = V(kb_t, ["kb"]); ksp = V(ksp_t, ["ksp"])
            for i in range(2):
                for (dst, r0) in ((qb, 768), (kb, 1024)):
                    st = stage()
                    p.dma(dq(), st[:, 0:NTOK], V(projT[r0 + i * 128:r0 + (i + 1) * 128, tsl], []))
                    p.copy(dst[:, i, :], st[:, 0:NTOK], eng="gpsimd")
            row0 = s * 32
            spec = []
            for si, r in enumerate(SPEC_ROWS):
                R = row0 + r
                rs_ = min(max(R - 4, 0), 120)
                spec.append(rs_)
            for i in range(2):
                st = stage()
                for si, rs_ in enumerate(spec):
                    p.dma(dq(), st[:, si * 512:(si + 1) * 512], V(projT[1024 + i * 128:1024 + (i + 1) * 128, rs_ * 64:rs_ * 64 + 512], []))
                p.copy(ksp[:, i, :], st, eng="gpsimd")
            ve = V(ve_t, ["ve"]); vo = V(vo_t, ["vo"]); vs = V(vs_t, ["vs"])
            for dst in (ve, vo, vs):
                p.memset(dst[:, :, :, 64:65], 1.0, eng="gpsimd")
            st = stage()
            p.dma(dq(), st, V(nvtok[t0:t0 + NTOK, :].rearrange("(n p) c -> p n c", p=128), []))
            p.copy(ve[:, :, :, 0:64], V(st.ap.rearrange("p (a h d) -> p a h d", a=16, h=4), st.keys), eng="vector")
            st = stage()
            p.dma(dq(), st[:, 0:15 * 256], V(nvtok[t0 + 64:t0 + 64 + 15 * 128, :].rearrange("(n p) c -> p n c", p=128), []))
            p.copy(vo[:, 0:15, :, 0:64], V(st.ap[:, 0:15 * 256].rearrange("p (a h d) -> p a h d", a=15, h=4), st.keys), eng="vector")
            for half in range(2):
                st = stage()
                for q4 in range(4):
                    si = half * 4 + q4
                    rs_ = spec[si]
                    p.dma(dq(), st[:, q4 * 1024:(q4 + 1) * 1024],
                          V(nvtok[rs_ * 64:rs_ * 64 + 512, :].rearrange("(n p) c -> p n c", p=128), []))
                p.copy(vs[:, half * 16:(half + 1) * 16, :, 0:64],
                       V(st.ap.rearrange("p (a h d) -> p a h d", a=16, h=4), st.keys), eng="vector")
            bias = V(bias_t, ["bias"])
            for i in range(9):
                p.dma(dq(), bias[:, i, :], V(W["rpbg%d" % l][s, :, i, :], []))
            for i in range(9):
                p.stt(bias[:, i, :], bias[:, i, :], 8.0, maskf, ALU.mult, ALU.add)
            for r in range(32):
                par = r % 2
                if r in SPEC_ROWS:
                    sidx = SPEC_ROWS.index(r)
                    bi = 1 + sidx

                    def ktile(tile, off, t, sidx=sidx):
                        return ksp[off:off + 64, tile, sidx * 512 + t * 128:sidx * 512 + (t + 1) * 128]

                    def vtile(t, h, sidx=sidx):
                        return vs[:, sidx * 4 + t, h, :]
                else:
                    bi = 0
                    lr = r - 4

                    def ktile(tile, off, t, lr=lr):
                        return kb[off:off + 64, tile, (lr + 2 * t) * 64:(lr + 2 * t + 2) * 64]
                    if lr % 2 == 0:
                        def vtile(t, h, lr=lr):
                            return ve[:, lr // 2 + t, h, :]
                    else:
                        def vtile(t, h, lr=lr):
                            return vo[:, (lr - 1) // 2 + t, h, :]
                pob = 6 + par
                po = V(ps[0:64, pob, 0:260], ["ps%d" % pob])
                for hl in range(2):
                    off = 64 * hl
                    bank = 2 + 2 * par + hl
                    psc = V(ps[:, bank, :], ["ps%d" % bank])
                    for j in range(2):
                        for t in range(4):
                            p.mm(psc[:, (j * 4 + t) * 64:(j * 4 + t + 1) * 64], ktile(j, off, t),
                                 qb[off:off + 64, j, r * 64:(r + 1) * 64])
                    sc = V(sc_t[:, hl], ["sc%d" % hl])
                    bview = V(bias.ap[:, bi, :].rearrange("p (h x) -> p h x", h=4)[:, hl::2, :], bias.keys)
                    p.tt(V(sc.ap.rearrange("p (h x) -> p h x", h=2), sc.keys),
                         V(psc.ap.rearrange("p (h x) -> p h x", h=2), psc.keys), bview, ALU.add)
                    pT = V(pT_t[:, hl], ["pT%d" % hl])
                    p.act(pT, sc, AF.Exp, scale=0.125)
                    for j in range(2):
                        h = 2 * j + hl
                        for t in range(4):
                            p.mm(po[:, h * 65:(h + 1) * 65], pT[:, (j * 4 + t) * 64:(j * 4 + t + 1) * 64], vtile(t, h),
                                 start=(t == 0), stop=(t == 3))
                pos = V(pos_t[:, par], ["pos%d" % par])
                p.copy(pos, po, eng="scalar")
                rden = V(rden_t[:, par], ["rden%d" % par])
                posv = V(pos.ap.rearrange("p (h d) -> p h d", h=4), pos.keys)
                p.recip(rden, V(posv.ap[:, :, 64:65].rearrange("p h o -> p (h o)"), pos.keys))
                yst = V(yst_t[:, par], ["yst%d" % par])
                for h in range(4):
                    p.ts(yst[:, h * 64:(h + 1) * 64], posv[:, h, 0:64], rden[:, h:h + 1], None, ALU.mult, eng="gpsimd")
                ytr = V(ytr_t[:, par], ["ytr%d" % par])
                for j in range(2):
                    pt = V(ps[:, 0 + j, 0:64], ["ps%d" % j])
                    p.tr(pt, yst[:, j * 128:(j + 1) * 128], idn[0:64, 0:64])
                    p.copy(ytr[:, j, :], pt, eng=("vector" if j else "scalar"))
                    p.dma(dq(), V(S["ynaT"][j * 128:(j + 1) * 128, t0 + r * 64:t0 + (r + 1) * 64], []), ytr[:, j, :])
        p.finish("sync")
        p.emit(block)


def phase_C(nc, p, W, S, l):
    T = TSEQ
    with _phase(nc) as es:
        def sb(name, shape, dtype):
            return es.enter_context(nc.sbuf_tensor("C%d_%s" % (l, name), shape, dtype))
        cst_t = sb("cst_s", [128, 11, 128], F32)
        cstb_t = sb("cstb", [128, 128], BF16)
        stg_t = sb("stg", [128, 2, 2048], F32)
        qnT_t = sb("qnTb", [128, T], BF16)
        knT_t = sb("knTb", [128, T], BF16)
        vT_t = sb("vTb", [128, T], BF16)
        kn_t = sb("knb", [128, NSC, 128], BF16)
        v_t = sb("vb", [128, NSC, 128], BF16)
        oacc_t = sb("oacc", [128, NSC, 128], F32)
        ab_t = sb("abs", [128, NSC, 16], F32)
        par_t = sb("pars", [128, 8], F32)
        NPS = 12
        pre_t = sb("pre", [128, 2, NPS, NSC], F32)
        S_t = sb("S", [128, 2, 128], F32)
        Sb_t = sb("Sb", [128, 2, 128], BF16)
        vnew_t = sb("vnew", [128, 2, 128], BF16)
        NF = 6; NB = 16
        wf_t = sb("wf", [128, 2, 2, NF, 128], F32)
        wb_t = sb("wb", [128, 2, 2, NB, 128], BF16)
        ps = es.enter_context(nc.psum_tensor("C%d_ps" % l, [128, 8, 512], F32))
        block = es.enter_context(nc.Block())
        cst = V(cst_t, ["cst"])

        def C(i):
            return cst[:, i, :]
        identb = V(cstb_t, ["cstb"])
        dq_i = [0]

        def dq():
            dq_i[0] += 1
            return "sync" if dq_i[0] % 2 else "gpsimd"
        p.dma("sync", cst, V(W["cst"], []))
        p.copy(identb, C(C_IDENT))
        ab = V(ab_t, ["ab"]); par = V(par_t, ["par"])
        p.dma("sync", ab, V(S["abtok"].rearrange("(n p) c -> p n c", p=128), []))
        qnT = V(qnT_t, ["qnT"]); knT = V(knT_t, ["knT"]); vT = V(vT_t, ["vT"])
        kn = V(kn_t, ["kn"]); vv = V(v_t, ["vv"])
        pre = V(pre_t, ["pre"])
        mA = V(cst.ap[:, C_SELA, 0:1], cst.keys)
        mB = V(cst.ap[:, C_SELB, 0:1], cst.keys)
        ev = [0]

        def evac(out, in_):
            ev[0] += 1
            p.copy(out, in_, eng=("scalar" if ev[0] % 2 else "vector"))
        si = 0
        for h in range(4):
            p.dma("sync", par[:, 0:4], V(W["par%d" % l][h], []))
            for (dst, r0) in ((qnT, h * 128), (knT, 512 + h * 128), (vT, 1024 + h * 128)):
                for c4 in range(4):
                    st = V(stg_t[:, si % 2], ["stg%d" % (si % 2)])
                    si += 1
                    p.dma(dq(), st, V(S["gqkvn"][r0:r0 + 128, c4 * 2048:(c4 + 1) * 2048], []))
                    p.copy(dst[:, c4 * 2048:(c4 + 1) * 2048], st, eng=("gpsimd" if c4 % 2 else "vector"))
            for sc in range(NSC):
                tok = slice(sc * 128, (sc + 1) * 128)
                bk = V(ps[:, sc % 2, :], ["ps%d" % (sc % 2)])
                p.mm(bk[:, 0:128], knT[:, tok], identb)
                p.mm(bk[:, 128:256], vT[:, tok], identb)
                evac(kn[:, sc, :], bk[:, 0:128])
                evac(vv[:, sc, :], bk[:, 128:256])
            p.act(par[:, 4:6], par[:, 0:2], AF.Exp)
            p.ts(par[:, 6:8], par[:, 4:6], -1.0, None, ALU.mult)
            for d in range(2):
                def S_(i, d=d):
                    return V(pre_t[:, d, i, :], ["pre%d_%d" % (d, i)])
                a_v = V(ab_t[:, :, 4 * d + h], ["ab"]); b_v = V(ab_t[:, :, 8 + 4 * d + h], ["ab"])
                p.ts(S_(11), a_v, par[:, 2 + d:3 + d], None, ALU.add)
                p.act(S_(11), S_(11), AF.Exp)
                p.act(S_(0), S_(11), AF.Ln, bias=1.0)
                p.ts(S_(0), S_(0), par[:, 6 + d:7 + d], None, ALU.mult)
                p.act(S_(1), b_v, AF.Sigmoid)
                tri = C(C_TRIF if d == 0 else C_TRIB)
                pb = V(ps[:, 0, :], ["ps0"])
                p.mm(pb[:, 0:NSC], tri, S_(0))
                p.mm(pb[:, 64:64 + NSC], C(C_BLK), S_(0))
                p.mm(pb[:, 128:128 + NSC], C(C_SELA), S_(0))
                p.mm(pb[:, 192:192 + NSC], C(C_SELB), S_(0))
                p.copy(S_(2), pb[:, 0:NSC])
                p.copy(S_(3), pb[:, 64:64 + NSC])
                p.act(S_(9), pb[:, 128:128 + NSC], AF.Exp)
                p.act(S_(10), pb[:, 192:192 + NSC], AF.Exp)
                p.act(S_(4), S_(2), AF.Exp)
                p.tt(S_(5), S_(1), S_(4), ALU.mult)
                p.tt(S_(11), S_(3), S_(2), ALU.subtract)
                p.act(S_(11), S_(11), AF.Exp)
                p.ts(S_(6), S_(11), mA, None, ALU.mult)
                p.ts(S_(7), S_(11), mB, None, ALU.mult)
                p.ts(S_(8), S_(1), -1.0, None, ALU.mult)
            for d in range(2):
                p.memset(V(S_t[:, d], ["S%d" % d]), 0.0)
                p.memset(V(Sb_t[:, d], ["Sb%d" % d]), 0.0)
                p.memset(V(vnew_t[:, d], ["vn%d" % d]), 0.0)

            def bufs(d, step):
                par_ = step % 2

                def F(i):
                    return V(wf_t[:, d, par_, i], ["wf%d%d_%d" % (d, par_, i)])

                def B(i):
                    return V(wb_t[:, d, par_, i], ["wb%d%d_%d" % (d, par_, i)])
                return F, B

            def sl(bk, i):
                return bk[:, i * 128:(i + 1) * 128]

            def prep_gen(d, sc, step):
                tok = slice(sc * 128, (sc + 1) * 128)
                F, B = bufs(d, step)

                def sc_(i):
                    return V(pre_t[:, d, i, sc:sc + 1], ["pre%d_%d" % (d, i)])
                b0 = V(ps[:, 4 * d + 0, :], ["ps%d" % (4 * d)])
                b1 = V(ps[:, 4 * d + 1, :], ["ps%d" % (4 * d + 1)])
                Gdiag = F(0)
                p.ts(Gdiag, C(C_IDENT), sc_(2), None, ALU.mult); yield
                p.mm(sl(b0, 0), C(C_ONES), Gdiag, start=True, stop=False)
                p.mm(sl(b0, 0), C(C_IDENT), C(C_MSF if d == 0 else C_MSB), start=False, stop=True)
                p.mm(sl(b0, 1), C(C_ONES), Gdiag, start=True, stop=False)
                p.mm(sl(b0, 1), C(C_IDENT), C(C_MIF if d == 0 else C_MIB), start=False, stop=True)
                p.mm(sl(b0, 2), knT[:, tok], knT[:, tok])
                p.mm(sl(b0, 3), knT[:, tok], qnT[:, tok]); yield
                p.ts(F(1), sl(b0, 0), sc_(2), 0.0, ALU.subtract, ALU.max); yield
                p.act(F(1), F(1), AF.Exp, scale=-1.0); yield
                p.ts(F(2), sl(b0, 1), sc_(2), 0.0, ALU.subtract, ALU.min); yield
                p.act(F(2), F(2), AF.Exp); yield
                A = B(0)
                p.stt(A, sl(b0, 2), sc_(8), F(1), ALU.mult, ALU.mult); yield
                p.tt(B(1), sl(b0, 3), F(2), ALU.mult); yield
                p.mm(sl(b1, 0), A, identb); yield
                N = B(2)
                evac(N, sl(b1, 0)); yield
                P = B(3)
                p.tt(P, N, identb, ALU.add); yield
                M, Mt = N, A
                slot = 1
                for j in range(1, 6):
                    Mn = B(4 + (j % 2) * 2); Mtn = B(5 + (j % 2) * 2)
                    s_mt = sl(b1, slot % 4); slot += 1
                    p.mm(s_mt, M, Mt); yield
                    evac(Mtn, s_mt); yield
                    if j < 5:
                        s_m = sl(b1, slot % 4); slot += 1
                        p.mm(s_m, Mt, M); yield
                        evac(Mn, s_m); yield
                    s_p = sl(b1, slot % 4); slot += 1
                    p.mm(s_p, Mtn, P); yield
                    Pn = B(8 + (j % 2))
                    p.tt(Pn, s_p, P, ALU.add); yield
                    P = Pn
                    M, Mt = Mn, Mtn
                Vb = B(10); Kbg = B(11); kdA = B(12); kdB = B(13)
                p.ts(Vb, vv[:, sc, :], sc_(1), None, ALU.mult, eng="gpsimd")
                p.ts(Kbg, kn[:, sc, :], sc_(5), None, ALU.mult, eng="gpsimd")
                p.ts(kdA, kn[:, sc, :], sc_(6), None, ALU.mult, eng="gpsimd")
                p.ts(kdB, kn[:, sc, :], sc_(7), None, ALU.mult, eng="gpsimd"); yield
                p.mm(sl(b1, 2), P, Vb)
                p.mm(sl(b1, 3), Kbg, P); yield
                evac(F(3), sl(b1, 2)); yield
                evac(B(14), sl(b1, 3)); yield

            def scan_gen(d, sc, step):
                F, B = bufs(d, step)

                def sc_(i):
                    return V(pre_t[:, d, i, sc:sc + 1], ["pre%d_%d" % (d, i)])
                b2 = V(ps[:, 4 * d + 2, :], ["ps%d" % (4 * d + 2)])
                b3 = V(ps[:, 4 * d + 3, :], ["ps%d" % (4 * d + 3)])
                u_sb = F(3); wT = B(14); intraT = B(1); kdA = B(12); kdB = B(13)
                St = V(S_t[:, d], ["S%d" % d]); Sb = V(Sb_t[:, d], ["Sb%d" % d]); vn = V(vnew_t[:, d], ["vn%d" % d])
                oacc = V(oacc_t[:, sc, :], ["oacc%d" % sc])
                first = (step < NSC // 2)
                for half in ((0, 1) if d == 0 else (1, 0)):
                    rows = slice(half * 64, (half + 1) * 64)
                    ctok = slice(sc * 128 + half * 64, sc * 128 + (half + 1) * 64)
                    p.mm(sl(b3, 0)[rows], wT[:, rows], Sb); yield
                    p.tt(vn[rows], u_sb[rows], sl(b3, 0)[rows], ALU.subtract); yield
                    p.mm(sl(b3, 1), kdA if half == 0 else kdB, vn)
                    p.mm(sl(b2, 2)[rows], qnT[:, ctok], Sb)
                    p.mm(sl(b2, 3)[rows], intraT[:, rows], vn); yield
                    egl = sc_(9 + half)
                    p.stt(Sb, St, egl, sl(b3, 1), ALU.mult, ALU.add); yield
                    p.stt(St, St, egl, sl(b3, 1), ALU.mult, ALU.add); yield
                    tiv = F(4)
                    p.copy(tiv[rows], sl(b2, 3)[rows], eng="scalar"); yield
                    eG = V(pre_t[rows, d, 4, sc:sc + 1], ["pre%d_4" % d])
                    if first:
                        p.stt(oacc[rows], sl(b2, 2)[rows], eG, tiv[rows], ALU.mult, ALU.add); yield
                    else:
                        p.stt(F(5)[rows], sl(b2, 2)[rows], eG, tiv[rows], ALU.mult, ALU.add); yield
                        p.tt(oacc[rows], oacc[rows], F(5)[rows], ALU.add, eng="gpsimd"); yield

            def drive(gens):
                gens = list(gens)
                while gens:
                    for g in list(gens):
                        try:
                            next(g)
                        except StopIteration:
                            gens.remove(g)
            drive([prep_gen(0, 0, 0), prep_gen(1, NSC - 1, 0)])
            for step in range(NSC):
                gl = [scan_gen(0, step, step), scan_gen(1, NSC - 1 - step, step)]
                if step + 1 < NSC:
                    gl += [prep_gen(0, step + 1, step + 1), prep_gen(1, NSC - 2 - step, step + 1)]
                drive(gl)
            for c4 in range(4):
                st = V(stg_t[:, si % 2], ["stg%d" % (si % 2)])
                si += 1
                for q in range(16):
                    sc = c4 * 16 + q
                    bk = V(ps[:, sc % 2, 0:128], ["ps%d" % (sc % 2)])
                    p.tr(bk, V(oacc_t[:, sc, :], ["oacc%d" % sc]), C(C_IDENT))
                    evac(st[:, q * 128:(q + 1) * 128], bk)
                p.dma(dq(), V(S["oT"][h * 128:(h + 1) * 128, c4 * 2048:(c4 + 1) * 2048], []), st)
        p.finish("sync")
        p.emit(block)


def phase_D(nc, p, xsrc, xdst, W, S, l, last):
    NBk = NTOK // TB
    with _phase(nc) as es:
        def sb(name, shape, dtype):
            return es.enter_context(nc.sbuf_tensor("D%d_%s" % (l, name), shape, dtype))
        xT_t = sb("xTs", [128, 8, NTOK], F32)
        yh_t = sb("yh", [128, 8, NTOK], BF16)
        wbuf_t = sb("wbuf", [128, 12288], BF16)
        stg_t = sb("stg", [128, 2, 4096], F32)
        hg_t = sb("hg", [128, 2, 4, TB], BF16)
        s1_t = sb("s1", [128, 2, TB], F32)
        gT_t = sb("gT", [32, NTOK], F32)
        sel_t = sb("sels", [32, 32, 128], F32)
        idn_t = sb("idns", [128, 128], F32)
        ones_t = sb("ones", [128, 128], F32)
        sq_t = sb("sq", [128, 2, TB], F32)
        tmp_t = sb("tmp", [128, 2, TB], F32)
        rs_t = sb("rs", [128, TB], F32)
        rstd_t = sb("rstd", [128, TB], F32)
        small = sb("small", [128, 128], F32)
        wr_t = sb("wrs", [128, 8, 36], F32)
        rb_t = sb("rbs", [128, 36], F32)
        rt_t = sb("rt", [128, 2, 160], F32)
        ps = es.enter_context(nc.psum_tensor("D%d_ps" % l, [128, 8, 512], F32))
        block = es.enter_context(nc.Block())
        dq_i = [0]

        def dq():
            dq_i[0] += 1
            return "sync" if dq_i[0] % 2 else "gpsimd"
        stg_i = [0]

        def stage():
            i = stg_i[0] % 2
            stg_i[0] += 1
            return V(stg_t[:, i], ["stg%d" % i])

        def PS(b):
            return V(ps[:, b, :], ["ps%d" % b])
        ones = V(ones_t, ["ones"]); idn = V(idn_t, ["idn"]); sel = V(sel_t, ["sel"])
        p.memset(ones, 1.0)
        sm = V(small[:, 0:64], ["sm"])
        epsv = V(small[:, 120:121], ["epsv"])
        p.memset(epsv, EPS)
        EPSV[0] = epsv
        p.dma("sync", sm, V(W["smD%d" % l], []))
        p.dma("sync", idn, V(W["idn"], []))
        p.dma("gpsimd", sel, V(W["sel"], []))
        p.dma("sync", V(wr_t, ["wr"]), V(W["wr%d" % l].rearrange("(c p) n -> p c n", p=128), []))
        p.dma("sync", V(rb_t, ["rb"]), V(W["rb%d" % l], []))
        wr = V(wr_t, ["wr"]); rb = V(rb_t, ["rb"])
        cT = sm[:, 0:8]; nfw = sm[:, 8:16]; fw = sm[:, 16:24]; gnw = sm[:, 24:25]; bada = sm[:, 32:64]
        cond = V(small[:, 64:72], ["cond"]); mod_sb = V(small[:, 72:104], ["mod"]); A2 = V(small[:, 104:112], ["A2"])
        wst = [V(stg_t[:, i].rearrange("p (c n) -> p c n", c=8), ["stg%d" % i]) for i in range(2)]
        emit_mod(p, nc, None, cT, W["wada%d" % l][:, 2048:6144], bada, 32, V(ps[:, 0, 0:32], ["ps0"]), wst, cond, mod_sb)
        gate1 = mod_sb[:, 0:8]; B2 = mod_sb[:, 8:16]; gate2 = mod_sb[:, 24:32]
        p.stt(A2, mod_sb[:, 16:24], 1.0, nfw, ALU.add, ALU.mult)
        w1_d = W["w1_%d" % l]; w3_d = W["w3_%d" % l]; w2_d = W["w2_%d" % l]; wout_d = W["wout%d" % l]
        it = 0
        for s in range(NSH):
            t0 = s * NTOK
            tsl = slice(t0, t0 + NTOK)

            def xblk(kc, blk):
                return V(xT_t[:, kc, blk * TB:(blk + 1) * TB], ["x%d" % blk])

            def yblk(kc, blk):
                return V(yh_t[:, kc, blk * TB:(blk + 1) * TB], ["yh%d" % blk])
            allx = ["x%d" % b for b in range(NBk)]; ally = ["yh%d" % b for b in range(NBk)]
            for kc in range(8):
                p.dma(dq(), V(xT_t[:, kc, :], allx), V(xsrc[kc * 128:(kc + 1) * 128, tsl], []))
            for (src, k0) in ((S["ycT"], 0), (S["ynaT"], 2)):
                st = stage()
                p.dma(dq(), st, V(src[:, tsl].rearrange("(c p) n -> p c n", p=128), []))
                p.copy(V(yh_t[:, k0:k0 + 2, :], ally), V(st.ap.rearrange("p (c n) -> p c n", c=2), st.keys), eng="gpsimd")
            for h in range(4):
                st = stage()
                p.dma(dq(), st[:, 0:NTOK], V(S["oT"][h * 128:(h + 1) * 128, tsl], []))
                p.dma(dq(), st[:, NTOK:2 * NTOK], V(S["projT"][3072 + h * 128:3072 + (h + 1) * 128, tsl], []))
                for blk in range(NBk):
                    o = st[:, blk * TB:(blk + 1) * TB]; z = st[:, NTOK + blk * TB:NTOK + (blk + 1) * TB]
                    sq = V(sq_t[:, blk % 2], ["sq%d" % (blk % 2)]); tmp = V(tmp_t[:, blk % 2], ["tmp%d" % (blk % 2)])
                    rs = V(rs_t, ["rs"])
                    p.act(sq, o, AF.Square)
                    p.mm(PS(1), ones, sq)
                    p.act(rs, PS(1), AF.Sqrt, scale=1.0 / 128.0, bias=epsv)
                    p.recip(rs, rs)
                    p.stt(tmp, o, gnw, rs, ALU.mult, ALU.mult)
                    p.act(sq, z, AF.Silu)
                    p.tt(yblk(4 + h, blk), tmp, sq, ALU.mult)
            woutb = V(wbuf_t[:, 0:8192].rearrange("p (c n) -> p c n", c=8), ["w1b", "w3b"])
            for half in range(2):
                st = stage()
                p.dma(dq(), st, V(wout_d[:, half * 512:(half + 1) * 512].rearrange("(c p) n -> p c n", p=128), []))
                p.copy(woutb[:, :, half * 512:(half + 1) * 512], V(st.ap.rearrange("p (c n) -> p c n", c=8), st.keys), eng="gpsimd")
            gT = V(gT_t, ["gT"])
            for blk in range(NBk):
                for dtl in range(8):
                    bank = 1 + dtl % 4
                    for kc in range(8):
                        p.mm(PS(bank), woutb[:, kc, dtl * 128:(dtl + 1) * 128], yblk(kc, blk), start=(kc == 0), stop=(kc == 7))
                    p.stt(xblk(dtl, blk), PS(bank), gate1[:, dtl:dtl + 1], xblk(dtl, blk), ALU.mult, ALU.add)
                sq = [V(sq_t[:, i], ["sq%d" % i]) for i in range(2)]
                tmp = [V(tmp_t[:, i], ["tmp%d" % i]) for i in range(2)]
                st = stage()
                hF = V(st.ap.rearrange("p (c n) -> p c n", c=8), st.keys)
                xv = V(xT_t, ["x%d" % blk])
                hT = V(yh_t[:, :, blk * TB:(blk + 1) * TB], ["yh%d" % blk])
                emit_hmix_block(p, xv, blk, ones, sq, PS(0), V(rs_t, ["rs"]), V(rstd_t, ["rstd"]), tmp, A2, B2, hT, hF=hF)
                for tt_ in range(4):
                    L = V(ps[:, 5 + tt_ % 2, 0:36], ["ps%d" % (5 + tt_ % 2)])
                    for kc in range(8):
                        p.mm(L, hF[:, kc, tt_ * 128:(tt_ + 1) * 128], wr[:, kc, :], start=(kc == 0), stop=(kc == 7))
                    R = V(rt_t[:, tt_ % 2], ["rt%d" % (tt_ % 2)])
                    Lb = R[:, 0:36]; lg = R[:, 0:4]; le = R[:, 4:36]
                    m = R[:, 36:37]; negm = R[:, 37:38]; ohg = R[:, 40:44]; e4 = R[:, 44:48]; ssum = R[:, 38:39]
                    pgt = R[:, 39:40]; pen = R[:, 48:52]; lem = R[:, 52:84]; m1 = R[:, 84:85]; oh1 = R[:, 88:120]
                    lem2 = R[:, 120:152]; m2 = R[:, 85:86]; dd = R[:, 86:87]; ed = R[:, 87:88]
                    c1 = R[:, 152:153]; c2 = R[:, 153:154]; den = R[:, 154:155]
                    p.tt(Lb, L, rb, ALU.add)
                    p.reduce(m, lg, ALU.max)
                    p.ts(ohg, lg, m, None, ALU.is_equal)
                    p.ts(negm, m, -1.0, None, ALU.mult)
                    p.act(e4, lg, AF.Exp, bias=negm)
                    p.reduce(ssum, e4, ALU.add)
                    p.recip(pgt, ssum)
                    p.ts(pen, ohg, 1.0, 1e30, ALU.subtract, ALU.mult)
                    for g in range(4):
                        p.ts(lem[:, g * 8:(g + 1) * 8], le[:, g * 8:(g + 1) * 8], pen[:, g:g + 1], None, ALU.add)
                    p.reduce(m1, lem, ALU.max)
                    p.ts(oh1, lem, m1, None, ALU.is_equal)
                    p.stt(lem2, oh1, -1e30, lem, ALU.mult, ALU.add)
                    p.reduce(m2, lem2, ALU.max)
                    p.tt(dd, m2, m1, ALU.subtract)
                    p.act(ed, dd, AF.Exp)
                    p.ts(den, ed, 1.0, None, ALU.add)
                    p.recip(den, den)
                    p.tt(c1, den, pgt, ALU.mult)
                    p.tt(c2, c1, ed, ALU.mult)
                    p.ts(lem2, lem2, m2, None, ALU.is_equal)
                    p.ts(oh1, oh1, c1, None, ALU.mult)
                    p.stt(oh1, lem2, c2, oh1, ALU.mult, ALU.add)
                    gp = V(ps[0:32, 7, 0:128], ["ps7"])
                    p.tr(gp, oh1, idn)
                    c0 = blk * TB + tt_ * 128
                    p.copy(gT[:, c0:c0 + 128], gp, eng="scalar")
            w1b = V(wbuf_t[:, 0:4096].rearrange("p (c n) -> p c n", c=8), ["w1b"])
            w3b = V(wbuf_t[:, 4096:8192].rearrange("p (c n) -> p c n", c=8), ["w3b"])
            w2b = V(wbuf_t[:, 8192:12288].rearrange("p (c n) -> p c n", c=4), ["w2b"])
            for e in range(32):
                for (dst, src, cc) in ((w1b, w1_d[e], 8), (w3b, w3_d[e], 8), (w2b, w2_d[e], 4)):
                    st = stage()
                    p.dma(dq(), st, V(src.rearrange("(c p) n -> p c n", p=128), []))
                    p.copy(dst, V(st.ap.rearrange("p (c n) -> p c n", c=cc), st.keys), eng="gpsimd")
                for blk in range(NBk):
                    bsl = slice(blk * TB, (blk + 1) * TB)
                    hT = V(yh_t[:, :, bsl], ["yh%d" % blk])
                    gb = PS(4)
                    p.mm(gb, sel[:, e, :], gT[:, bsl])
                    hg = V(hg_t[:, it % 2], ["hg%d" % (it % 2)])
                    for ht in range(4):
                        h1 = PS(ht % 2); h3 = PS(2 + ht % 2)
                        for kc in range(8):
                            p.mm(h1, w1b[:, kc, ht * 128:(ht + 1) * 128], hT[:, kc, :], start=(kc == 0), stop=(kc == 7))
                        for kc in range(8):
                            p.mm(h3, w3b[:, kc, ht * 128:(ht + 1) * 128], hT[:, kc, :], start=(kc == 0), stop=(kc == 7))
                        s1 = V(s1_t[:, ht % 2], ["s1%d" % (ht % 2)])
                        p.act(s1, h1, AF.Silu)
                        p.tt(s1, s1, gb, ALU.mult)
                        p.tt(hg[:, ht, :], h3, s1, ALU.mult)
                    for dtl in range(8):
                        yb = PS(5 + dtl % 3)
                        for ht in range(4):
                            p.mm(yb, w2b[:, ht, dtl * 128:(dtl + 1) * 128], hg[:, ht, :], start=(ht == 0), stop=(ht == 3))
                        p.stt(xblk(dtl, blk), yb, gate2[:, dtl:dtl + 1], xblk(dtl, blk), ALU.mult, ALU.add)
                    it += 1
            for blk in range(NBk):
                bsl = slice(blk * TB, (blk + 1) * TB)
                if last:
                    xv = V(xT_t, ["x%d" % blk])
                    for kc in range(8):
                        sqv = V(sq_t[:, kc % 2], ["sq%d" % (kc % 2)])
                        p.act(sqv, xv[:, kc, bsl], AF.Square)
                        p.mm(PS(0), ones, sqv, start=(kc == 0), stop=(kc == 7))
                    rs = V(rs_t, ["rs"]); rstd = V(rstd_t, ["rstd"])
                    p.act(rs, PS(0), AF.Sqrt, scale=1.0 / 1024.0, bias=epsv)
                    p.recip(rstd, rs)
                    for kc in range(8):
                        p.stt(xblk(kc, blk), xblk(kc, blk), fw[:, kc:kc + 1], rstd, ALU.mult, ALU.mult)
                for kc in range(8):
                    p.dma(dq(), V(xdst[kc * 128:(kc + 1) * 128, t0 + blk * TB:t0 + (blk + 1) * TB], []), xblk(kc, blk))
        p.finish("sync")
        p.emit(block)


FUSED_W_SHAPES = {
    "cT": [128, 8], "maskf": [128, 1024], "cst": [128, 11, 128], "sel": [32, 32, 128], "idn": [128, 128],
}
for _l in range(2):
    FUSED_W_SHAPES.update({
        "wada%d" % _l: [1024, 6144], "badaA%d" % _l: [128, 16], "nw%d" % _l: [128, 8], "win%d" % _l: [1024, 3600],
        "cw%d" % _l: [128, 2, 3], "gw%d" % _l: [128, 12, 3], "rpbg%d" % _l: [4, 128, 9, 1024], "par%d" % _l: [4, 128, 4],
        "smD%d" % _l: [128, 64], "wout%d" % _l: [1024, 1024], "wr%d" % _l: [1024, 36], "rb%d" % _l: [128, 36],
        "w1_%d" % _l: [32, 1024, 512], "w3_%d" % _l: [32, 1024, 512], "w2_%d" % _l: [32, 512, 1024],
    })


def build_F(nlayers=2, phases="ABCD"):
    nc = bass.Bass("TRN2", target_bir_lowering=False)
    dt = nc.dram_tensor
    xT_d = dt("xT", [1024, TSEQ], F32, kind="ExternalInput").ap()
    W = {k: dt(k, shp, F32, kind="ExternalInput").ap() for k, shp in FUSED_W_SHAPES.items()
         if not (k[-1].isdigit() and int(k[-1]) >= nlayers)}
    out_d = dt("outT", [1024, TSEQ], F32, kind="ExternalOutput").ap()
    S = {
        "projT": dt("s_projT", [3600, TSEQ], F32, kind="Internal").ap(),
        "nvtok": dt("s_nvtok", [TSEQ, 256], F32, kind="Internal").ap(),
        "abtok": dt("s_abtok", [TSEQ, 16], F32, kind="Internal").ap(),
        "ycT": dt("s_ycT", [256, TSEQ], F32, kind="Internal").ap(),
        "ynaT": dt("s_ynaT", [256, TSEQ], F32, kind="Internal").ap(),
        "gqkvn": dt("s_gqkvn", [1536, TSEQ], F32, kind="Internal").ap(),
        "oT": dt("s_oT", [512, TSEQ], F32, kind="Internal").ap(),
    }
    x1_d = dt("s_x1T", [1024, TSEQ], F32, kind="Internal").ap()
    dbg = {}
    if phases != "ABCD":
        for k, v in S.items():
            dbg[k] = dt("dbg_" + k, list(v.shape), F32, kind="ExternalOutput").ap()
    p = Prog(nc)
    for l in range(nlayers):
        xsrc = xT_d if l == 0 else x1_d
        last = (l == nlayers - 1)
        xdst = out_d if last else x1_d
        if "A" in phases:
            phase_A(nc, p, xsrc, W, S, l)
        if "B" in phases:
            phase_B(nc, p, W, S, l)
        if "C" in phases:
            phase_C(nc, p, W, S, l)
        if "D" in phases:
            phase_D(nc, p, xsrc, xdst, W, S, l, last and nlayers == 2)
    if dbg:
        with nc.Block() as block:
            i = 0
            for k in S:
                p.dma("sync" if i % 2 else "gpsimd", V(dbg[k], []), V(S[k], []))
                i += 1
            p.finish("sync")
            p.emit(block)
    return nc


def prep_F(P, nlayers=2):
    table_mask = None
    ins = []
    sel = np.zeros((32, 32, 128), np.float32)
    for e in range(32):
        sel[e, e, :] = 1.0
    idn = np.eye(128, dtype=np.float32)
    cst = gdn_consts()
    shared = {}
    for l in range(nlayers):
        table, maskf = _na_tables(P["na_rpb"][l])
        rpbg = np.zeros((4, 128, 9, 1024), np.float32)
        for s in range(4):
            rpbg[s, :, 0] = table(3)
            for si, r in enumerate(SPEC_ROWS):
                R = s * 32 + r
                rs = min(max(R - 4, 0), 120)
                rpbg[s, :, 1 + si] = table(rs - R + 7)
        par = np.zeros((4, 128, 4), np.float32)
        for h in range(4):
            par[h, :, 0] = P["gdn_a_log"][l][0, h]; par[h, :, 1] = P["gdn_a_log"][l][1, h]
            par[h, :, 2] = P["gdn_dt_bias"][l][0, h]; par[h, :, 3] = P["gdn_dt_bias"][l][1, h]
        rbv = np.concatenate([P["router_group_b"][l], P["router_expert_b"][l]])
        shared.update({
            "maskf": maskf,
            "wada%d" % l: P["w_ada"][l], "badaA%d" % l: _lay_pc(P["b_ada"][l][0:2048], 16),
            "nw%d" % l: _lay_pc(P["norm_mix_w"][l], 8), "win%d" % l: P["w_in"][l],
            "cw%d" % l: np.ascontiguousarray(P["conv_a_w"][l].T.reshape(2, 128, 3).transpose(1, 0, 2)),
            "gw%d" % l: np.ascontiguousarray(P["gdn_conv_w"][l].T.reshape(12, 128, 3).transpose(1, 0, 2)),
            "rpbg%d" % l: rpbg, "par%d" % l: par,
            "wout%d" % l: P["w_out"][l],
            "wr%d" % l: np.ascontiguousarray(np.concatenate([P["router_group_w"][l], P["router_expert_w"][l]], 1)),
            "rb%d" % l: np.ascontiguousarray(np.broadcast_to(rbv[None, :], (128, 36))).astype(np.float32),
            "w1_%d" % l: P["expert_w1"][l], "w3_%d" % l: P["expert_w3"][l], "w2_%d" % l: P["expert_w2"][l],
        })
    shared.update({"cst": cst, "sel": sel, "idn": idn})
    for b in range(2):
        d = dict(shared)
        d["xT"] = np.ascontiguousarray(P["x"][b].T)
        d["cT"] = _lay_pc(P["c"][b], 8)
        for l in range(nlayers):
            sm = np.zeros((128, 64), np.float32)
            sm[:, 0:8] = _lay_pc(P["c"][b], 8)
            sm[:, 8:16] = _lay_pc(P["norm_ffn_w"][l], 8)
            sm[:, 16:24] = _lay_pc(P["final_norm_w"], 8)
            sm[:, 24] = P["gdn_norm_w"][l]
            sm[:, 32:64] = _lay_pc(P["b_ada"][l][2048:6144], 32)
            d["smD%d" % l] = sm
        ins.append(d)
    return ins

_NC = {}


def kernel(**inputs):
    P = {k: np.ascontiguousarray(np.asarray(v, dtype=np.float32)) for k, v in inputs.items()}
    if "F" not in _NC:
        _NC["F"] = build_F()
    ins = prep_F(P)
    res = run_bass_kernel_spmd(_NC["F"], ins, core_ids=[0, 1]).results
    out = np.stack([np.ascontiguousarray(res[b]["outT"].T) for b in range(2)], 0)
    return out.astype(np.float32)
```

```python
import numpy as np
import concourse.bass as bass
import concourse.mybir as mybir
from concourse.bass_utils import run_bass_kernel_spmd

F32 = mybir.dt.float32
BF16 = mybir.dt.bfloat16
AF = mybir.ActivationFunctionType
ALU = mybir.AluOpType
AX = mybir.AxisListType

ENGS = ["tensor", "vector", "scalar", "gpsimd", "sync"]


class V:
    __slots__ = ("ap", "keys")

    def __init__(self, ap, keys):
        if ap is not None and type(ap).__name__.endswith("TensorHandle"):
            ap = ap[:]
        self.ap = ap
        self.keys = tuple(keys)

    def __getitem__(self, idx):
        return V(self.ap[idx], self.keys)

    def k(self, *keys):
        return V(self.ap, keys)


class Prog:
    def __init__(self, nc, n_dma_sems=24):
        self.nc = nc
        self.q = {e: [] for e in ENGS}
        self.cnt = {e: 0 for e in ENGS}
        self.sem = {e: nc.alloc_semaphore("c_" + e) for e in ENGS}
        self.dsem = [nc.alloc_semaphore("d_%d" % i) for i in range(n_dma_sems)]
        self.duse = [0] * n_dma_sems
        self.dnext = 0
        self.waited = {e: {} for e in ENGS}
        self.last_w = {}
        self.readers = {}
        self.semid = {}
        for e in ENGS:
            self.semid[id(self.sem[e])] = e
        self.nops = 0

    def _collect(self, eng, reads, writes):
        toks = []
        for r in reads:
            for key in r.keys:
                t = self.last_w.get(key)
                if t is not None:
                    toks.append((t, True))
                if isinstance(key, str) and key.startswith("ps"):
                    for t in self.readers.get(key, ()):
                        toks.append((t, False))
        for w in writes:
            for key in w.keys:
                t = self.last_w.get(key)
                if t is not None:
                    toks.append((t, True))
                for t in self.readers.get(key, ()):
                    toks.append((t, False))
        waits = {}
        mysem = self.sem[eng]
        for (sem, val), hard in toks:
            if sem is mysem:
                if eng == "tensor" or eng == "sync" or not hard:
                    continue
            sid = id(sem)
            if self.waited[eng].get(sid, 0) >= val:
                continue
            if waits.get(sid, (None, 0))[1] < val:
                waits[sid] = (sem, val)
        for sid, (sem, val) in waits.items():
            self.waited[eng][sid] = val
        return list(waits.values())

    def _commit(self, tok, reads, writes):
        for r in reads:
            for key in r.keys:
                self.readers.setdefault(key, []).append(tok)
        for w in writes:
            for key in w.keys:
                self.last_w[key] = tok
                self.readers[key] = []

    def op(self, eng, fn, reads=(), writes=()):
        waits = self._collect(eng, reads, writes)
        self.cnt[eng] += 1
        tok = (self.sem[eng], self.cnt[eng])
        self.q[eng].append((fn, waits, (self.sem[eng], 1)))
        self._commit(tok, reads, writes)
        self.nops += 1

    def dma(self, eng, out, in_, **kw):
        half = len(self.dsem) // 2
        if eng == "gpsimd":
            self.dnext_sw = (getattr(self, "dnext_sw", -1) + 1) % half
            i = half + self.dnext_sw
        else:
            self.dnext = (self.dnext + 1) % half
            i = self.dnext
        sem = self.dsem[i]
        waits = self._collect(eng, [in_], [out])
        if self.duse[i] > 0:
            sid = id(sem)
            val = 16 * self.duse[i]
            if self.waited[eng].get(sid, 0) < val:
                waits = [w for w in waits if w[0] is not sem] + [(sem, val)]
                self.waited[eng][sid] = val
        self.duse[i] += 1
        tok = (sem, 16 * self.duse[i])
        oap, iap = out.ap, in_.ap
        self.q[eng].append((lambda e: e.dma_start(out=oap, in_=iap, **kw), waits, (sem, 16)))
        self._commit(tok, [in_], [out])
        self.nops += 1
        return tok

    def finish(self, eng="sync"):
        waits = []
        for i, sem in enumerate(self.dsem):
            if self.duse[i] > 0:
                waits.append((sem, 16 * self.duse[i]))
        self.q[eng].append((None, waits, None))

    def wait_all(self, eng, views):
        waits = self._collect(eng, views, [])
        self.q[eng].append((None, waits, None))

    def emit(self, block):
        nc = self.nc
        for e in ENGS:
            items = self.q[e]
            if not items:
                continue

            def body(engine, items=items):
                for fn, waits, inc in items:
                    for sem, val in waits:
                        engine.wait_ge(sem, val)
                    if fn is None:
                        continue
                    ins = fn(engine)
                    if inc is not None:
                        ins.then_inc(inc[0], inc[1])
            getattr(block, e)(body)
        self.q = {e: [] for e in ENGS}

    def mm(self, out, lhsT, rhs, start=True, stop=True):
        o, l, r = out.ap, lhsT.ap, rhs.ap
        self.op("tensor", lambda e: e.matmul(o, l, r, start=start, stop=stop), [lhsT, rhs], [out])

    def tr(self, out, in_, ident):
        o, i, d = out.ap, in_.ap, ident.ap
        self.op("tensor", lambda e: e.transpose(o, i, d), [in_, ident], [out])

    def act(self, out, in_, func, bias=None, scale=None, accum_out=None, eng="scalar"):
        o, i = out.ap, in_.ap
        kw = {}
        rd = [in_]
        wr = [out]
        if bias is not None:
            if isinstance(bias, V):
                kw["bias"] = bias.ap
                rd.append(bias)
            else:
                kw["bias"] = bias
        if scale is not None:
            if isinstance(scale, V):
                kw["scale"] = scale.ap
                rd.append(scale)
            else:
                kw["scale"] = scale
        if accum_out is not None:
            kw["accum_out"] = accum_out.ap
            wr.append(accum_out)
        self.op("scalar", lambda e: e.activation(o, i, func, **kw), rd, wr)

    def tt(self, out, in0, in1, op, eng="vector"):
        o, a, b = out.ap, in0.ap, in1.ap
        self.op(eng, lambda e: e.tensor_tensor(o, a, b, op), [in0, in1], [out])

    def ts(self, out, in0, s1, s2, op0, op1=None, eng="vector", accum_out=None):
        o, a = out.ap, in0.ap
        rd = [in0]
        wr = [out]
        if isinstance(s1, V):
            rd.append(s1)
            s1 = s1.ap
        if isinstance(s2, V):
            rd.append(s2)
            s2 = s2.ap
        kw = {}
        if accum_out is not None:
            kw["accum_out"] = accum_out.ap
            wr.append(accum_out)
        if op1 is None:
            self.op(eng, lambda e: e.tensor_scalar(o, a, s1, s2, op0, **kw), rd, wr)
        else:
            self.op(eng, lambda e: e.tensor_scalar(o, a, s1, s2, op0, op1, **kw), rd, wr)

    def stt(self, out, in0, scalar, in1, op0, op1, eng="vector"):
        o, a, b = out.ap, in0.ap, in1.ap
        rd = [in0, in1]
        if isinstance(scalar, V):
            rd.append(scalar)
            scalar = scalar.ap
        self.op(eng, lambda e: e.scalar_tensor_tensor(o, a, scalar, b, op0, op1), rd, [out])

    def copy(self, out, in_, eng="vector"):
        o, i = out.ap, in_.ap
        if eng == "scalar":
            self.op(eng, lambda e: e.copy(o, i), [in_], [out])
        else:
            self.op(eng, lambda e: e.tensor_copy(o, i), [in_], [out])

    def memset(self, out, val, eng="vector"):
        o = out.ap
        self.op(eng, lambda e: e.memset(o, val), [], [out])

    def recip(self, out, in_):
        o, i = out.ap, in_.ap
        self.op("vector", lambda e: e.reciprocal(o, i), [in_], [out])

    def reduce(self, out, in_, op, axis=AX.X):
        o, i = out.ap, in_.ap
        self.op("vector", lambda e: e.tensor_reduce(o, i, axis, op), [in_], [out])


EPS = 1e-6
NTOK = 2048
TB = 512


def emit_mod(p, nc, pools, cT, wada_d, bada, ncol_tiles, modps, wst, cond, mod_sb):
    p.act(cond, cT, AF.Silu)
    nchunk = (ncol_tiles * 128) // 512
    for ch in range(nchunk):
        buf = wst[ch % 2]
        p.dma("sync" if ch % 2 == 0 else "gpsimd", buf,
              V(wada_d[:, ch * 512:(ch + 1) * 512].rearrange("(c p) n -> p c n", p=128), []))
        for jj in range(4):
            j = ch * 4 + jj
            for kc in range(8):
                p.mm(modps[:, j:j + 1], buf[:, kc, jj * 128:(jj + 1) * 128], cond[:, kc:kc + 1],
                     start=(kc == 0), stop=(kc == 7))
    p.tt(mod_sb, modps[:, 0:ncol_tiles], bada, ALU.add)


def emit_hmix_block(p, xT, blk, ones, sq, ss, rs, rstd_b, tmp, A1, B1, hT, hF=None):
    sl = slice(blk * TB, (blk + 1) * TB)
    for kc in range(8):
        s = sq[kc % 2]
        p.act(s, xT[:, kc, sl], AF.Square)
        p.mm(ss, ones, s, start=(kc == 0), stop=(kc == 7))
    p.act(rs, ss, AF.Sqrt, scale=1.0 / 1024.0, bias=EPSV[0])
    p.recip(rstd_b, rs)
    for kc in range(8):
        t = tmp[kc % 2]
        p.stt(t, xT[:, kc, sl], A1[:, kc:kc + 1], rstd_b, ALU.mult, ALU.mult)
        if hF is not None:
            p.ts(hF[:, kc, :], t, B1[:, kc:kc + 1], None, ALU.add)
            p.copy(hT[:, kc, :], hF[:, kc, :], eng="scalar")
        else:
            p.act(hT[:, kc, :], t, AF.Identity, bias=B1[:, kc:kc + 1])


EPSV = [None]


def build_A():
    nc = bass.Bass("TRN2", target_bir_lowering=False)
    dt = nc.dram_tensor
    xT_d = dt("xT", [1024, NTOK], F32, kind="ExternalInput").ap()
    cT_d = dt("cT", [128, 8], F32, kind="ExternalInput").ap()
    wada_d = dt("wada", [1024, 2048], F32, kind="ExternalInput").ap()
    bada_d = dt("bada", [128, 16], F32, kind="ExternalInput").ap()
    nw_d = dt("nw", [128, 8], F32, kind="ExternalInput").ap()
    win_d = dt("win", [1024, 3600], F32, kind="ExternalInput").ap()
    out_d = dt("projT", [3600, NTOK], F32, kind="ExternalOutput").ap()
    from contextlib import ExitStack
    with ExitStack() as es:
        def sb(name, shape, dtype):
            return es.enter_context(nc.sbuf_tensor(name, shape, dtype))
        xT = V(sb("xTs", [128, 8, NTOK], F32), ["xT"])
        wst_t = sb("wst", [128, 2, 8, 512], F32)
        wst = [V(wst_t[:, i], ["wst%d" % i]) for i in range(2)]
        winb = sb("winb", [128, 8, 3600], BF16)
        hT_t = sb("hT", [128, 2, 8, TB], BF16)
        sq_t = sb("sq", [128, 2, TB], F32)
        tmp_t = sb("tmp", [128, 2, TB], F32)
        rs = V(sb("rs", [128, TB], F32), ["rs"])
        rstd_b = V(sb("rstd", [128, TB], F32), ["rstd"])
        stage_t = sb("stage", [128, 4, TB], F32)
        small = sb("small", [128, 64], F32)
        ones = V(sb("ones", [128, 128], F32), ["ones"])
        epsv = V(sb("epsv", [128, 1], F32), ["epsv"])
        EPSV[0] = epsv
        ps = es.enter_context(nc.psum_tensor("ps", [128, 8, 512], F32))
        block = es.enter_context(nc.Block())
        p = Prog(nc)
        cT = V(small[:, 0:8], ["cT"]); cond = V(small[:, 8:16], ["cond"])
        bada = V(small[:, 16:32], ["bada"]); mod_sb = V(small[:, 32:48], ["mod"])
        nw = V(small[:, 48:56], ["nw"]); A1 = V(small[:, 56:64], ["A1"])
        modps = V(ps[:, 0, 0:16], ["ps0"])
        ss = V(ps[:, 1, :], ["ps1"])
        p.memset(ones, 1.0)
        p.memset(epsv, EPS)
        p.dma("sync", cT, V(cT_d, []))
        p.dma("sync", bada, V(bada_d, []))
        p.dma("sync", nw, V(nw_d, []))
        for kc in range(8):
            p.dma("gpsimd" if kc % 2 else "sync", xT[:, kc, :], V(xT_d[kc * 128:(kc + 1) * 128, :], []))
        emit_mod(p, nc, None, cT, wada_d, bada, 16, modps, wst, cond, mod_sb)
        B1 = mod_sb[:, 0:8]
        p.stt(A1, mod_sb[:, 8:16], 1.0, nw, ALU.add, ALU.mult)
        ci = 0
        for c0 in range(0, 3600, 512):
            cw = min(512, 3600 - c0)
            buf = wst[ci % 2]
            p.dma("sync" if ci % 2 == 0 else "gpsimd", buf[:, :, 0:cw],
                  V(win_d[:, c0:c0 + cw].rearrange("(c p) n -> p c n", p=128), []))
            for kc in range(8):
                dst = V(winb[:, kc, c0:c0 + cw], ["winb%d" % ci])
                p.copy(dst, buf[:, kc, 0:cw], eng=("gpsimd" if kc % 2 else "vector"))
            ci += 1
        nev = 0
        for blk in range(NTOK // TB):
            hT = V(hT_t[:, blk % 2], ["hT%d" % (blk % 2)])
            sq = [V(sq_t[:, i], ["sq%d" % i]) for i in range(2)]
            tmp = [V(tmp_t[:, i], ["tmp%d" % i]) for i in range(2)]
            emit_hmix_block(p, xT, blk, ones, sq, ss, rs, rstd_b, tmp, A1, B1, hT)
            for j in range(29):
                rows = min(128, 3600 - j * 128)
                bank = 2 + (nev % 6)
                pj = V(ps[0:rows, bank, :], ["ps%d" % bank])
                ci = (j * 128) // 512
                for kc in range(8):
                    p.mm(pj, V(winb[:, kc, j * 128:j * 128 + rows], ["winb%d" % ci]), hT[:, kc, :],
                         start=(kc == 0), stop=(kc == 7))
                st = V(stage_t[0:rows, nev % 4, :], ["stage%d" % (nev % 4)])
                if nev % 2 == 0:
                    p.copy(st, pj, eng="vector")
                else:
                    p.copy(st, pj, eng="scalar")
                p.dma("sync" if nev % 2 == 0 else "gpsimd",
                      V(out_d[j * 128:j * 128 + rows, blk * TB:(blk + 1) * TB], []), st)
                nev += 1
        p.finish("sync")
        p.emit(block)
    return nc


NSPEC = 8
SPEC_ROWS = [0, 1, 2, 3, 28, 29, 30, 31]


def build_B(parts=(1, 1, 1)):
    nc = bass.Bass("TRN2", target_bir_lowering=False)
    dt = nc.dram_tensor
    convin_d = dt("convin", [768, NTOK + 2], F32, kind="ExternalInput").ap()
    cw_d = dt("cw", [128, 2, 3], F32, kind="ExternalInput").ap()
    gqkv_d = dt("gqkv", [1536, NTOK + 2], F32, kind="ExternalInput").ap()
    gw_d = dt("gw", [128, 12, 3], F32, kind="ExternalInput").ap()
    naq_d = dt("naq", [256, NTOK], F32, kind="ExternalInput").ap()
    nak_d = dt("nak", [256, NTOK], F32, kind="ExternalInput").ap()
    nave_d = dt("nave", [128, 16, 256], F32, kind="ExternalInput").ap()
    navo_d = dt("navo", [128, 16, 256], F32, kind="ExternalInput").ap()
    kspec_d = dt("kspec", [256, NSPEC * 512], F32, kind="ExternalInput").ap()
    vspec_d = dt("vspec", [128, NSPEC * 4, 256], F32, kind="ExternalInput").ap()
    rpbg_d = dt("rpbg", [128, 9, 1024], F32, kind="ExternalInput").ap()
    maskf_d = dt("maskf", [128, 1024], F32, kind="ExternalInput").ap()
    yconv_d = dt("yconvT", [256, NTOK], F32, kind="ExternalOutput").ap()
    gqkvn_d = dt("gqkvn", [1536, NTOK], F32, kind="ExternalOutput").ap()
    yna_d = dt("yna", [NTOK, 256], F32, kind="ExternalOutput").ap()
    from contextlib import ExitStack
    with ExitStack() as es:
        def sb(name, shape, dtype):
            return es.enter_context(nc.sbuf_tensor(name, shape, dtype))
        NST = 3
        stg_t = sb("stg", [128, NST, 4096], F32)
        u_t = sb("u", [128, NTOK + 2], F32)
        acc_t = sb("acc", [128, NTOK], F32)
        sil_t = sb("sil", [128, 2, NTOK], F32)
        sq_t = sb("sq", [128, 2, TB], F32)
        rs_t = sb("rs", [128, 2, TB], F32)
        ones_t = sb("ones", [128, 128], F32)
        wsm = sb("wsm", [128, 64], F32)
        qb_t = sb("qb", [128, 2, NTOK], BF16)
        kb_t = sb("kb", [128, 2, NTOK], BF16)
        ksp_t = sb("ksp", [128, 2, NSPEC * 512], BF16)
        ve_t = sb("ve", [128, 16, 4, 65], BF16)
        vo_t = sb("vo", [128, 16, 4, 65], BF16)
        vs_t = sb("vs", [128, NSPEC * 4, 4, 65], BF16)
        bias_t = sb("biass", [128, 9, 1024], F32)
        maskf_t = sb("maskfs", [128, 1024], F32)
        sc_t = sb("sc", [128, 2, 512], F32)
        pT_t = sb("pT", [128, 2, 512], BF16)
        pos_t = sb("pos", [64, 2, 260], F32)
        rden_t = sb("rden", [64, 2, 4], F32)
        yst_t = sb("yst", [64, 2, 256], F32)
        ps = es.enter_context(nc.psum_tensor("ps", [128, 8, 512], F32))
        block = es.enter_context(nc.Block())
        p = Prog(nc)
        stg_i = [0]

        def stage():
            i = stg_i[0] % NST
            stg_i[0] += 1
            return V(stg_t[:, i], ["stg%d" % i])
        dq_i = [0]

        def dq():
            dq_i[0] += 1
            return "sync" if dq_i[0] % 2 else "gpsimd"
        ones = V(ones_t, ["ones"])
        p.memset(ones, 1.0)
        cw = V(wsm[:, 0:6], ["cw"])
        gw = V(wsm[:, 6:42], ["gw"])
        epsv = V(wsm[:, 42:43], ["epsv"])
        p.memset(epsv, EPS)
        p.dma("sync", cw, V(cw_d.rearrange("p a b -> p (a b)"), []))
        p.dma("sync", gw, V(gw_d.rearrange("p a b -> p (a b)"), []))
        u = V(u_t, ["u"]); acc = V(acc_t, ["acc"])

        def conv3(src, wv, base):
            p.ts(acc, src[:, 0:NTOK], wv[:, base:base + 1], None, ALU.mult)
            p.stt(acc, src[:, 1:NTOK + 1], wv[:, base + 1:base + 2], acc, ALU.mult, ALU.add)
            p.stt(acc, src[:, 2:NTOK + 2], wv[:, base + 2:base + 3], acc, ALU.mult, ALU.add)

        for ct in (range(2) if parts[0] else []):
            bufs = []
            for g in range(3):
                st = stage()
                p.dma(dq(), st[:, 0:NTOK + 2], V(convin_d[g * 256 + ct * 128:g * 256 + (ct + 1) * 128, :], []))
                bufs.append(st)
            cb, cc, cx = bufs
            p.tt(u, cc[:, 0:NTOK + 2], cx[:, 0:NTOK + 2], ALU.mult)
            conv3(u, cw, ct * 3)
            yo = V(sil_t[:, ct], ["sil%d" % ct])
            p.tt(yo, acc, cb[:, 1:NTOK + 1], ALU.mult)
            p.dma(dq(), V(yconv_d[ct * 128:(ct + 1) * 128, :], []), yo)

        ssb = 0
        for ct in (range(12) if parts[1] else []):
            st = stage()
            p.dma(dq(), st[:, 0:NTOK + 2], V(gqkv_d[ct * 128:(ct + 1) * 128, :], []))
            conv3(st, gw, ct * 3)
            so = V(sil_t[:, ct % 2], ["sil%d" % (ct % 2)])
            p.act(so, acc, AF.Silu)
            if ct < 8:
                qscale = (128.0 ** -0.5) if ct < 4 else 1.0
                for blk in range(NTOK // TB):
                    sl = slice(blk * TB, (blk + 1) * TB)
                    sq = V(sq_t[:, ssb % 2], ["sq%d" % (ssb % 2)])
                    rs = V(rs_t[:, ssb % 2], ["rs%d" % (ssb % 2)])
                    bank = ssb % 2
                    ss = V(ps[:, bank, :], ["ps%d" % bank])
                    ssb += 1
                    p.act(sq, so[:, sl], AF.Square)
                    p.mm(ss, ones, sq)
                    p.act(rs, ss, AF.Sqrt, bias=epsv)
                    p.recip(rs, rs)
                    p.stt(so[:, sl], so[:, sl], qscale, rs, ALU.mult, ALU.mult)
            p.dma(dq(), V(gqkvn_d[ct * 128:(ct + 1) * 128, :], []), so)

        def load_cast(dst_views, src_aps, width):
            for dv, sa in zip(dst_views, src_aps):
                st = stage()
                p.dma(dq(), st[:, 0:width], V(sa, []))
                p.copy(dv, st[:, 0:width], eng="gpsimd")
        qb = V(qb_t, ["qb"]); kb = V(kb_t, ["kb"]); ksp = V(ksp_t, ["ksp"])
        load_cast([qb[:, i, :] for i in range(2)], [naq_d[i * 128:(i + 1) * 128, :] for i in range(2)], NTOK)
        load_cast([kb[:, i, :] for i in range(2)], [nak_d[i * 128:(i + 1) * 128, :] for i in range(2)], NTOK)
        load_cast([ksp[:, i, :] for i in range(2)], [kspec_d[i * 128:(i + 1) * 128, :] for i in range(2)], NSPEC * 512)
        ve = V(ve_t, ["ve"]); vo = V(vo_t, ["vo"]); vs = V(vs_t, ["vs"])
        for (dst, src_d, nt) in ((ve, nave_d, 16), (vo, navo_d, 16), (vs, vspec_d, 32)):
            p.memset(dst[:, :, :, 64:65], 1.0, eng="gpsimd")
            for c0 in range(0, nt, 16):
                st = stage()
                p.dma(dq(), st[:, 0:16 * 256], V(src_d[:, c0:c0 + 16, :].rearrange("p a b -> p (a b)"), []))
                p.copy(dst[:, c0:c0 + 16, :, 0:64],
                       V(st.ap[:, 0:16 * 256].rearrange("p (a h d) -> p a h d", a=16, h=4), st.keys), eng="vector")
        bias = V(bias_t, ["bias"]); maskf = V(maskf_t, ["maskf"])
        p.dma("sync", maskf, V(maskf_d, []))
        for i in range(9):
            p.dma(dq(), bias[:, i, :], V(rpbg_d[:, i, :], []))
        for i in range(9):
            p.stt(bias[:, i, :], bias[:, i, :], 8.0, maskf, ALU.mult, ALU.add)
        for r in (range(32) if parts[2] else []):
            par = r % 2
            if r in SPEC_ROWS:
                s = SPEC_ROWS.index(r)
                bi = 1 + s

                def ktile(tile, off, t, s=s):
                    return ksp[off:off + 64, tile, s * 512 + t * 128:s * 512 + (t + 1) * 128]

                def vtile(t, h, s=s):
                    return vs[:, s * 4 + t, h, :]
            else:
                bi = 0
                lr = r - 4

                def ktile(tile, off, t, lr=lr):
                    return kb[off:off + 64, tile, (lr + 2 * t) * 64:(lr + 2 * t + 2) * 64]
                if lr % 2 == 0:
                    def vtile(t, h, lr=lr):
                        return ve[:, lr // 2 + t, h, :]
                else:
                    def vtile(t, h, lr=lr):
                        return vo[:, (lr - 1) // 2 + t, h, :]
            pob = 6 + par
            po = V(ps[0:64, pob, 0:260], ["ps%d" % pob])
            for hl in range(2):
                off = 64 * hl
                bank = 2 + 2 * par + hl
                psc = V(ps[:, bank, :], ["ps%d" % bank])
                for j in range(2):
                    for t in range(4):
                        p.mm(psc[:, (j * 4 + t) * 64:(j * 4 + t + 1) * 64], ktile(j, off, t),
                             qb[off:off + 64, j, r * 64:(r + 1) * 64])
                sc = V(sc_t[:, hl], ["sc%d" % hl])
                bview = V(bias.ap[:, bi, :].rearrange("p (h x) -> p h x", h=4)[:, hl::2, :], bias.keys)
                p.tt(V(sc.ap.rearrange("p (h x) -> p h x", h=2), sc.keys),
                     V(psc.ap.rearrange("p (h x) -> p h x", h=2), psc.keys), bview, ALU.add)
                pT = V(pT_t[:, hl], ["pT%d" % hl])
                p.act(pT, sc, AF.Exp, scale=0.125)
                for j in range(2):
                    h = 2 * j + hl
                    for t in range(4):
                        p.mm(po[:, h * 65:(h + 1) * 65], pT[:, (j * 4 + t) * 64:(j * 4 + t + 1) * 64], vtile(t, h),
                             start=(t == 0), stop=(t == 3))
            pos = V(pos_t[:, par], ["pos%d" % par])
            p.copy(pos, po, eng="scalar")
            rden = V(rden_t[:, par], ["rden%d" % par])
            posv = V(pos.ap.rearrange("p (h d) -> p h d", h=4), pos.keys)
            p.recip(rden, V(posv.ap[:, :, 64:65].rearrange("p h o -> p (h o)"), pos.keys))
            yst = V(yst_t[:, par], ["yst%d" % par])
            for h in range(4):
                p.ts(yst[:, h * 64:(h + 1) * 64], posv[:, h, 0:64], rden[:, h:h + 1], None, ALU.mult, eng="gpsimd")
            p.dma(dq(), V(yna_d[r * 64:(r + 1) * 64, :], []), yst)
        p.finish("sync")
        p.emit(block)
    return nc


def _pad_cols(a, lo, hi, n):
    out = np.zeros((a.shape[0], hi - lo), a.dtype)
    s0, s1 = max(lo, 0), min(hi, n)
    out[:, s0 - lo:s1 - lo] = a[:, s0:s1]
    return out


def _na_tables(rpb):
    kc = np.arange(64)[:, None]; qc = np.arange(64)[None, :]
    cs = np.clip(qc - 8, 0, 48)
    inwin = (kc >= cs) & (kc < cs + 16)
    dc = np.clip(kc - qc + 15, 0, 30)
    mask = np.where(inwin, 0.0, -240000.0).astype(np.float32)
    maskf = np.zeros((128, 4, 4, 64), np.float32)
    maskf[:] = np.concatenate([mask, mask], 0)[:, None, None, :]
    def table(dr0):
        tb = np.zeros((128, 4, 4, 64), np.float32)
        for a in range(2):
            for t in range(4):
                dr = dr0 + 2 * t + a
                tb[a * 64:(a + 1) * 64, :, t, :] = np.transpose(rpb[:, dr][:, dc], (1, 0, 2))
        return tb.reshape(128, 1024)
    return table, maskf.reshape(128, 1024)


def prep_B(projT, P, l):
    table, maskf = _na_tables(P["na_rpb"][l])
    cw = np.ascontiguousarray(P["conv_a_w"][l].T.reshape(2, 128, 3).transpose(1, 0, 2))
    gw = np.ascontiguousarray(P["gdn_conv_w"][l].T.reshape(12, 128, 3).transpose(1, 0, 2))
    ins = []
    for core in range(8):
        b = core // 4; t0 = (core % 4) * NTOK
        pb = projT[:, b * 8192:(b + 1) * 8192]
        vb = pb[1280:1536].T
        v = vb[t0:t0 + NTOK]
        nave = np.ascontiguousarray(v.reshape(16, 128, 256).transpose(1, 0, 2))
        navo = np.zeros((128, 16, 256), np.float32)
        navo[:, :15] = v[64:64 + 15 * 128].reshape(15, 128, 256).transpose(1, 0, 2)
        kspec = np.zeros((256, NSPEC * 512), np.float32)
        vspec = np.zeros((128, NSPEC * 4, 256), np.float32)
        rpbg = np.zeros((128, 9, 1024), np.float32)
        rpbg[:, 0] = table(3)
        row0 = (core % 4) * 32
        for s, r in enumerate(SPEC_ROWS):
            R = row0 + r
            rs = min(max(R - 4, 0), 120)
            kspec[:, s * 512:(s + 1) * 512] = pb[1024:1280, rs * 64:rs * 64 + 512]
            vspec[:, s * 4:(s + 1) * 4] = vb[rs * 64:rs * 64 + 512].reshape(4, 128, 256).transpose(1, 0, 2)
            rpbg[:, 1 + s] = table(rs - R + 7)
        ins.append({
            "convin": _pad_cols(pb[0:768], t0 - 1, t0 + NTOK + 1, 8192),
            "cw": cw, "gw": gw,
            "gqkv": _pad_cols(pb[1536:3072], t0 - 1, t0 + NTOK + 1, 8192),
            "naq": np.ascontiguousarray(pb[768:1024, t0:t0 + NTOK]),
            "nak": np.ascontiguousarray(pb[1024:1280, t0:t0 + NTOK]),
            "nave": nave, "navo": navo, "kspec": kspec, "vspec": vspec,
            "rpbg": rpbg, "maskf": maskf,
        })
    return ins


NSC = 64
C_IDENT, C_ONES, C_TRIF, C_TRIB, C_BLK, C_SELA, C_SELB, C_MSF, C_MSB, C_MIF, C_MIB = range(11)


def gdn_consts():
    i = np.arange(128)
    t = i[:, None]; c = i[None, :]
    same = (t // 64) == (c // 64)
    cs = np.zeros((128, 11, 128), np.float32)
    cs[:, C_IDENT] = np.eye(128)
    cs[:, C_ONES] = 1.0
    cs[:, C_TRIF] = same & (t <= c)
    cs[:, C_TRIB] = same & (t >= c)
    cs[:, C_BLK] = same
    cs[:, C_SELA] = (t < 64) & (c >= 0)
    cs[:, C_SELB] = (t >= 64) & (c >= 0)
    cc = i[:, None]; ss = i[None, :]
    same2 = (cc // 64) == (ss // 64)
    cs[:, C_MSF] = np.where(same2 & (cc > ss), 0.0, 30000.0)
    cs[:, C_MSB] = np.where(same2 & (cc < ss), 0.0, 30000.0)
    sp = i[:, None]; cf = i[None, :]
    cs[:, C_MIF] = np.where(same2 & (cf >= sp), 0.0, -30000.0)
    cs[:, C_MIB] = np.where(same2 & (cf <= sp), 0.0, -30000.0)
    return cs


def build_C(nsc_run=NSC, mode=2):
    nc = bass.Bass("TRN2", target_bir_lowering=False)
    dt = nc.dram_tensor
    T = NSC * 128
    qnT_d = dt("qnT", [128, T], F32, kind="ExternalInput").ap()
    knT_d = dt("knT", [128, T], F32, kind="ExternalInput").ap()
    kn_d = dt("kn", [128, NSC, 128], F32, kind="ExternalInput").ap()
    v_d = dt("v", [128, NSC, 128], F32, kind="ExternalInput").ap()
    ab_d = dt("ab", [128, NSC, 4], F32, kind="ExternalInput").ap()
    par_d = dt("par", [128, 4], F32, kind="ExternalInput").ap()
    cst_d = dt("cst", [128, 11, 128], F32, kind="ExternalInput").ap()
    o_d = dt("o", [T, 128], F32, kind="ExternalOutput").ap()
    from contextlib import ExitStack
    with ExitStack() as es:
        def sb(name, shape, dtype):
            return es.enter_context(nc.sbuf_tensor(name, shape, dtype))
        cst_t = sb("cst_s", [128, 11, 128], F32)
        cstb_t = sb("cstb", [128, 128], BF16)
        stg_t = sb("stg", [128, 2, 2048], F32)
        qnT_t = sb("qnTb", [128, T], BF16)
        knT_t = sb("knTb", [128, T], BF16)
        kn_t = sb("knb", [128, NSC, 128], BF16)
        v_t = sb("vb", [128, NSC, 128], BF16)
        oacc_t = sb("oacc", [128, NSC, 128], F32)
        ab_t = sb("abs", [128, NSC, 4], F32)
        par_t = sb("pars", [128, 8], F32)
        NPS = 12
        pre_t = sb("pre", [128, 2, NPS, NSC], F32)
        S_t = sb("S", [128, 2, 128], F32)
        Sb_t = sb("Sb", [128, 2, 128], BF16)
        vnew_t = sb("vnew", [128, 2, 128], BF16)
        NF = 6; NB = 16
        wf_t = sb("wf", [128, 2, 2, NF, 128], F32)
        wb_t = sb("wb", [128, 2, 2, NB, 128], BF16)
        ps = es.enter_context(nc.psum_tensor("ps", [128, 8, 512], F32))
        block = es.enter_context(nc.Block())
        p = Prog(nc)
        cst = V(cst_t, ["cst"])

        def C(i):
            return cst[:, i, :]
        identb = V(cstb_t, ["cstb"])
        dq_i = [0]

        def dq():
            dq_i[0] += 1
            return "sync" if dq_i[0] % 2 else "gpsimd"
        p.dma("sync", cst, V(cst_d, []))
        p.copy(identb, C(C_IDENT))
        ab = V(ab_t, ["ab"]); par = V(par_t, ["par"])
        p.dma("sync", ab, V(ab_d, []))
        p.dma("sync", par[:, 0:4], V(par_d, []))
        si = 0
        for (dst_t, src, kind) in ((qnT_t, qnT_d, "T"), (knT_t, knT_d, "T"), (kn_t, kn_d, "N"), (v_t, v_d, "N")):
            for c4 in range(4):
                st = V(stg_t[:, si % 2], ["stg%d" % (si % 2)])
                si += 1
                if kind == "T":
                    p.dma(dq(), st, V(src[:, c4 * 2048:(c4 + 1) * 2048], []))
                    p.copy(V(dst_t[:, c4 * 2048:(c4 + 1) * 2048], [dst_t.name if hasattr(dst_t, "name") else id(dst_t)]), st,
                           eng=("gpsimd" if c4 % 2 else "vector"))
                else:
                    p.dma(dq(), st, V(src[:, c4 * 16:(c4 + 1) * 16, :].rearrange("p a b -> p (a b)"), []))
                    p.copy(V(dst_t[:, c4 * 16:(c4 + 1) * 16, :].rearrange("p a b -> p (a b)"), [id(dst_t)]), st,
                           eng=("gpsimd" if c4 % 2 else "vector"))
        qnT = V(qnT_t, [qnT_t.name if hasattr(qnT_t, "name") else id(qnT_t)])
        knT = V(knT_t, [knT_t.name if hasattr(knT_t, "name") else id(knT_t)])
        kn = V(kn_t, [id(kn_t)]); vv = V(v_t, [id(v_t)])
        p.act(par[:, 4:6], par[:, 0:2], AF.Exp)
        p.ts(par[:, 6:8], par[:, 4:6], -1.0, None, ALU.mult)
        pre = V(pre_t, ["pre"])
        mA = V(cst.ap[:, C_SELA, 0:1], cst.keys)
        mB = V(cst.ap[:, C_SELB, 0:1], cst.keys)
        for d in range(2):
            def S_(i, d=d):
                return V(pre_t[:, d, i, :], ["pre%d_%d" % (d, i)])
            a_v = V(ab_t[:, :, d], ["ab"]); b_v = V(ab_t[:, :, 2 + d], ["ab"])
            p.ts(S_(11), a_v, par[:, 2 + d:3 + d], None, ALU.add)
            p.act(S_(11), S_(11), AF.Exp)
            p.act(S_(0), S_(11), AF.Ln, bias=1.0)
            p.ts(S_(0), S_(0), par[:, 6 + d:7 + d], None, ALU.mult)
            p.act(S_(1), b_v, AF.Sigmoid)
            tri = C(C_TRIF if d == 0 else C_TRIB)
            pb = V(ps[:, 0, :], ["ps0"])
            p.mm(pb[:, 0:NSC], tri, S_(0))
            p.mm(pb[:, 64:64 + NSC], C(C_BLK), S_(0))
            p.mm(pb[:, 128:128 + NSC], C(C_SELA), S_(0))
            p.mm(pb[:, 192:192 + NSC], C(C_SELB), S_(0))
            p.copy(S_(2), pb[:, 0:NSC])
            p.copy(S_(3), pb[:, 64:64 + NSC])
            p.act(S_(9), pb[:, 128:128 + NSC], AF.Exp)
            p.act(S_(10), pb[:, 192:192 + NSC], AF.Exp)
            p.act(S_(4), S_(2), AF.Exp)
            p.tt(S_(5), S_(1), S_(4), ALU.mult)
            p.tt(S_(11), S_(3), S_(2), ALU.subtract)
            p.act(S_(11), S_(11), AF.Exp)
            p.ts(S_(6), S_(11), mA, None, ALU.mult)
            p.ts(S_(7), S_(11), mB, None, ALU.mult)
            p.ts(S_(8), S_(1), -1.0, None, ALU.mult)
        for d in range(2):
            p.memset(V(S_t[:, d], ["S%d" % d]), 0.0)
            p.memset(V(Sb_t[:, d], ["Sb%d" % d]), 0.0)
            p.memset(V(vnew_t[:, d], ["vn%d" % d]), 0.0)
        ev = [0]

        def evac(out, in_):
            ev[0] += 1
            p.copy(out, in_, eng=("scalar" if ev[0] % 2 else "vector"))

        def do_sc(d, sc, step):
            par_ = step % 2
            tok = slice(sc * 128, (sc + 1) * 128)

            def sc_(i):
                return V(pre_t[:, d, i, sc:sc + 1], ["pre%d_%d" % (d, i)])

            def F(i):
                return V(wf_t[:, d, par_, i], ["wf%d%d_%d" % (d, par_, i)])

            def B(i):
                return V(wb_t[:, d, par_, i], ["wb%d%d_%d" % (d, par_, i)])
            b0 = V(ps[:, 4 * d + 0, :], ["ps%d" % (4 * d)])
            b1 = V(ps[:, 4 * d + 1, :], ["ps%d" % (4 * d + 1)])
            b2 = V(ps[:, 4 * d + 2, :], ["ps%d" % (4 * d + 2)])
            b3 = V(ps[:, 4 * d + 3, :], ["ps%d" % (4 * d + 3)])

            def sl(bk, i):
                return bk[:, i * 128:(i + 1) * 128]
            Gdiag = F(0)
            p.ts(Gdiag, C(C_IDENT), sc_(2), None, ALU.mult)
            p.mm(sl(b0, 0), C(C_ONES), Gdiag, start=True, stop=False)
            p.mm(sl(b0, 0), C(C_IDENT), C(C_MSF if d == 0 else C_MSB), start=False, stop=True)
            p.mm(sl(b0, 1), C(C_ONES), Gdiag, start=True, stop=False)
            p.mm(sl(b0, 1), C(C_IDENT), C(C_MIF if d == 0 else C_MIB), start=False, stop=True)
            p.mm(sl(b0, 2), knT[:, tok], knT[:, tok])
            p.mm(sl(b0, 3), knT[:, tok], qnT[:, tok])
            p.ts(F(1), sl(b0, 0), sc_(2), 0.0, ALU.subtract, ALU.max)
            p.act(F(1), F(1), AF.Exp, scale=-1.0)
            p.ts(F(2), sl(b0, 1), sc_(2), 0.0, ALU.subtract, ALU.min)
            p.act(F(2), F(2), AF.Exp)
            A = B(0)
            p.stt(A, sl(b0, 2), sc_(8), F(1), ALU.mult, ALU.mult)
            p.tt(B(1), sl(b0, 3), F(2), ALU.mult)
            intraT = B(1)
            p.mm(sl(b1, 0), A, identb)
            N = B(2)
            evac(N, sl(b1, 0))
            P = B(3)
            p.tt(P, N, identb, ALU.add)
            M, Mt = N, A
            slot = 1
            for j in range(1, 6):
                Mn = B(4 + (j % 2) * 2); Mtn = B(5 + (j % 2) * 2)
                s_mt = sl(b1, slot % 4); slot += 1
                p.mm(s_mt, M, Mt)
                evac(Mtn, s_mt)
                if j < 5:
                    s_m = sl(b1, slot % 4); slot += 1
                    p.mm(s_m, Mt, M)
                    evac(Mn, s_m)
                s_p = sl(b1, slot % 4); slot += 1
                p.mm(s_p, Mtn, P)
                Pn = B(8 + (j % 2))
                p.tt(Pn, s_p, P, ALU.add)
                P = Pn
                M, Mt = Mn, Mtn
            Vb = B(10); Kbg = B(11); kdA = B(12); kdB = B(13)
            p.ts(Vb, vv[:, sc, :], sc_(1), None, ALU.mult, eng="gpsimd")
            p.ts(Kbg, kn[:, sc, :], sc_(5), None, ALU.mult, eng="gpsimd")
            p.ts(kdA, kn[:, sc, :], sc_(6), None, ALU.mult, eng="gpsimd")
            p.ts(kdB, kn[:, sc, :], sc_(7), None, ALU.mult, eng="gpsimd")
            p.mm(sl(b2, 0), P, Vb)
            p.mm(sl(b2, 1), Kbg, P)
            u_sb = F(3); wT = B(14)
            evac(u_sb, sl(b2, 0))
            evac(wT, sl(b2, 1))
            if mode < 2:
                p.copy(V(oacc_t[:, sc, :], ["oacc%d" % sc]), u_sb)
                return
            S = V(S_t[:, d], ["S%d" % d]); Sb = V(Sb_t[:, d], ["Sb%d" % d]); vn = V(vnew_t[:, d], ["vn%d" % d])
            oacc = V(oacc_t[:, sc, :], ["oacc%d" % sc])
            first = (step < NSC // 2)
            for half in ((0, 1) if d == 0 else (1, 0)):
                rows = slice(half * 64, (half + 1) * 64)
                ctok = slice(sc * 128 + half * 64, sc * 128 + (half + 1) * 64)
                p.mm(sl(b3, 0)[rows], wT[:, rows], Sb)
                p.tt(vn[rows], u_sb[rows], sl(b3, 0)[rows], ALU.subtract)
                p.mm(sl(b2, 2)[rows], qnT[:, ctok], Sb)
                p.mm(sl(b2, 3)[rows], intraT[:, rows], vn)
                p.mm(sl(b3, 1), kdA if half == 0 else kdB, vn)
                egl = sc_(9 + half)
                p.stt(Sb, S, egl, sl(b3, 1), ALU.mult, ALU.add)
                p.stt(S, S, egl, sl(b3, 1), ALU.mult, ALU.add)
                tiv = F(4)
                p.copy(tiv[rows], sl(b2, 3)[rows], eng="scalar")
                eG = V(pre_t[rows, d, 4, sc:sc + 1], ["pre%d_4" % d])
                if first:
                    p.stt(oacc[rows], sl(b2, 2)[rows], eG, tiv[rows], ALU.mult, ALU.add)
                else:
                    p.stt(F(5)[rows], sl(b2, 2)[rows], eG, tiv[rows], ALU.mult, ALU.add)
                    p.tt(oacc[rows], oacc[rows], F(5)[rows], ALU.add, eng="gpsimd")
        for step in range(nsc_run if mode > 0 else 0):
            do_sc(0, step, step)
            do_sc(1, NSC - 1 - step, step)
        for c4 in range(4):
            p.dma(dq(), V(o_d.rearrange("(n p) d -> p n d", p=128)[:, c4 * 16:(c4 + 1) * 16, :], []),
                  V(oacc_t[:, c4 * 16:(c4 + 1) * 16, :], ["oacc%d" % i for i in range(c4 * 16, (c4 + 1) * 16)]))
        p.finish("sync")
        p.emit(block)
    return nc


def prep_C(projT, gqkvn_full, P, l):
    cst = gdn_consts()
    ins = []
    for core in range(8):
        b = core // 4; h = core % 4
        tk = slice(b * 8192, (b + 1) * 8192)
        qT = gqkvn_full[h * 128:(h + 1) * 128, tk]
        kT = gqkvn_full[512 + h * 128:512 + (h + 1) * 128, tk]
        vT = gqkvn_full[1024 + h * 128:1024 + (h + 1) * 128, tk]
        def tokmaj(aT):
            return np.ascontiguousarray(aT.T.reshape(NSC, 128, 128).transpose(1, 0, 2))
        abT = np.stack([projT[3584 + h, tk], projT[3588 + h, tk], projT[3592 + h, tk], projT[3596 + h, tk]], -1)
        ab = np.ascontiguousarray(abT.reshape(NSC, 128, 4).transpose(1, 0, 2))
        par = np.zeros((128, 4), np.float32)
        par[:, 0] = P["gdn_a_log"][l][0, h]; par[:, 1] = P["gdn_a_log"][l][1, h]
        par[:, 2] = P["gdn_dt_bias"][l][0, h]; par[:, 3] = P["gdn_dt_bias"][l][1, h]
        ins.append({"qnT": np.ascontiguousarray(qT), "knT": np.ascontiguousarray(kT), "kn": tokmaj(kT),
                    "v": tokmaj(vT), "ab": ab, "par": par, "cst": cst})
    return ins


def build_D(last, n_exp=32):
    nc = bass.Bass("TRN2", target_bir_lowering=False)
    dt = nc.dram_tensor
    xT_d = dt("xT", [1024, NTOK], F32, kind="ExternalInput").ap()
    yc_d = dt("ycT", [256, NTOK], F32, kind="ExternalInput").ap()
    yn_d = dt("ynT", [256, NTOK], F32, kind="ExternalInput").ap()
    oT_d = dt("oT", [512, NTOK], F32, kind="ExternalInput").ap()
    zT_d = dt("zT", [512, NTOK], F32, kind="ExternalInput").ap()
    sm_d = dt("sm", [128, 64], F32, kind="ExternalInput").ap()
    wada_d = dt("wada", [1024, 4096], F32, kind="ExternalInput").ap()
    wout_d = dt("wout", [1024, 1024], F32, kind="ExternalInput").ap()
    wr_d = dt("wr", [1024, 36], F32, kind="ExternalInput").ap()
    rb_d = dt("rb", [128, 36], F32, kind="ExternalInput").ap()
    w1_d = dt("w1", [32, 1024, 512], F32, kind="ExternalInput").ap()
    w3_d = dt("w3", [32, 1024, 512], F32, kind="ExternalInput").ap()
    w2_d = dt("w2", [32, 512, 1024], F32, kind="ExternalInput").ap()
    sel_d = dt("sel", [32, 32, 128], F32, kind="ExternalInput").ap()
    idn_d = dt("idn", [128, 128], F32, kind="ExternalInput").ap()
    out_d = dt("outT", [1024, NTOK], F32, kind="ExternalOutput").ap()
    NB = NTOK // TB
    from contextlib import ExitStack
    with ExitStack() as es:
        def sb(name, shape, dtype):
            return es.enter_context(nc.sbuf_tensor(name, shape, dtype))
        xT_t = sb("xTs", [128, 8, NTOK], F32)
        yh_t = sb("yh", [128, 8, NTOK], BF16)
        wbuf_t = sb("wbuf", [128, 12288], BF16)
        stg_t = sb("stg", [128, 2, 4096], F32)
        hg_t = sb("hg", [128, 2, 4, TB], BF16)
        s1_t = sb("s1", [128, 2, TB], F32)
        gT_t = sb("gT", [32, NTOK], F32)
        sel_t = sb("sels", [32, 32, 128], F32)
        idn_t = sb("idns", [128, 128], F32)
        ones_t = sb("ones", [128, 128], F32)
        sq_t = sb("sq", [128, 2, TB], F32)
        tmp_t = sb("tmp", [128, 2, TB], F32)
        rs_t = sb("rs", [128, TB], F32)
        rstd_t = sb("rstd", [128, TB], F32)
        small = sb("small", [128, 128], F32)
        wr_t = sb("wrs", [128, 8, 36], F32)
        rb_t = sb("rbs", [128, 36], F32)
        rt_t = sb("rt", [128, 2, 160], F32)
        ps = es.enter_context(nc.psum_tensor("ps", [128, 8, 512], F32))
        block = es.enter_context(nc.Block())
        p = Prog(nc)
        dq_i = [0]

        def dq():
            dq_i[0] += 1
            return "sync" if dq_i[0] % 2 else "gpsimd"
        stg_i = [0]

        def stage():
            i = stg_i[0] % 2
            stg_i[0] += 1
            return V(stg_t[:, i], ["stg%d" % i])

        def PS(b):
            return V(ps[:, b, :], ["ps%d" % b])
        ones = V(ones_t, ["ones"]); idn = V(idn_t, ["idn"]); sel = V(sel_t, ["sel"])
        p.memset(ones, 1.0)
        sm = V(small[:, 0:64], ["sm"])
        epsv = V(small[:, 120:121], ["epsv"])
        p.memset(epsv, EPS)
        EPSV[0] = epsv
        p.dma("sync", sm, V(sm_d, []))
        p.dma("sync", idn, V(idn_d, []))
        p.dma("gpsimd", sel, V(sel_d, []))
        p.dma("sync", V(wr_t, ["wr"]), V(wr_d.rearrange("(c p) n -> p c n", p=128), []))
        p.dma("sync", V(rb_t, ["rb"]), V(rb_d, []))
        wr = V(wr_t, ["wr"]); rb = V(rb_t, ["rb"])
        cT = sm[:, 0:8]; nfw = sm[:, 8:16]; fw = sm[:, 16:24]; gnw = sm[:, 24:25]; bada = sm[:, 32:64]
        cond = V(small[:, 64:72], ["cond"]); mod_sb = V(small[:, 72:104], ["mod"]); A2 = V(small[:, 104:112], ["A2"])
        xT = V(xT_t, [])

        def xblk(kc, blk):
            return V(xT_t[:, kc, blk * TB:(blk + 1) * TB], ["x%d" % blk])

        def yblk(kc, blk):
            return V(yh_t[:, kc, blk * TB:(blk + 1) * TB], ["yh%d" % blk])
        allx = ["x%d" % b for b in range(NB)]; ally = ["yh%d" % b for b in range(NB)]
        for kc in range(8):
            p.dma(dq(), V(xT_t[:, kc, :], allx), V(xT_d[kc * 128:(kc + 1) * 128, :], []))
        wst = [V(stg_t[:, i].rearrange("p (c n) -> p c n", c=8), ["stg%d" % i]) for i in range(2)]
        emit_mod(p, nc, None, cT, wada_d, bada, 32, V(ps[:, 0, 0:32], ["ps0"]), wst, cond, mod_sb)
        gate1 = mod_sb[:, 0:8]; B2 = mod_sb[:, 8:16]; gate2 = mod_sb[:, 24:32]
        p.stt(A2, mod_sb[:, 16:24], 1.0, nfw, ALU.add, ALU.mult)
        for (src, k0) in ((yc_d, 0), (yn_d, 2)):
            st = stage()
            p.dma(dq(), st, V(src.rearrange("(c p) n -> p c n", p=128), []))
            p.copy(V(yh_t[:, k0:k0 + 2, :], ally), V(st.ap.rearrange("p (c n) -> p c n", c=2), st.keys), eng="gpsimd")
        for h in range(4):
            st = stage()
            p.dma(dq(), st[:, 0:NTOK], V(oT_d[h * 128:(h + 1) * 128, :], []))
            p.dma(dq(), st[:, NTOK:2 * NTOK], V(zT_d[h * 128:(h + 1) * 128, :], []))
            for blk in range(NB):
                o = st[:, blk * TB:(blk + 1) * TB]; z = st[:, NTOK + blk * TB:NTOK + (blk + 1) * TB]
                sq = V(sq_t[:, blk % 2], ["sq%d" % (blk % 2)]); tmp = V(tmp_t[:, blk % 2], ["tmp%d" % (blk % 2)])
                rs = V(rs_t, ["rs"])
                p.act(sq, o, AF.Square)
                p.mm(PS(1), ones, sq)
                p.act(rs, PS(1), AF.Sqrt, scale=1.0 / 128.0, bias=epsv)
                p.recip(rs, rs)
                p.stt(tmp, o, gnw, rs, ALU.mult, ALU.mult)
                p.act(sq, z, AF.Silu)
                p.tt(yblk(4 + h, blk), tmp, sq, ALU.mult)
        woutb = V(wbuf_t[:, 0:8192].rearrange("p (c n) -> p c n", c=8), ["w1b", "w3b"])
        for half in range(2):
            st = stage()
            p.dma(dq(), st, V(wout_d[:, half * 512:(half + 1) * 512].rearrange("(c p) n -> p c n", p=128), []))
            p.copy(woutb[:, :, half * 512:(half + 1) * 512], V(st.ap.rearrange("p (c n) -> p c n", c=8), st.keys), eng="gpsimd")
        gT = V(gT_t, ["gT"])
        for blk in range(NB):
            for dtl in range(8):
                bank = 1 + dtl % 4
                for kc in range(8):
                    p.mm(PS(bank), woutb[:, kc, dtl * 128:(dtl + 1) * 128], yblk(kc, blk), start=(kc == 0), stop=(kc == 7))
                p.stt(xblk(dtl, blk), PS(bank), gate1[:, dtl:dtl + 1], xblk(dtl, blk), ALU.mult, ALU.add)
            sq = [V(sq_t[:, i], ["sq%d" % i]) for i in range(2)]
            tmp = [V(tmp_t[:, i], ["tmp%d" % i]) for i in range(2)]
            st = stage()
            hF = V(st.ap.rearrange("p (c n) -> p c n", c=8), st.keys)
            xv = V(xT_t, ["x%d" % blk])
            hT = V(yh_t[:, :, blk * TB:(blk + 1) * TB], ["yh%d" % blk])
            emit_hmix_block(p, xv, blk, ones, sq, PS(0), V(rs_t, ["rs"]), V(rstd_t, ["rstd"]), tmp, A2, B2, hT, hF=hF)
            for tt_ in range(4):
                L = V(ps[:, 5 + tt_ % 2, 0:36], ["ps%d" % (5 + tt_ % 2)])
                for kc in range(8):
                    p.mm(L, hF[:, kc, tt_ * 128:(tt_ + 1) * 128], wr[:, kc, :], start=(kc == 0), stop=(kc == 7))
                R = V(rt_t[:, tt_ % 2], ["rt%d" % (tt_ % 2)])
                Lb = R[:, 0:36]; lg = R[:, 0:4]; le = R[:, 4:36]
                m = R[:, 36:37]; negm = R[:, 37:38]; ohg = R[:, 40:44]; e4 = R[:, 44:48]; ssum = R[:, 38:39]
                pgt = R[:, 39:40]; pen = R[:, 48:52]; lem = R[:, 52:84]; m1 = R[:, 84:85]; oh1 = R[:, 88:120]
                lem2 = R[:, 120:152]; m2 = R[:, 85:86]; dd = R[:, 86:87]; ed = R[:, 87:88]
                c1 = R[:, 152:153]; c2 = R[:, 153:154]; den = R[:, 154:155]
                p.tt(Lb, L, rb, ALU.add)
                p.reduce(m, lg, ALU.max)
                p.ts(ohg, lg, m, None, ALU.is_equal)
                p.ts(negm, m, -1.0, None, ALU.mult)
                p.act(e4, lg, AF.Exp, bias=negm)
                p.reduce(ssum, e4, ALU.add)
                p.recip(pgt, ssum)
                p.ts(pen, ohg, 1.0, 1e30, ALU.subtract, ALU.mult)
                for g in range(4):
                    p.ts(lem[:, g * 8:(g + 1) * 8], le[:, g * 8:(g + 1) * 8], pen[:, g:g + 1], None, ALU.add)
                p.reduce(m1, lem, ALU.max)
                p.ts(oh1, lem, m1, None, ALU.is_equal)
                p.stt(lem2, oh1, -1e30, lem, ALU.mult, ALU.add)
                p.reduce(m2, lem2, ALU.max)
                p.tt(dd, m2, m1, ALU.subtract)
                p.act(ed, dd, AF.Exp)
                p.ts(den, ed, 1.0, None, ALU.add)
                p.recip(den, den)
                p.tt(c1, den, pgt, ALU.mult)
                p.tt(c2, c1, ed, ALU.mult)
                p.ts(lem2, lem2, m2, None, ALU.is_equal)
                p.ts(oh1, oh1, c1, None, ALU.mult)
                p.stt(oh1, lem2, c2, oh1, ALU.mult, ALU.add)
                gp = V(ps[0:32, 7, 0:128], ["ps7"])
                p.tr(gp, oh1, idn)
                c0 = blk * TB + tt_ * 128
                p.copy(gT[:, c0:c0 + 128], gp, eng="scalar")
        w1b = V(wbuf_t[:, 0:4096].rearrange("p (c n) -> p c n", c=8), ["w1b"])
        w3b = V(wbuf_t[:, 4096:8192].rearrange("p (c n) -> p c n", c=8), ["w3b"])
        w2b = V(wbuf_t[:, 8192:12288].rearrange("p (c n) -> p c n", c=4), ["w2b"])
        it = 0
        for e in range(n_exp):
            for (dst, src, cc) in ((w1b, w1_d[e], 8), (w3b, w3_d[e], 8), (w2b, w2_d[e], 4)):
                st = stage()
                p.dma(dq(), st, V(src.rearrange("(c p) n -> p c n", p=128), []))
                p.copy(dst, V(st.ap.rearrange("p (c n) -> p c n", c=cc), st.keys), eng="gpsimd")
            for blk in range(NB):
                bsl = slice(blk * TB, (blk + 1) * TB)
                hT = V(yh_t[:, :, bsl], ["yh%d" % blk])
                gb = PS(4)
                p.mm(gb, sel[:, e, :], gT[:, bsl])
                hg = V(hg_t[:, it % 2], ["hg%d" % (it % 2)])
                for ht in range(4):
                    h1 = PS(ht % 2); h3 = PS(2 + ht % 2)
                    for kc in range(8):
                        p.mm(h1, w1b[:, kc, ht * 128:(ht + 1) * 128], hT[:, kc, :], start=(kc == 0), stop=(kc == 7))
                    for kc in range(8):
                        p.mm(h3, w3b[:, kc, ht * 128:(ht + 1) * 128], hT[:, kc, :], start=(kc == 0), stop=(kc == 7))
                    s1 = V(s1_t[:, ht % 2], ["s1%d" % (ht % 2)])
                    p.act(s1, h1, AF.Silu)
                    p.tt(s1, s1, gb, ALU.mult)
                    p.tt(hg[:, ht, :], h3, s1, ALU.mult)
                for dtl in range(8):
                    yb = PS(5 + dtl % 3)
                    for ht in range(4):
                        p.mm(yb, w2b[:, ht, dtl * 128:(dtl + 1) * 128], hg[:, ht, :], start=(ht == 0), stop=(ht == 3))
                    p.stt(xblk(dtl, blk), yb, gate2[:, dtl:dtl + 1], xblk(dtl, blk), ALU.mult, ALU.add)
                it += 1
        for blk in range(NB):
            bsl = slice(blk * TB, (blk + 1) * TB)
            if last:
                xv = V(xT_t, ["x%d" % blk])
                for kc in range(8):
                    s = V(sq_t[:, kc % 2], ["sq%d" % (kc % 2)])
                    p.act(s, xv[:, kc, bsl], AF.Square)
                    p.mm(PS(0), ones, s, start=(kc == 0), stop=(kc == 7))
                rs = V(rs_t, ["rs"]); rstd = V(rstd_t, ["rstd"])
                p.act(rs, PS(0), AF.Sqrt, scale=1.0 / 1024.0, bias=epsv)
                p.recip(rstd, rs)
                for kc in range(8):
                    p.stt(xblk(kc, blk), xblk(kc, blk), fw[:, kc:kc + 1], rstd, ALU.mult, ALU.mult)
            for kc in range(8):
                p.dma(dq(), V(out_d[kc * 128:(kc + 1) * 128, bsl], []), xblk(kc, blk))
        p.finish("sync")
        p.emit(block)
    return nc


def _lay_pc(v, n):
    return np.ascontiguousarray(np.asarray(v).reshape(n, 128).T)


def prep_D(xT_full, ycT_full, ynaT_full, oT_full, projT, P, l):
    sel = np.zeros((32, 32, 128), np.float32)
    for e in range(32):
        sel[e, e, :] = 1.0
    idn = np.eye(128, dtype=np.float32)
    wr = np.ascontiguousarray(np.concatenate([P["router_group_w"][l], P["router_expert_w"][l]], 1))
    rbv = np.concatenate([P["router_group_b"][l], P["router_expert_b"][l]])
    rb = np.ascontiguousarray(np.broadcast_to(rbv[None, :], (128, 36))).astype(np.float32)
    wada = np.ascontiguousarray(P["w_ada"][l][:, 2048:6144])
    ins = []
    for core in range(8):
        b = core // 4
        tk = slice(core * NTOK, (core + 1) * NTOK)
        sm = np.zeros((128, 64), np.float32)
        sm[:, 0:8] = _lay_pc(P["c"][b], 8)
        sm[:, 8:16] = _lay_pc(P["norm_ffn_w"][l], 8)
        sm[:, 16:24] = _lay_pc(P["final_norm_w"], 8)
        sm[:, 24] = P["gdn_norm_w"][l]
        sm[:, 32:64] = _lay_pc(P["b_ada"][l][2048:6144], 32)
        ins.append({
            "xT": np.ascontiguousarray(xT_full[:, tk]), "ycT": np.ascontiguousarray(ycT_full[:, tk]),
            "ynT": np.ascontiguousarray(ynaT_full[:, tk]), "oT": np.ascontiguousarray(oT_full[:, tk]),
            "zT": np.ascontiguousarray(projT[3072:3584, tk]), "sm": sm, "wada": wada,
            "wout": P["w_out"][l], "wr": wr, "rb": rb,
            "w1": P["expert_w1"][l], "w3": P["expert_w3"][l], "w2": P["expert_w2"][l], "sel": sel, "idn": idn,
        })
    return ins


def prep_A(xT_full, P, l):
    ins = []
    wada = np.ascontiguousarray(P["w_ada"][l][:, 0:2048])
    for core in range(8):
        b = core // 4
        ins.append({
            "xT": np.ascontiguousarray(xT_full[:, core * NTOK:(core + 1) * NTOK]),
            "cT": _lay_pc(P["c"][b], 8),
            "wada": wada,
            "bada": _lay_pc(P["b_ada"][l][0:2048], 16),
            "nw": _lay_pc(P["norm_mix_w"][l], 8),
            "win": P["w_in"][l],
        })
    return ins


TSEQ = 8192
NSH = 4


def _phase(nc):
    from contextlib import ExitStack
    return ExitStack()


def phase_A(nc, p, xsrc, W, S, l):
    with _phase(nc) as es:
        def sb(name, shape, dtype):
            return es.enter_context(nc.sbuf_tensor("A%d_%s" % (l, name), shape, dtype))
        xT = V(sb("xTs", [128, 8, NTOK], F32), ["xT"])
        wst_t = sb("wst", [128, 2, 8, 512], F32)
        wst = [V(wst_t[:, i], ["wst%d" % i]) for i in range(2)]
        winb = sb("winb", [128, 8, 3600], BF16)
        hT_t = sb("hT", [128, 2, 8, TB], BF16)
        sq_t = sb("sq", [128, 2, TB], F32)
        tmp_t = sb("tmp", [128, 2, TB], F32)
        rs = V(sb("rs", [128, TB], F32), ["rs"])
        rstd_b = V(sb("rstd", [128, TB], F32), ["rstd"])
        stage_t = sb("stage", [128, 4, TB], F32)
        tk_t = sb("tk", [128, 2, 272], F32)
        small = sb("small", [128, 64], F32)
        ones = V(sb("ones", [128, 128], F32), ["ones"])
        epsv = V(sb("epsv", [128, 1], F32), ["epsv"])
        EPSV[0] = epsv
        ps = es.enter_context(nc.psum_tensor("A%d_ps" % l, [128, 8, 512], F32))
        block = es.enter_context(nc.Block())
        cT = V(small[:, 0:8], ["cT"]); cond = V(small[:, 8:16], ["cond"])
        bada = V(small[:, 16:32], ["bada"]); mod_sb = V(small[:, 32:48], ["mod"])
        nw = V(small[:, 48:56], ["nw"]); A1 = V(small[:, 56:64], ["A1"])
        modps = V(ps[:, 0, 0:16], ["ps0"])
        ss = V(ps[:, 1, :], ["ps1"])
        p.memset(ones, 1.0)
        p.memset(epsv, EPS)
        p.dma("sync", cT, V(W["cT"], []))
        p.dma("sync", bada, V(W["badaA%d" % l], []))
        p.dma("sync", nw, V(W["nw%d" % l], []))
        emit_mod(p, nc, None, cT, W["wada%d" % l][:, 0:2048], bada, 16, modps, wst, cond, mod_sb)
        B1 = mod_sb[:, 0:8]
        p.stt(A1, mod_sb[:, 8:16], 1.0, nw, ALU.add, ALU.mult)
        win_d = W["win%d" % l]
        ci = 0
        for c0 in range(0, 3600, 512):
            cw = min(512, 3600 - c0)
            buf = wst[ci % 2]
            p.dma("sync" if ci % 2 == 0 else "gpsimd", buf[:, :, 0:cw],
                  V(win_d[:, c0:c0 + cw].rearrange("(c p) n -> p c n", p=128), []))
            for kc in range(8):
                dst = V(winb[:, kc, c0:c0 + cw], ["winb%d" % ci])
                p.copy(dst, buf[:, kc, 0:cw], eng=("gpsimd" if kc % 2 else "vector"))
            ci += 1
        nev = 0
        ntk = 0
        for s in range(NSH):
            t0 = s * NTOK
            for kc in range(8):
                p.dma("gpsimd" if kc % 2 else "sync", xT[:, kc, :], V(xsrc[kc * 128:(kc + 1) * 128, t0:t0 + NTOK], []))
            for blk in range(NTOK // TB):
                hT = V(hT_t[:, blk % 2], ["hT%d" % (blk % 2)])
                sq = [V(sq_t[:, i], ["sq%d" % i]) for i in range(2)]
                tmp = [V(tmp_t[:, i], ["tmp%d" % i]) for i in range(2)]
                emit_hmix_block(p, xT, blk, ones, sq, ss, rs, rstd_b, tmp, A1, B1, hT)
                for j in range(29):
                    rows = min(128, 3600 - j * 128)
                    bank = 2 + (nev % 5)
                    pj = V(ps[0:rows, bank, :], ["ps%d" % bank])
                    cj = (j * 128) // 512
                    for kc in range(8):
                        p.mm(pj, V(winb[:, kc, j * 128:j * 128 + rows], ["winb%d" % cj]), hT[:, kc, :],
                             start=(kc == 0), stop=(kc == 7))
                    st = V(stage_t[0:rows, nev % 4, :], ["stage%d" % (nev % 4)])
                    p.copy(st, pj, eng=("vector" if nev % 2 == 0 else "scalar"))
                    p.dma("sync" if nev % 2 == 0 else "gpsimd",
                          V(S["projT"][j * 128:j * 128 + rows, t0 + blk * TB:t0 + (blk + 1) * TB], []), st)
                    nev += 1
                for tt_ in range(4):
                    pt = V(ps[:, 7, 0:272], ["ps7"])
                    tsl = slice(tt_ * 128, (tt_ + 1) * 128)
                    for kc in range(8):
                        p.mm(pt[:, 0:256], hT[:, kc, tsl], V(winb[:, kc, 1280:1536], ["winb2"]),
                             start=(kc == 0), stop=(kc == 7))
                    for kc in range(8):
                        p.mm(pt[:, 256:272], hT[:, kc, tsl], V(winb[:, kc, 3584:3600], ["winb7"]),
                             start=(kc == 0), stop=(kc == 7))
                    tk = V(tk_t[:, ntk % 2], ["tk%d" % (ntk % 2)])
                    ntk += 1
                    p.copy(tk, pt, eng="vector")
                    r0 = t0 + blk * TB + tt_ * 128
                    p.dma("sync", V(S["nvtok"][r0:r0 + 128, :], []), tk[:, 0:256])
                    p.dma("gpsimd", V(S["abtok"][r0:r0 + 128, :], []), tk[:, 256:272])
        p.finish("sync")
        p.emit(block)


def phase_B(nc, p, W, S, l):
    with _phase(nc) as es:
        def sb(name, shape, dtype):
            return es.enter_context(nc.sbuf_tensor("B%d_%s" % (l, name), shape, dtype))
        NST = 3
        stg_t = sb("stg", [128, NST, 4096], F32)
        u_t = sb("u", [128, NTOK + 2], F32)
        acc_t = sb("acc", [128, NTOK], F32)
        sil_t = sb("sil", [128, 2, NTOK], F32)
        sq_t = sb("sq", [128, 2, TB], F32)
        rs_t = sb("rs", [128, 2, TB], F32)
        ones_t = sb("ones", [128, 128], F32)
        idn_t = sb("idn", [128, 128], F32)
        wsm = sb("wsm", [128, 64], F32)
        qb_t = sb("qb", [128, 2, NTOK], BF16)
        kb_t = sb("kb", [128, 2, NTOK], BF16)
        ksp_t = sb("ksp", [128, 2, NSPEC * 512], BF16)
        ve_t = sb("ve", [128, 16, 4, 65], BF16)
        vo_t = sb("vo", [128, 16, 4, 65], BF16)
        vs_t = sb("vs", [128, NSPEC * 4, 4, 65], BF16)
        bias_t = sb("biass", [128, 9, 1024], F32)
        maskf_t = sb("maskfs", [128, 1024], F32)
        sc_t = sb("sc", [128, 2, 512], F32)
        pT_t = sb("pT", [128, 2, 512], BF16)
        pos_t = sb("pos", [64, 2, 260], F32)
        rden_t = sb("rden", [64, 2, 4], F32)
        yst_t = sb("yst", [64, 2, 256], F32)
        ytr_t = sb("ytr", [128, 2, 2, 64], F32)
        ps = es.enter_context(nc.psum_tensor("B%d_ps" % l, [128, 8, 512], F32))
        block = es.enter_context(nc.Block())
        stg_i = [0]

        def stage():
            i = stg_i[0] % NST
            stg_i[0] += 1
            return V(stg_t[:, i], ["stg%d" % i])
        dq_i = [0]

        def dq():
            dq_i[0] += 1
            return "sync" if dq_i[0] % 2 else "gpsimd"
        ones = V(ones_t, ["ones"]); idn = V(idn_t, ["idn"])
        p.memset(ones, 1.0)
        p.dma("sync", idn, V(W["idn"], []))
        cw = V(wsm[:, 0:6], ["cw"])
        gw = V(wsm[:, 6:42], ["gw"])
        epsv = V(wsm[:, 42:43], ["epsv"])
        p.memset(epsv, EPS)
        p.dma("sync", cw, V(W["cw%d" % l].rearrange("p a b -> p (a b)"), []))
        p.dma("sync", gw, V(W["gw%d" % l].rearrange("p a b -> p (a b)"), []))
        u = V(u_t, ["u"]); acc = V(acc_t, ["acc"])
        maskf = V(maskf_t, ["maskf"])
        p.dma("sync", maskf, V(W["maskf"], []))
        projT = S["projT"]; nvtok = S["nvtok"]

        def conv3(src, wv, base):
            p.ts(acc, src[:, 0:NTOK], wv[:, base:base + 1], None, ALU.mult)
            p.stt(acc, src[:, 1:NTOK + 1], wv[:, base + 1:base + 2], acc, ALU.mult, ALU.add)
            p.stt(acc, src[:, 2:NTOK + 2], wv[:, base + 2:base + 3], acc, ALU.mult, ALU.add)

        def load_halo(st, row0, t0):
            lo = t0 - 1; hi = t0 + NTOK + 1
            a = 0; b = NTOK + 2
            if lo < 0:
                p.memset(st[:, 0:1], 0.0, eng="gpsimd")
                lo = 0; a = 1
            if hi > TSEQ:
                p.memset(st[:, NTOK + 1:NTOK + 2], 0.0, eng="gpsimd")
                hi = TSEQ; b = NTOK + 1
            p.dma(dq(), st[:, a:b], V(projT[row0:row0 + 128, lo:hi], []))
        ssb = 0
        for s in range(NSH):
            t0 = s * NTOK
            tsl = slice(t0, t0 + NTOK)
            for ct in range(2):
                bufs = []
                for g in range(3):
                    st = stage()
                    load_halo(st, g * 256 + ct * 128, t0)
                    bufs.append(st)
                cb, cc, cx = bufs
                p.tt(u, cc[:, 0:NTOK + 2], cx[:, 0:NTOK + 2], ALU.mult)
                conv3(u, cw, ct * 3)
                yo = V(sil_t[:, ct], ["sil%d" % ct])
                p.tt(yo, acc, cb[:, 1:NTOK + 1], ALU.mult)
                p.dma(dq(), V(S["ycT"][ct * 128:(ct + 1) * 128, tsl], []), yo)
            for ct in range(12):
                st = stage()
                load_halo(st, 1536 + ct * 128, t0)
                conv3(st, gw, ct * 3)
                so = V(sil_t[:, ct % 2], ["sil%d" % (ct % 2)])
                p.act(so, acc, AF.Silu)
                if ct < 8:
                    qscale = (128.0 ** -0.5) if ct < 4 else 1.0
                    for blk in range(NTOK // TB):
                        sl = slice(blk * TB, (blk + 1) * TB)
                        sq = V(sq_t[:, ssb % 2], ["sq%d" % (ssb % 2)])
                        rs = V(rs_t[:, ssb % 2], ["rs%d" % (ssb % 2)])
                        bank = ssb % 2
                        ss = V(ps[:, bank, :], ["ps%d" % bank])
                        ssb += 1
                        p.act(sq, so[:, sl], AF.Square)
                        p.mm(ss, ones, sq)
                        p.act(rs, ss, AF.Sqrt, bias=epsv)
                        p.recip(rs, rs)
                        p.stt(so[:, sl], so[:, sl], qscale, rs, ALU.mult, ALU.mult)
                p.dma(dq(), V(S["gqkvn"][ct * 128:(ct + 1) * 128, tsl], []), so)
            qb = V(qb_t, ["qb"]); kb = V(kb_t, ["kb"]); ksp = V(ksp_t, ["ksp"])
            for i in range(2):
                for (dst, r0) in ((qb, 768), (kb, 1024)):
                    st = stage()
                    p.dma(dq(), st[:, 0:NTOK], V(projT[r0 + i * 128:r0 + (i + 1) * 128, tsl], []))
                    p.copy(dst[:, i, :], st[:, 0:NTOK], eng="gpsimd")
            row0 = s * 32
            spec = []
            for si, r in enumerate(SPEC_ROWS):
                R = row0 + r
                rs_ = min(max(R - 4, 0), 120)
                spec.append(rs_)
            for i in range(2):
                st = stage()
                for si, rs_ in enumerate(spec):
                    p.dma(dq(), st[:, si * 512:(si + 1) * 512], V(projT[1024 + i * 128:1024 + (i + 1) * 128, rs_ * 64:rs_ * 64 + 512], []))
                p.copy(ksp[:, i, :], st, eng="gpsimd")
            ve = V(ve_t, ["ve"]); vo = V(vo_t, ["vo"]); vs = V(vs_t, ["vs"])
            for dst in (ve, vo, vs):
                p.memset(dst[:, :, :, 64:65], 1.0, eng="gpsimd")
            st = stage()
            p.dma(dq(), st, V(nvtok[t0:t0 + NTOK, :].rearrange("(n p) c -> p n c", p=128), []))
            p.copy(ve[:, :, :, 0:64], V(st.ap.rearrange("p (a h d) -> p a h d", a=16, h=4), st.keys), eng="vector")
            st = stage()
            p.dma(dq(), st[:, 0:15 * 256], V(nvtok[t0 + 64:t0 + 64 + 15 * 128, :].rearrange("(n p) c -> p n c", p=128), []))
            p.copy(vo[:, 0:15, :, 0:64], V(st.ap[:, 0:15 * 256].rearrange("p (a h d) -> p a h d", a=15, h=4), st.keys), eng="vector")
            for half in range(2):
                st = stage()
                for q4 in range(4):
                    si = half * 4 + q4
                    rs_ = spec[si]
                    p.dma(dq(), st[:, q4 * 1024:(q4 + 1) * 1024],
                          V(nvtok[rs_ * 64:rs_ * 64 + 512, :].rearrange("(n p) c -> p n c", p=128), []))
                p.copy(vs[:, half * 16:(half + 1) * 16, :, 0:64],
                       V(st.ap.rearrange("p (a h d) -> p a h d", a=16, h=4), st.keys), eng="vector")
            bias = V(bias_t, ["bias"])
            for i in range(9):
                p.dma(dq(), bias[:, i, :], V(W["rpbg%d" % l][s, :, i, :], []))
            for i in range(9):
                p.stt(bias[:, i, :], bias[:, i, :], 8.0, maskf, ALU.mult, ALU.add)
            for r in range(32):
                par = r % 2
                if r in SPEC_ROWS:
                    sidx = SPEC_ROWS.index(r)
                    bi = 1 + sidx

                    def ktile(tile, off, t, sidx=sidx):
                        return ksp[off:off + 64, tile, sidx * 512 + t * 128:sidx * 512 + (t + 1) * 128]

                    def vtile(t, h, sidx=sidx):
                        return vs[:, sidx * 4 + t, h, :]
                else:
                    bi = 0
                    lr = r - 4

                    def ktile(tile, off, t, lr=lr):
                        return kb[off:off + 64, tile, (lr + 2 * t) * 64:(lr + 2 * t + 2) * 64]
                    if lr % 2 == 0:
                        def vtile(t, h, lr=lr):
                            return ve[:, lr // 2 + t, h, :]
                    else:
                        def vtile(t, h, lr=lr):
                            return vo[:, (lr - 1) // 2 + t, h, :]
                pob = 6 + par
                po = V(ps[0:64, pob, 0:260], ["ps%d" % pob])
                for hl in range(2):
                    off = 64 * hl
                    bank = 2 + 2 * par + hl
                    psc = V(ps[:, bank, :], ["ps%d" % bank])
                    for j in range(2):
                        for t in range(4):
                            p.mm(psc[:, (j * 4 + t) * 64:(j * 4 + t + 1) * 64], ktile(j, off, t),
                                 qb[off:off + 64, j, r * 64:(r + 1) * 64])
                    sc = V(sc_t[:, hl], ["sc%d" % hl])
                    bview = V(bias.ap[:, bi, :].rearrange("p (h x) -> p h x", h=4)[:, hl::2, :], bias.keys)
                    p.tt(V(sc.ap.rearrange("p (h x) -> p h x", h=2), sc.keys),
                         V(psc.ap.rearrange("p (h x) -> p h x", h=2), psc.keys), bview, ALU.add)
                    pT = V(pT_t[:, hl], ["pT%d" % hl])
                    p.act(pT, sc, AF.Exp, scale=0.125)
                    for j in range(2):
                        h = 2 * j + hl
                        for t in range(4):
                            p.mm(po[:, h * 65:(h + 1) * 65], pT[:, (j * 4 + t) * 64:(j * 4 + t + 1) * 64], vtile(t, h),
                                 start=(t == 0), stop=(t == 3))
                pos = V(pos_t[:, par], ["pos%d" % par])
                p.copy(pos, po, eng="scalar")
                rden = V(rden_t[:, par], ["rden%d" % par])
                posv = V(pos.ap.rearrange("p (h d) -> p h d", h=4), pos.keys)
                p.recip(rden, V(posv.ap[:, :, 64:65].rearrange("p h o -> p (h o)"), pos.keys))
                yst = V(yst_t[:, par], ["yst%d" % par])
                for h in range(4):
                    p.ts(yst[:, h * 64:(h + 1) * 64], posv[:, h, 0:64], rden[:, h:h + 1], None, ALU.mult, eng="gpsimd")
                ytr = V(ytr_t[:, par], ["ytr%d" % par])
                for j in range(2):
                    pt = V(ps[:, 0 + j, 0:64], ["ps%d" % j])
                    p.tr(pt, yst[:, j * 128:(j + 1) * 128], idn[0:64, 0:64])
                    p.copy(ytr[:, j, :], pt, eng=("vector" if j else "scalar"))
                    p.dma(dq(), V(S["ynaT"][j * 128:(j + 1) * 128, t0 + r * 64:t0 + (r + 1) * 64], []), ytr[:, j, :])
        p.finish("sync")
        p.emit(block)


def phase_C(nc, p, W, S, l):
    T = TSEQ
    with _phase(nc) as es:
        def sb(name, shape, dtype):
            return es.enter_context(nc.sbuf_tensor("C%d_%s" % (l, name), shape, dtype))
        cst_t = sb("cst_s", [128, 11, 128], F32)
        cstb_t = sb("cstb", [128, 128], BF16)
        stg_t = sb("stg", [128, 2, 2048], F32)
        qnT_t = sb("qnTb", [128, T], BF16)
        knT_t = sb("knTb", [128, T], BF16)
        vT_t = sb("vTb", [128, T], BF16)
        kn_t = sb("knb", [128, NSC, 128], BF16)
        v_t = sb("vb", [128, NSC, 128], BF16)
        oacc_t = sb("oacc", [128, NSC, 128], F32)
        ab_t = sb("abs", [128, NSC, 16], F32)
        par_t = sb("pars", [128, 8], F32)
        NPS = 12
        pre_t = sb("pre", [128, 2, NPS, NSC], F32)
        S_t = sb("S", [128, 2, 128], F32)
        Sb_t = sb("Sb", [128, 2, 128], BF16)
        vnew_t = sb("vnew", [128, 2, 128], BF16)
        NF = 6; NB = 16
        wf_t = sb("wf", [128, 2, 2, NF, 128], F32)
        wb_t = sb("wb", [128, 2, 2, NB, 128], BF16)
        ps = es.enter_context(nc.psum_tensor("C%d_ps" % l, [128, 8, 512], F32))
        block = es.enter_context(nc.Block())
        cst = V(cst_t, ["cst"])

        def C(i):
            return cst[:, i, :]
        identb = V(cstb_t, ["cstb"])
        dq_i = [0]

        def dq():
            dq_i[0] += 1
            return "sync" if dq_i[0] % 2 else "gpsimd"
        p.dma("sync", cst, V(W["cst"], []))
        p.copy(identb, C(C_IDENT))
        ab = V(ab_t, ["ab"]); par = V(par_t, ["par"])
        p.dma("sync", ab, V(S["abtok"].rearrange("(n p) c -> p n c", p=128), []))
        qnT = V(qnT_t, ["qnT"]); knT = V(knT_t, ["knT"]); vT = V(vT_t, ["vT"])
        kn = V(kn_t, ["kn"]); vv = V(v_t, ["vv"])
        pre = V(pre_t, ["pre"])
        mA = V(cst.ap[:, C_SELA, 0:1], cst.keys)
        mB = V(cst.ap[:, C_SELB, 0:1], cst.keys)
        ev = [0]

        def evac(out, in_):
            ev[0] += 1
            p.copy(out, in_, eng=("scalar" if ev[0] % 2 else "vector"))
        si = 0
        for h in range(4):
            p.dma("sync", par[:, 0:4], V(W["par%d" % l][h], []))
            for (dst, r0) in ((qnT, h * 128), (knT, 512 + h * 128), (vT, 1024 + h * 128)):
                for c4 in range(4):
                    st = V(stg_t[:, si % 2], ["stg%d" % (si % 2)])
                    si += 1
                    p.dma(dq(), st, V(S["gqkvn"][r0:r0 + 128, c4 * 2048:(c4 + 1) * 2048], []))
                    p.copy(dst[:, c4 * 2048:(c4 + 1) * 2048], st, eng=("gpsimd" if c4 % 2 else "vector"))
            for sc in range(NSC):
                tok = slice(sc * 128, (sc + 1) * 128)
                bk = V(ps[:, sc % 2, :], ["ps%d" % (sc % 2)])
                p.mm(bk[:, 0:128], knT[:, tok], identb)
                p.mm(bk[:, 128:256], vT[:, tok], identb)
                evac(kn[:, sc, :], bk[:, 0:128])
                evac(vv[:, sc, :], bk[:, 128:256])
            p.act(par[:, 4:6], par[:, 0:2], AF.Exp)
            p.ts(par[:, 6:8], par[:, 4:6], -1.0, None, ALU.mult)
            for d in range(2):
                def S_(i, d=d):
                    return V(pre_t[:, d, i, :], ["pre%d_%d" % (d, i)])
                a_v = V(ab_t[:, :, 4 * d + h], ["ab"]); b_v = V(ab_t[:, :, 8 + 4 * d + h], ["ab"])
                p.ts(S_(11), a_v, par[:, 2 + d:3 + d], None, ALU.add)
                p.act(S_(11), S_(11), AF.Exp)
                p.act(S_(0), S_(11), AF.Ln, bias=1.0)
                p.ts(S_(0), S_(0), par[:, 6 + d:7 + d], None, ALU.mult)
                p.act(S_(1), b_v, AF.Sigmoid)
                tri = C(C_TRIF if d == 0 else C_TRIB)
                pb = V(ps[:, 0, :], ["ps0"])
                p.mm(pb[:, 0:NSC], tri, S_(0))
                p.mm(pb[:, 64:64 + NSC], C(C_BLK), S_(0))
                p.mm(pb[:, 128:128 + NSC], C(C_SELA), S_(0))
                p.mm(pb[:, 192:192 + NSC], C(C_SELB), S_(0))
                p.copy(S_(2), pb[:, 0:NSC])
                p.copy(S_(3), pb[:, 64:64 + NSC])
                p.act(S_(9), pb[:, 128:128 + NSC], AF.Exp)
                p.act(S_(10), pb[:, 192:192 + NSC], AF.Exp)
                p.act(S_(4), S_(2), AF.Exp)
                p.tt(S_(5), S_(1), S_(4), ALU.mult)
                p.tt(S_(11), S_(3), S_(2), ALU.subtract)
                p.act(S_(11), S_(11), AF.Exp)
                p.ts(S_(6), S_(11), mA, None, ALU.mult)
                p.ts(S_(7), S_(11), mB, None, ALU.mult)
                p.ts(S_(8), S_(1), -1.0, None, ALU.mult)
            for d in range(2):
                p.memset(V(S_t[:, d], ["S%d" % d]), 0.0)
                p.memset(V(Sb_t[:, d], ["Sb%d" % d]), 0.0)
                p.memset(V(vnew_t[:, d], ["vn%d" % d]), 0.0)

            def bufs(d, step):
                par_ = step % 2

                def F(i):
                    return V(wf_t[:, d, par_, i], ["wf%d%d_%d" % (d, par_, i)])

                def B(i):
                    return V(wb_t[:, d, par_, i], ["wb%d%d_%d" % (d, par_, i)])
                return F, B

            def sl(bk, i):
                return bk[:, i * 128:(i + 1) * 128]

            def prep_gen(d, sc, step):
                tok = slice(sc * 128, (sc + 1) * 128)
                F, B = bufs(d, step)

                def sc_(i):
                    return V(pre_t[:, d, i, sc:sc + 1], ["pre%d_%d" % (d, i)])
                b0 = V(ps[:, 4 * d + 0, :], ["ps%d" % (4 * d)])
                b1 = V(ps[:, 4 * d + 1, :], ["ps%d" % (4 * d + 1)])
                Gdiag = F(0)
                p.ts(Gdiag, C(C_IDENT), sc_(2), None, ALU.mult); yield
                p.mm(sl(b0, 0), C(C_ONES), Gdiag, start=True, stop=False)
                p.mm(sl(b0, 0), C(C_IDENT), C(C_MSF if d == 0 else C_MSB), start=False, stop=True)
                p.mm(sl(b0, 1), C(C_ONES), Gdiag, start=True, stop=False)
                p.mm(sl(b0, 1), C(C_IDENT), C(C_MIF if d == 0 else C_MIB), start=False, stop=True)
                p.mm(sl(b0, 2), knT[:, tok], knT[:, tok])
                p.mm(sl(b0, 3), knT[:, tok], qnT[:, tok]); yield
                p.ts(F(1), sl(b0, 0), sc_(2), 0.0, ALU.subtract, ALU.max); yield
                p.act(F(1), F(1), AF.Exp, scale=-1.0); yield
                p.ts(F(2), sl(b0, 1), sc_(2), 0.0, ALU.subtract, ALU.min); yield
                p.act(F(2), F(2), AF.Exp); yield
                A = B(0)
                p.stt(A, sl(b0, 2), sc_(8), F(1), ALU.mult, ALU.mult); yield
                p.tt(B(1), sl(b0, 3), F(2), ALU.mult); yield
                p.mm(sl(b1, 0), A, identb); yield
                N = B(2)
                evac(N, sl(b1, 0)); yield
                P = B(3)
                p.tt(P, N, identb, ALU.add); yield
                M, Mt = N, A
                slot = 1
                for j in range(1, 6):
                    Mn = B(4 + (j % 2) * 2); Mtn = B(5 + (j % 2) * 2)
                    s_mt = sl(b1, slot % 4); slot += 1
                    p.mm(s_mt, M, Mt); yield
                    evac(Mtn, s_mt); yield
                    if j < 5:
                        s_m = sl(b1, slot % 4); slot += 1
                        p.mm(s_m, Mt, M); yield
                        evac(Mn, s_m); yield
                    s_p = sl(b1, slot % 4); slot += 1
                    p.mm(s_p, Mtn, P); yield
                    Pn = B(8 + (j % 2))
                    p.tt(Pn, s_p, P, ALU.add); yield
                    P = Pn
                    M, Mt = Mn, Mtn
                Vb = B(10); Kbg = B(11); kdA = B(12); kdB = B(13)
                p.ts(Vb, vv[:, sc, :], sc_(1), None, ALU.mult, eng="gpsimd")
                p.ts(Kbg, kn[:, sc, :], sc_(5), None, ALU.mult, eng="gpsimd")
                p.ts(kdA, kn[:, sc, :], sc_(6), None, ALU.mult, eng="gpsimd")
                p.ts(kdB, kn[:, sc, :], sc_(7), None, ALU.mult, eng="gpsimd"); yield
                p.mm(sl(b1, 2), P, Vb)
                p.mm(sl(b1, 3), Kbg, P); yield
                evac(F(3), sl(b1, 2)); yield
                evac(B(14), sl(b1, 3)); yield

            def scan_gen(d, sc, step):
                F, B = bufs(d, step)

                def sc_(i):
                    return V(pre_t[:, d, i, sc:sc + 1], ["pre%d_%d" % (d, i)])
                b2 = V(ps[:, 4 * d + 2, :], ["ps%d" % (4 * d + 2)])
                b3 = V(ps[:, 4 * d + 3, :], ["ps%d" % (4 * d + 3)])
                u_sb = F(3); wT = B(14); intraT = B(1); kdA = B(12); kdB = B(13)
                St = V(S_t[:, d], ["S%d" % d]); Sb = V(Sb_t[:, d], ["Sb%d" % d]); vn = V(vnew_t[:, d], ["vn%d" % d])
                oacc = V(oacc_t[:, sc, :], ["oacc%d" % sc])
                first = (step < NSC // 2)
                for half in ((0, 1) if d == 0 else (1, 0)):
                    rows = slice(half * 64, (half + 1) * 64)
                    ctok = slice(sc * 128 + half * 64, sc * 128 + (half + 1) * 64)
                    p.mm(sl(b3, 0)[rows], wT[:, rows], Sb); yield
                    p.tt(vn[rows], u_sb[rows], sl(b3, 0)[rows], ALU.subtract); yield
                    p.mm(sl(b3, 1), kdA if half == 0 else kdB, vn)
                    p.mm(sl(b2, 2)[rows], qnT[:, ctok], Sb)
                    p.mm(sl(b2, 3)[rows], intraT[:, rows], vn); yield
                    egl = sc_(9 + half)
                    p.stt(Sb, St, egl, sl(b3, 1), ALU.mult, ALU.add); yield
                    p.stt(St, St, egl, sl(b3, 1), ALU.mult, ALU.add); yield
                    tiv = F(4)
                    p.copy(tiv[rows], sl(b2, 3)[rows], eng="scalar"); yield
                    eG = V(pre_t[rows, d, 4, sc:sc + 1], ["pre%d_4" % d])
                    if first:
                        p.stt(oacc[rows], sl(b2, 2)[rows], eG, tiv[rows], ALU.mult, ALU.add); yield
                    else:
                        p.stt(F(5)[rows], sl(b2, 2)[rows], eG, tiv[rows], ALU.mult, ALU.add); yield
                        p.tt(oacc[rows], oacc[rows], F(5)[rows], ALU.add, eng="gpsimd"); yield

            def drive(gens):
                gens = list(gens)
                while gens:
                    for g in list(gens):
                        try:
                            next(g)
                        except StopIteration:
                            gens.remove(g)
            drive([prep_gen(0, 0, 0), prep_gen(1, NSC - 1, 0)])
            for step in range(NSC):
                gl = [scan_gen(0, step, step), scan_gen(1, NSC - 1 - step, step)]
                if step + 1 < NSC:
                    gl += [prep_gen(0, step + 1, step + 1), prep_gen(1, NSC - 2 - step, step + 1)]
                drive(gl)
            for c4 in range(4):
                st = V(stg_t[:, si % 2], ["stg%d" % (si % 2)])
                si += 1
                for q in range(16):
                    sc = c4 * 16 + q
                    bk = V(ps[:, sc % 2, 0:128], ["ps%d" % (sc % 2)])
                    p.tr(bk, V(oacc_t[:, sc, :], ["oacc%d" % sc]), C(C_IDENT))
                    evac(st[:, q * 128:(q + 1) * 128], bk)
                p.dma(dq(), V(S["oT"][h * 128:(h + 1) * 128, c4 * 2048:(c4 + 1) * 2048], []), st)
        p.finish("sync")
        p.emit(block)


def phase_D(nc, p, xsrc, xdst, W, S, l, last):
    NBk = NTOK // TB
    with _phase(nc) as es:
        def sb(name, shape, dtype):
            return es.enter_context(nc.sbuf_tensor("D%d_%s" % (l, name), shape, dtype))
        xT_t = sb("xTs", [128, 8, NTOK], F32)
        yh_t = sb("yh", [128, 8, NTOK], BF16)
        wbuf_t = sb("wbuf", [128, 12288], BF16)
        stg_t = sb("stg", [128, 2, 4096], F32)
        hg_t = sb("hg", [128, 2, 4, TB], BF16)
        s1_t = sb("s1", [128, 2, TB], F32)
        gT_t = sb("gT", [32, NTOK], F32)
        sel_t = sb("sels", [32, 32, 128], F32)
        idn_t = sb("idns", [128, 128], F32)
        ones_t = sb("ones", [128, 128], F32)
        sq_t = sb("sq", [128, 2, TB], F32)
        tmp_t = sb("tmp", [128, 2, TB], F32)
        rs_t = sb("rs", [128, TB], F32)
        rstd_t = sb("rstd", [128, TB], F32)
        small = sb("small", [128, 128], F32)
        wr_t = sb("wrs", [128, 8, 36], F32)
        rb_t = sb("rbs", [128, 36], F32)
        rt_t = sb("rt", [128, 2, 160], F32)
        ps = es.enter_context(nc.psum_tensor("D%d_ps" % l, [128, 8, 512], F32))
        block = es.enter_context(nc.Block())
        dq_i = [0]

        def dq():
            dq_i[0] += 1
            return "sync" if dq_i[0] % 2 else "gpsimd"
        stg_i = [0]

        def stage():
            i = stg_i[0] % 2
            stg_i[0] += 1
            return V(stg_t[:, i], ["stg%d" % i])

        def PS(b):
            return V(ps[:, b, :], ["ps%d" % b])
        ones = V(ones_t, ["ones"]); idn = V(idn_t, ["idn"]); sel = V(sel_t, ["sel"])
        p.memset(ones, 1.0)
        sm = V(small[:, 0:64], ["sm"])
        epsv = V(small[:, 120:121], ["epsv"])
        p.memset(epsv, EPS)
        EPSV[0] = epsv
        p.dma("sync", sm, V(W["smD%d" % l], []))
        p.dma("sync", idn, V(W["idn"], []))
        p.dma("gpsimd", sel, V(W["sel"], []))
        p.dma("sync", V(wr_t, ["wr"]), V(W["wr%d" % l].rearrange("(c p) n -> p c n", p=128), []))
        p.dma("sync", V(rb_t, ["rb"]), V(W["rb%d" % l], []))
        wr = V(wr_t, ["wr"]); rb = V(rb_t, ["rb"])
        cT = sm[:, 0:8]; nfw = sm[:, 8:16]; fw = sm[:, 16:24]; gnw = sm[:, 24:25]; bada = sm[:, 32:64]
        cond = V(small[:, 64:72], ["cond"]); mod_sb = V(small[:, 72:104], ["mod"]); A2 = V(small[:, 104:112], ["A2"])
        wst = [V(stg_t[:, i].rearrange("p (c n) -> p c n", c=8), ["stg%d" % i]) for i in range(2)]
        emit_mod(p, nc, None, cT, W["wada%d" % l][:, 2048:6144], bada, 32, V(ps[:, 0, 0:32], ["ps0"]), wst, cond, mod_sb)
        gate1 = mod_sb[:, 0:8]; B2 = mod_sb[:, 8:16]; gate2 = mod_sb[:, 24:32]
        p.stt(A2, mod_sb[:, 16:24], 1.0, nfw, ALU.add, ALU.mult)
        w1_d = W["w1_%d" % l]; w3_d = W["w3_%d" % l]; w2_d = W["w2_%d" % l]; wout_d = W["wout%d" % l]
        it = 0
        for s in range(NSH):
            t0 = s * NTOK
            tsl = slice(t0, t0 + NTOK)

            def xblk(kc, blk):
                return V(xT_t[:, kc, blk * TB:(blk + 1) * TB], ["x%d" % blk])

            def yblk(kc, blk):
                return V(yh_t[:, kc, blk * TB:(blk + 1) * TB], ["yh%d" % blk])
            allx = ["x%d" % b for b in range(NBk)]; ally = ["yh%d" % b for b in range(NBk)]
            for kc in range(8):
                p.dma(dq(), V(xT_t[:, kc, :], allx), V(xsrc[kc * 128:(kc + 1) * 128, tsl], []))
            for (src, k0) in ((S["ycT"], 0), (S["ynaT"], 2)):
                st = stage()
                p.dma(dq(), st, V(src[:, tsl].rearrange("(c p) n -> p c n", p=128), []))
                p.copy(V(yh_t[:, k0:k0 + 2, :], ally), V(st.ap.rearrange("p (c n) -> p c n", c=2), st.keys), eng="gpsimd")
            for h in range(4):
                st = stage()
                p.dma(dq(), st[:, 0:NTOK], V(S["oT"][h * 128:(h + 1) * 128, tsl], []))
                p.dma(dq(), st[:, NTOK:2 * NTOK], V(S["projT"][3072 + h * 128:3072 + (h + 1) * 128, tsl], []))
                for blk in range(NBk):
                    o = st[:, blk * TB:(blk + 1) * TB]; z = st[:, NTOK + blk * TB:NTOK + (blk + 1) * TB]
                    sq = V(sq_t[:, blk % 2], ["sq%d" % (blk % 2)]); tmp = V(tmp_t[:, blk % 2], ["tmp%d" % (blk % 2)])
                    rs = V(rs_t, ["rs"])
                    p.act(sq, o, AF.Square)
                    p.mm(PS(1), ones, sq)
                    p.act(rs, PS(1), AF.Sqrt, scale=1.0 / 128.0, bias=epsv)
                    p.recip(rs, rs)
                    p.stt(tmp, o, gnw, rs, ALU.mult, ALU.mult)
                    p.act(sq, z, AF.Silu)
                    p.tt(yblk(4 + h, blk), tmp, sq, ALU.mult)
            woutb = V(wbuf_t[:, 0:8192].rearrange("p (c n) -> p c n", c=8), ["w1b", "w3b"])
            for half in range(2):
                st = stage()
                p.dma(dq(), st, V(wout_d[:, half * 512:(half + 1) * 512].rearrange("(c p) n -> p c n", p=128), []))
                p.copy(woutb[:, :, half * 512:(half + 1) * 512], V(st.ap.rearrange("p (c n) -> p c n", c=8), st.keys), eng="gpsimd")
            gT = V(gT_t, ["gT"])
            for blk in range(NBk):
                for dtl in range(8):
                    bank = 1 + dtl % 4
                    for kc in range(8):
                        p.mm(PS(bank), woutb[:, kc, dtl * 128:(dtl + 1) * 128], yblk(kc, blk), start=(kc == 0), stop=(kc == 7))
                    p.stt(xblk(dtl, blk), PS(bank), gate1[:, dtl:dtl + 1], xblk(dtl, blk), ALU.mult, ALU.add)
                sq = [V(sq_t[:, i], ["sq%d" % i]) for i in range(2)]
                tmp = [V(tmp_t[:, i], ["tmp%d" % i]) for i in range(2)]
                st = stage()
                hF = V(st.ap.rearrange("p (c n) -> p c n", c=8), st.keys)
                xv = V(xT_t, ["x%d" % blk])
                hT = V(yh_t[:, :, blk * TB:(blk + 1) * TB], ["yh%d" % blk])
                emit_hmix_block(p, xv, blk, ones, sq, PS(0), V(rs_t, ["rs"]), V(rstd_t, ["rstd"]), tmp, A2, B2, hT, hF=hF)
                for tt_ in range(4):
                    L = V(ps[:, 5 + tt_ % 2, 0:36], ["ps%d" % (5 + tt_ % 2)])
                    for kc in range(8):
                        p.mm(L, hF[:, kc, tt_ * 128:(tt_ + 1) * 128], wr[:, kc, :], start=(kc == 0), stop=(kc == 7))
                    R = V(rt_t[:, tt_ % 2], ["rt%d" % (tt_ % 2)])
                    Lb = R[:, 0:36]; lg = R[:, 0:4]; le = R[:, 4:36]
                    m = R[:, 36:37]; negm = R[:, 37:38]; ohg = R[:, 40:44]; e4 = R[:, 44:48]; ssum = R[:, 38:39]
                    pgt = R[:, 39:40]; pen = R[:, 48:52]; lem = R[:, 52:84]; m1 = R[:, 84:85]; oh1 = R[:, 88:120]
                    lem2 = R[:, 120:152]; m2 = R[:, 85:86]; dd = R[:, 86:87]; ed = R[:, 87:88]
                    c1 = R[:, 152:153]; c2 = R[:, 153:154]; den = R[:, 154:155]
                    p.tt(Lb, L, rb, ALU.add)
                    p.reduce(m, lg, ALU.max)
                    p.ts(ohg, lg, m, None, ALU.is_equal)
                    p.ts(negm, m, -1.0, None, ALU.mult)
                    p.act(e4, lg, AF.Exp, bias=negm)
                    p.reduce(ssum, e4, ALU.add)
                    p.recip(pgt, ssum)
                    p.ts(pen, ohg, 1.0, 1e30, ALU.subtract, ALU.mult)
                    for g in range(4):
                        p.ts(lem[:, g * 8:(g + 1) * 8], le[:, g * 8:(g + 1) * 8], pen[:, g:g + 1], None, ALU.add)
                    p.reduce(m1, lem, ALU.max)
                    p.ts(oh1, lem, m1, None, ALU.is_equal)
                    p.stt(lem2, oh1, -1e30, lem, ALU.mult, ALU.add)
                    p.reduce(m2, lem2, ALU.max)
                    p.tt(dd, m2, m1, ALU.subtract)
                    p.act(ed, dd, AF.Exp)
                    p.ts(den, ed, 1.0, None, ALU.add)
                    p.recip(den, den)
                    p.tt(c1, den, pgt, ALU.mult)
                    p.tt(c2, c1, ed, ALU.mult)
                    p.ts(lem2, lem2, m2, None, ALU.is_equal)
                    p.ts(oh1, oh1, c1, None, ALU.mult)
                    p.stt(oh1, lem2, c2, oh1, ALU.mult, ALU.add)
                    gp = V(ps[0:32, 7, 0:128], ["ps7"])
                    p.tr(gp, oh1, idn)
                    c0 = blk * TB + tt_ * 128
                    p.copy(gT[:, c0:c0 + 128], gp, eng="scalar")
            w1b = V(wbuf_t[:, 0:4096].rearrange("p (c n) -> p c n", c=8), ["w1b"])
            w3b = V(wbuf_t[:, 4096:8192].rearrange("p (c n) -> p c n", c=8), ["w3b"])
            w2b = V(wbuf_t[:, 8192:12288].rearrange("p (c n) -> p c n", c=4), ["w2b"])
            def load_w(e, which):
                for (dst, src, cc, nm) in ((w1b, w1_d[e], 8, "h"), (w3b, w3_d[e], 8, "h"), (w2b, w2_d[e], 4, "y")):
                    if nm != which:
                        continue
                    st = stage()
                    p.dma(dq(), st, V(src.rearrange("(c p) n -> p c n", p=128), []))
                    p.copy(dst, V(st.ap.rearrange("p (c n) -> p c n", c=cc), st.keys), eng="gpsimd")

            def h_phase(e, blk, itx):
                if blk == 0:
                    load_w(e, "h")
                bsl = slice(blk * TB, (blk + 1) * TB)
                hT = V(yh_t[:, :, bsl], ["yh%d" % blk])
                gb = PS(4)
                p.mm(gb, sel[:, e, :], gT[:, bsl])
                hg = V(hg_t[:, itx % 2], ["hg%d" % (itx % 2)])
                for ht in range(4):
                    h1 = PS(ht % 2); h3 = PS(2 + ht % 2)
                    for kc in range(8):
                        p.mm(h1, w1b[:, kc, ht * 128:(ht + 1) * 128], hT[:, kc, :], start=(kc == 0), stop=(kc == 7))
                    for kc in range(8):
                        p.mm(h3, w3b[:, kc, ht * 128:(ht + 1) * 128], hT[:, kc, :], start=(kc == 0), stop=(kc == 7))
                    s1 = V(s1_t[:, ht % 2], ["s1%d" % (ht % 2)])
                    p.act(s1, h1, AF.Silu)
                    p.tt(s1, s1, gb, ALU.mult)
                    p.tt(hg[:, ht, :], h3, s1, ALU.mult)

            def y_phase(e, blk, itx):
                if blk == 0:
                    load_w(e, "y")
                hg = V(hg_t[:, itx % 2], ["hg%d" % (itx % 2)])
                for dtl in range(8):
                    yb = PS(5 + dtl % 3)
                    for ht in range(4):
                        p.mm(yb, w2b[:, ht, dtl * 128:(dtl + 1) * 128], hg[:, ht, :], start=(ht == 0), stop=(ht == 3))
                    p.stt(xblk(dtl, blk), yb, gate2[:, dtl:dtl + 1], xblk(dtl, blk), ALU.mult, ALU.add)
            seq = [(e, blk) for e in range(32) for blk in range(NBk)]
            h_phase(seq[0][0], seq[0][1], 0)
            for i, (e, blk) in enumerate(seq):
                if i + 1 < len(seq):
                    h_phase(seq[i + 1][0], seq[i + 1][1], i + 1)
                y_phase(e, blk, i)
            for blk in range(NBk):
                bsl = slice(blk * TB, (blk + 1) * TB)
                if last:
                    xv = V(xT_t, ["x%d" % blk])
                    for kc in range(8):
                        sqv = V(sq_t[:, kc % 2], ["sq%d" % (kc % 2)])
                        p.act(sqv, xv[:, kc, bsl], AF.Square)
                        p.mm(PS(0), ones, sqv, start=(kc == 0), stop=(kc == 7))
                    rs = V(rs_t, ["rs"]); rstd = V(rstd_t, ["rstd"])
                    p.act(rs, PS(0), AF.Sqrt, scale=1.0 / 1024.0, bias=epsv)
                    p.recip(rstd, rs)
                    for kc in range(8):
                        p.stt(xblk(kc, blk), xblk(kc, blk), fw[:, kc:kc + 1], rstd, ALU.mult, ALU.mult)
                for kc in range(8):
                    p.dma(dq(), V(xdst[kc * 128:(kc + 1) * 128, t0 + blk * TB:t0 + (blk + 1) * TB], []), xblk(kc, blk))
        p.finish("sync")
        p.emit(block)


FUSED_W_SHAPES = {
    "cT": [128, 8], "maskf": [128, 1024], "cst": [128, 11, 128], "sel": [32, 32, 128], "idn": [128, 128],
}
for _l in range(2):
    FUSED_W_SHAPES.update({
        "wada%d" % _l: [1024, 6144], "badaA%d" % _l: [128, 16], "nw%d" % _l: [128, 8], "win%d" % _l: [1024, 3600],
        "cw%d" % _l: [128, 2, 3], "gw%d" % _l: [128, 12, 3], "rpbg%d" % _l: [4, 128, 9, 1024], "par%d" % _l: [4, 128, 4],
        "smD%d" % _l: [128, 64], "wout%d" % _l: [1024, 1024], "wr%d" % _l: [1024, 36], "rb%d" % _l: [128, 36],
        "w1_%d" % _l: [32, 1024, 512], "w3_%d" % _l: [32, 1024, 512], "w2_%d" % _l: [32, 512, 1024],
    })


def build_F(nlayers=2, phases="ABCD"):
    nc = bass.Bass("TRN2", target_bir_lowering=False)
    dt = nc.dram_tensor
    xT_d = dt("xT", [1024, TSEQ], F32, kind="ExternalInput").ap()
    W = {k: dt(k, shp, F32, kind="ExternalInput").ap() for k, shp in FUSED_W_SHAPES.items()
         if not (k[-1].isdigit() and int(k[-1]) >= nlayers)}
    out_d = dt("outT", [1024, TSEQ], F32, kind="ExternalOutput").ap()
    S = {
        "projT": dt("s_projT", [3600, TSEQ], F32, kind="Internal").ap(),
        "nvtok": dt("s_nvtok", [TSEQ, 256], F32, kind="Internal").ap(),
        "abtok": dt("s_abtok", [TSEQ, 16], F32, kind="Internal").ap(),
        "ycT": dt("s_ycT", [256, TSEQ], F32, kind="Internal").ap(),
        "ynaT": dt("s_ynaT", [256, TSEQ], F32, kind="Internal").ap(),
        "gqkvn": dt("s_gqkvn", [1536, TSEQ], F32, kind="Internal").ap(),
        "oT": dt("s_oT", [512, TSEQ], F32, kind="Internal").ap(),
    }
    x1_d = dt("s_x1T", [1024, TSEQ], F32, kind="Internal").ap()
    dbg = {}
    if phases != "ABCD":
        for k, v in S.items():
            dbg[k] = dt("dbg_" + k, list(v.shape), F32, kind="ExternalOutput").ap()
    p = Prog(nc)
    for l in range(nlayers):
        xsrc = xT_d if l == 0 else x1_d
        last = (l == nlayers - 1)
        xdst = out_d if last else x1_d
        if "A" in phases:
            phase_A(nc, p, xsrc, W, S, l)
        if "B" in phases:
            phase_B(nc, p, W, S, l)
        if "C" in phases:
            phase_C(nc, p, W, S, l)
        if "D" in phases:
            phase_D(nc, p, xsrc, xdst, W, S, l, last and nlayers == 2)
    if dbg:
        with nc.Block() as block:
            i = 0
            for k in S:
                p.dma("sync" if i % 2 else "gpsimd", V(dbg[k], []), V(S[k], []))
                i += 1
            p.finish("sync")
            p.emit(block)
    return nc


def prep_F(P, nlayers=2):
    table_mask = None
    ins = []
    sel = np.zeros((32, 32, 128), np.float32)
    for e in range(32):
        sel[e, e, :] = 1.0
    idn = np.eye(128, dtype=np.float32)
    cst = gdn_consts()
    shared = {}
    for l in range(nlayers):
        table, maskf = _na_tables(P["na_rpb"][l])
        rpbg = np.zeros((4, 128, 9, 1024), np.float32)
        for s in range(4):
            rpbg[s, :, 0] = table(3)
            for si, r in enumerate(SPEC_ROWS):
                R = s * 32 + r
                rs = min(max(R - 4, 0), 120)
                rpbg[s, :, 1 + si] = table(rs - R + 7)
        par = np.zeros((4, 128, 4), np.float32)
        for h in range(4):
            par[h, :, 0] = P["gdn_a_log"][l][0, h]; par[h, :, 1] = P["gdn_a_log"][l][1, h]
            par[h, :, 2] = P["gdn_dt_bias"][l][0, h]; par[h, :, 3] = P["gdn_dt_bias"][l][1, h]
        rbv = np.concatenate([P["router_group_b"][l], P["router_expert_b"][l]])
        shared.update({
            "maskf": maskf,
            "wada%d" % l: P["w_ada"][l], "badaA%d" % l: _lay_pc(P["b_ada"][l][0:2048], 16),
            "nw%d" % l: _lay_pc(P["norm_mix_w"][l], 8), "win%d" % l: P["w_in"][l],
            "cw%d" % l: np.ascontiguousarray(P["conv_a_w"][l].T.reshape(2, 128, 3).transpose(1, 0, 2)),
            "gw%d" % l: np.ascontiguousarray(P["gdn_conv_w"][l].T.reshape(12, 128, 3).transpose(1, 0, 2)),
            "rpbg%d" % l: rpbg, "par%d" % l: par,
            "wout%d" % l: P["w_out"][l],
            "wr%d" % l: np.ascontiguousarray(np.concatenate([P["router_group_w"][l], P["router_expert_w"][l]], 1)),
            "rb%d" % l: np.ascontiguousarray(np.broadcast_to(rbv[None, :], (128, 36))).astype(np.float32),
            "w1_%d" % l: P["expert_w1"][l], "w3_%d" % l: P["expert_w3"][l], "w2_%d" % l: P["expert_w2"][l],
        })
    shared.update({"cst": cst, "sel": sel, "idn": idn})
    for b in range(2):
        d = dict(shared)
        d["xT"] = np.ascontiguousarray(P["x"][b].T)
        d["cT"] = _lay_pc(P["c"][b], 8)
        for l in range(nlayers):
            sm = np.zeros((128, 64), np.float32)
            sm[:, 0:8] = _lay_pc(P["c"][b], 8)
            sm[:, 8:16] = _lay_pc(P["norm_ffn_w"][l], 8)
            sm[:, 16:24] = _lay_pc(P["final_norm_w"], 8)
            sm[:, 24] = P["gdn_norm_w"][l]
            sm[:, 32:64] = _lay_pc(P["b_ada"][l][2048:6144], 32)
            d["smD%d" % l] = sm
        ins.append(d)
    return ins

_NC = {}


def kernel(**inputs):
    P = {k: np.ascontiguousarray(np.asarray(v, dtype=np.float32)) for k, v in inputs.items()}
    if "F" not in _NC:
        _NC["F"] = build_F()
    ins = prep_F(P)
    res = run_bass_kernel_spmd(_NC["F"], ins, core_ids=[0, 1]).results
    out = np.stack([np.ascontiguousarray(res[b]["outT"].T) for b in range(2)], 0)
    return out.astype(np.float32)
```
